# Optimizing a Trainium2 kernel written in Bass

```python
import math
import jax
import jax.numpy as jnp
from jax import lax
import numpy as np

D_MODEL = 1024
BATCH = 8
SEQ = 4096
DEPTH = 2

N_MEM = 256
EPS = 1e-6
ROPE_THETA = 10000.0
ATT_HEADS = 8
ATT_HEAD_DIM = 64
ATT_WIDTH = ATT_HEADS * ATT_HEAD_DIM
POOL_WIDTH = D_MODEL - ATT_WIDTH
POOL_WINDOWS = (2, 4, 8, 16)
POOL_GROUPS = len(POOL_WINDOWS)
POOL_GROUP_DIM = POOL_WIDTH // POOL_GROUPS
HY_IN_DIM = 3 * ATT_WIDTH + POOL_WIDTH
MOBA_BLOCK = 256
MOBA_TOPK = 3
Q_CHUNK = 128
SSD_EXPAND = 2
SSD_D_INNER = SSD_EXPAND * D_MODEL
SSD_HEAD_DIM = 64
SSD_HEADS = SSD_D_INNER // SSD_HEAD_DIM
SSD_GROUPS = 4
SSD_STATE = 128
SSD_CONV = 4
SSD_CHUNK = 128
SSD_CONV_DIM = SSD_D_INNER + 2 * SSD_GROUPS * SSD_STATE
SSD_IN_DIM = SSD_D_INNER + SSD_CONV_DIM + SSD_HEADS
DT_MIN = 0.001
DT_MAX = 0.1
XA_HEADS = 4
XA_HEAD_DIM = D_MODEL // XA_HEADS
D_FF = 2816
N_EXPERTS = 8
TOP_K = 2
D_FF_EXPERT = 3584
MOE_BLOCK = 256

kernel_name = 'hybrid_moba_pool_ssd_moe'


def _rmsnorm(x, gain):
    xf = x.astype(jnp.float32)
    y = xf * lax.rsqrt(jnp.mean(xf * xf, axis=-1, keepdims=True) + EPS)
    return (y * gain.astype(jnp.float32)).astype(x.dtype)


def _rope(x, positions):
    half = x.shape[-1] // 2
    inv_freq = ROPE_THETA ** (-jnp.arange(half, dtype=jnp.float32) / half)
    ang = positions.astype(jnp.float32)[:, None] * inv_freq[None, :]
    cos = jnp.cos(ang).astype(x.dtype)
    sin = jnp.sin(ang).astype(x.dtype)
    x1, x2 = x[..., :half], x[..., half:]
    return jnp.concatenate([x1 * cos - x2 * sin, x2 * cos + x1 * sin], axis=-1)


def _moba_attention(q, k, v):
    b, h, s, dh = q.shape
    n_blk = -(-s // MOBA_BLOCK)
    pad = n_blk * MOBA_BLOCK - s
    kp = jnp.pad(k, ((0, 0), (0, 0), (0, pad), (0, 0)))
    vp = jnp.pad(v, ((0, 0), (0, 0), (0, pad), (0, 0)))
    k_blocks = kp.reshape(b, h, n_blk, MOBA_BLOCK, dh)
    v_blocks = vp.reshape(b, h, n_blk, MOBA_BLOCK, dh)
    k_mean = jnp.mean(k_blocks.astype(jnp.float32), axis=3)
    scale = dh ** -0.5
    n_gate = max(n_blk, MOBA_TOPK)
    bi = jnp.arange(b)[:, None, None, None]
    hi = jnp.arange(h)[None, :, None, None]
    blk_ids = jnp.arange(n_blk)
    slot_ids = jnp.arange(MOBA_TOPK)
    local = jnp.arange(MOBA_BLOCK)

    def chunk(c):
        q0 = c * Q_CHUNK
        own = q0 // MOBA_BLOCK
        qc = lax.dynamic_slice_in_dim(q, q0, Q_CHUNK, axis=2)
        qpos = q0 + jnp.arange(Q_CHUNK)
        gate = jnp.einsum('bhqd,bhnd->bhqn', qc.astype(jnp.float32), k_mean)
        gate = jnp.where(blk_ids < own, gate, -jnp.inf)
        if n_gate > n_blk:
            gate = jnp.pad(gate, ((0, 0), (0, 0), (0, 0), (0, n_gate - n_blk)),
                           constant_values=-jnp.inf)
        _, idx = lax.top_k(gate, MOBA_TOPK)
        idx = jnp.minimum(idx, n_blk - 1)
        slot_ok = slot_ids < own
        kg = k_blocks[bi, hi, idx]
        vg = v_blocks[bi, hi, idx]
        s_sel = jnp.einsum('bhqd,bhqjkd->bhqjk', qc, kg).astype(jnp.float32) * scale
        s_sel = jnp.where(slot_ok[:, None], s_sel, -jnp.inf)
        s_sel = s_sel.reshape(b, h, Q_CHUNK, MOBA_TOPK * MOBA_BLOCK)
        k_own = lax.dynamic_slice_in_dim(kp, own * MOBA_BLOCK, MOBA_BLOCK, axis=2)
        v_own = lax.dynamic_slice_in_dim(vp, own * MOBA_BLOCK, MOBA_BLOCK, axis=2)
        kpos = own * MOBA_BLOCK + local
        s_own = jnp.einsum('bhqd,bhkd->bhqk', qc, k_own).astype(jnp.float32) * scale
        s_own = jnp.where(kpos[None, :] <= qpos[:, None], s_own, -jnp.inf)
        p = jax.nn.softmax(jnp.concatenate([s_sel, s_own], axis=-1), axis=-1).astype(v.dtype)
        p_sel = p[..., :MOBA_TOPK * MOBA_BLOCK].reshape(b, h, Q_CHUNK, MOBA_TOPK, MOBA_BLOCK)
        p_own = p[..., MOBA_TOPK * MOBA_BLOCK:]
        return (jnp.einsum('bhqjk,bhqjkd->bhqd', p_sel, vg)
                + jnp.einsum('bhqk,bhkd->bhqd', p_own, v_own))

    outs = lax.map(chunk, jnp.arange(s // Q_CHUNK))
    return outs.transpose(1, 2, 0, 3, 4).reshape(b, h, s, dh)


def _multiscale_pool(u, w_pool, pool_scale):
    b, s, _ = u.shape
    uf = u.astype(jnp.float32)
    cs = jnp.concatenate([jnp.zeros((b, 1, POOL_WIDTH), jnp.float32),
                          jnp.cumsum(uf, axis=1)], axis=1)
    t = jnp.arange(s)
    groups = []
    for g, win in enumerate(POOL_WINDOWS):
        sl = slice(g * POOL_GROUP_DIM, (g + 1) * POOL_GROUP_DIM)
        cs_g = cs[..., sl]
        lo = jnp.maximum(t + 1 - win, 0)
        cnt = jnp.minimum(t + 1, win).astype(jnp.float32)
        groups.append((cs_g[:, 1:] - cs_g[:, lo]) / cnt[None, :, None] - uf[..., sl])
    pooled = jnp.stack(groups, axis=2).astype(u.dtype)
    mixed = jnp.einsum('bsgc,gcd->bsgd', pooled, w_pool).reshape(b, s, POOL_WIDTH)
    return mixed * pool_scale


def _attn_pool_mixer(hn, positions, w_in, q_norm, k_norm, w_pool, pool_scale, w_out):
    b, s, _ = hn.shape
    proj = hn @ w_in
    q, k, v, u = jnp.split(proj, [ATT_WIDTH, 2 * ATT_WIDTH, 3 * ATT_WIDTH], axis=-1)

    def heads(t):
        return t.reshape(b, s, ATT_HEADS, ATT_HEAD_DIM).transpose(0, 2, 1, 3)

    q = _rope(_rmsnorm(heads(q), q_norm), positions)
    k = _rope(_rmsnorm(heads(k), k_norm), positions)
    a = _moba_attention(q, k, heads(v)).transpose(0, 2, 1, 3).reshape(b, s, ATT_WIDTH)
    p = _multiscale_pool(u, w_pool, pool_scale)
    return jnp.concatenate([a, p.astype(a.dtype)], axis=-1) @ w_out


def _ssd_chunked(x, dt, a, bmat, cmat):
    b, s, h, p = x.shape
    l = SSD_CHUNK
    c = s // l
    g = SSD_GROUPS
    r = h // g
    n = bmat.shape[-1]
    xdt = (x.astype(jnp.float32) * dt[..., None]).reshape(b, c, l, g, r, p)
    da = (dt * a).reshape(b, c, l, g, r)
    bm = bmat.astype(jnp.float32).reshape(b, c, l, g, n)
    cm = cmat.astype(jnp.float32).reshape(b, c, l, g, n)
    a_cum = jnp.cumsum(da, axis=2)
    causal = jnp.tril(jnp.ones((l, l), bool))
    seg = a_cum[:, :, :, None] - a_cum[:, :, None, :]
    decay = jnp.exp(jnp.where(causal[None, None, :, :, None, None], seg, -jnp.inf))
    cb = jnp.einsum('bctgn,bcsgn->bctsg', cm, bm)
    y_diag = jnp.einsum('bctsg,bctsgr,bcsgrp->bctgrp', cb, decay, xdt)
    to_end = jnp.exp(a_cum[:, :, -1:] - a_cum)
    states = jnp.einsum('bcsgn,bcsgr,bcsgrp->bcgrpn', bm, to_end, xdt)
    chunk_decay = jnp.exp(a_cum[:, :, -1])

    def carry_state(state, inp):
        st, dec = inp
        return state * dec[..., None, None] + st, state

    init = jnp.zeros((b, g, r, p, n), jnp.float32)
    _, s_in = lax.scan(carry_state, init,
                       (jnp.moveaxis(states, 1, 0), jnp.moveaxis(chunk_decay, 1, 0)))
    s_in = jnp.moveaxis(s_in, 0, 1)
    y_off = jnp.einsum('bctgn,bcgrpn,bctgr->bctgrp', cm, s_in, jnp.exp(a_cum))
    return (y_diag + y_off).reshape(b, s, h, p)


def _ssd_mixer(hn, w_in, conv_w, conv_b, dt_bias, a_log, d_skip, norm_g, w_out):
    b, s, _ = hn.shape
    proj = hn @ w_in
    z, xbc, dt = jnp.split(proj, [SSD_D_INNER, SSD_D_INNER + SSD_CONV_DIM], axis=-1)
    xpad = jnp.pad(xbc, ((0, 0), (SSD_CONV - 1, 0), (0, 0)))
    conv = xpad[:, 0:s] * conv_w[0]
    for i in range(1, SSD_CONV):
        conv = conv + xpad[:, i:i + s] * conv_w[i]
    xbc = jax.nn.silu(conv + conv_b)
    xs, bmat, cmat = jnp.split(xbc, [SSD_D_INNER, SSD_D_INNER + SSD_GROUPS * SSD_STATE], axis=-1)
    xs = xs.reshape(b, s, SSD_HEADS, SSD_HEAD_DIM)
    bmat = bmat.reshape(b, s, SSD_GROUPS, SSD_STATE)
    cmat = cmat.reshape(b, s, SSD_GROUPS, SSD_STATE)
    dt = jax.nn.softplus(dt.astype(jnp.float32) + dt_bias.astype(jnp.float32))
    a = -jnp.exp(a_log.astype(jnp.float32))
    y = _ssd_chunked(xs, dt, a, bmat, cmat)
    y = (y + d_skip.astype(jnp.float32)[:, None] * xs.astype(jnp.float32)).astype(hn.dtype)
    y = (y.reshape(b, s, SSD_D_INNER) * jax.nn.silu(z)).reshape(b, s, SSD_GROUPS, -1)
    y = _rmsnorm(y, norm_g.reshape(SSD_GROUPS, -1)).reshape(b, s, SSD_D_INNER)
    return y @ w_out


def _memory_cross_attention(hn, mem_n, w_q, w_kv, q_norm, k_norm, w_o):
    b, s, _ = hn.shape
    m = mem_n.shape[1]
    q = _rmsnorm((hn @ w_q).reshape(b, s, XA_HEADS, XA_HEAD_DIM), q_norm)
    k, v = jnp.split(mem_n @ w_kv, 2, axis=-1)
    k = _rmsnorm(k.reshape(b, m, XA_HEADS, XA_HEAD_DIM), k_norm)
    v = v.reshape(b, m, XA_HEADS, XA_HEAD_DIM)
    sc = jnp.einsum('bshd,bmhd->bhsm', q, k).astype(jnp.float32) * (XA_HEAD_DIM ** -0.5)
    p = jax.nn.softmax(sc, axis=-1).astype(v.dtype)
    o = jnp.einsum('bhsm,bmhd->bshd', p, v).reshape(b, s, D_MODEL)
    return o @ w_o


def _swiglu(hn, w_gate, w_up, w_down):
    return (jax.nn.silu(hn @ w_gate) * (hn @ w_up)) @ w_down


def _moe_swiglu(hn, w_router, w_gate, w_up, w_down):
    b, s, d = hn.shape
    n = b * s
    xt = hn.reshape(n, d)
    logits = (xt @ w_router).astype(jnp.float32)
    top_logit, top_e = lax.top_k(logits, TOP_K)
    gates = jax.nn.softmax(top_logit, axis=-1)
    e_flat = top_e.reshape(-1)
    tok_flat = jnp.arange(n * TOP_K, dtype=jnp.int32) // TOP_K
    g_flat = gates.reshape(-1)
    order = jnp.argsort(e_flat)
    e_sorted = e_flat[order]
    tok_sorted = tok_flat[order]
    g_sorted = g_flat[order]
    counts = jnp.zeros((N_EXPERTS,), jnp.int32).at[e_flat].add(1)
    start = jnp.cumsum(counts) - counts
    padded = (counts + MOE_BLOCK - 1) // MOE_BLOCK * MOE_BLOCK
    pad_end = jnp.cumsum(padded)
    pad_start = pad_end - padded
    rank = jnp.arange(n * TOP_K, dtype=jnp.int32) - start[e_sorted]
    dest = pad_start[e_sorted] + rank
    n_rows = (-(-(n * TOP_K) // MOE_BLOCK) + N_EXPERTS) * MOE_BLOCK
    n_blocks = n_rows // MOE_BLOCK
    xbuf = jnp.zeros((n_rows, d), hn.dtype).at[dest].set(xt[tok_sorted])
    blk_start = jnp.arange(n_blocks, dtype=jnp.int32) * MOE_BLOCK
    blk_e = jnp.minimum(jnp.sum(blk_start[:, None] >= pad_end[None, :], axis=1), N_EXPERTS - 1)

    def expert_block(args):
        xb, e = args
        return (jax.nn.silu(xb @ w_gate[e]) * (xb @ w_up[e])) @ w_down[e]

    ybuf = lax.map(expert_block, (xbuf.reshape(n_blocks, MOE_BLOCK, d), blk_e)).reshape(n_rows, d)
    y = jax.ops.segment_sum(ybuf[dest] * g_sorted[:, None].astype(ybuf.dtype), tok_sorted,
                            num_segments=n)
    return y.reshape(b, s, d)


def setup_inputs(seed: int = 0) -> dict:
    key = jax.random.key(seed)
    ks = iter(jax.random.split(key, 64))
    f32 = jnp.float32
    ne = (DEPTH + 1) // 2
    no = DEPTH // 2

    def w(shape, fan_in):
        return jax.random.normal(next(ks), shape, f32) * (fan_in ** -0.5)

    def gain(shape, s=0.02):
        return 1.0 + s * jax.random.normal(next(ks), shape, f32)

    x = jax.random.normal(next(ks), (BATCH, SEQ, D_MODEL), f32)
    mem = jax.random.normal(next(ks), (BATCH, N_MEM, D_MODEL), f32)
    positions = jnp.arange(SEQ, dtype=jnp.int32)
    dt0 = jnp.exp(jax.random.uniform(next(ks), (no, SSD_HEADS), f32,
                                     math.log(DT_MIN), math.log(DT_MAX)))
    ssd_dt_bias = dt0 + jnp.log(-jnp.expm1(-dt0))
    ssd_a_log = jnp.log(jax.random.uniform(next(ks), (no, SSD_HEADS), f32, 1.0, 16.0))
    return {
        'x': x,
        'mem': mem,
        'positions': positions,
        'mix_norm': gain((DEPTH, D_MODEL)),
        'xa_norm': gain((DEPTH, D_MODEL)),
        'mem_norm': gain((DEPTH, D_MODEL)),
        'ffn_norm': gain((DEPTH, D_MODEL)),
        'xa_wq': w((DEPTH, D_MODEL, D_MODEL), D_MODEL),
        'xa_wkv': w((DEPTH, D_MODEL, 2 * D_MODEL), D_MODEL),
        'xa_q_norm': gain((DEPTH, XA_HEAD_DIM)),
        'xa_k_norm': gain((DEPTH, XA_HEAD_DIM)),
        'xa_wo': w((DEPTH, D_MODEL, D_MODEL), D_MODEL),
        'hy_w_in': w((ne, D_MODEL, HY_IN_DIM), D_MODEL),
        'hy_q_norm': gain((ne, ATT_HEAD_DIM)),
        'hy_k_norm': gain((ne, ATT_HEAD_DIM)),
        'pool_w': w((ne, POOL_GROUPS, POOL_GROUP_DIM, POOL_GROUP_DIM), POOL_GROUP_DIM),
        'pool_scale': gain((ne, POOL_WIDTH), 0.1),
        'hy_w_out': w((ne, D_MODEL, D_MODEL), D_MODEL),
        'ffn_w_gate': w((ne, D_MODEL, D_FF), D_MODEL),
        'ffn_w_up': w((ne, D_MODEL, D_FF), D_MODEL),
        'ffn_w_down': w((ne, D_FF, D_MODEL), D_FF),
        'ssd_w_in': w((no, D_MODEL, SSD_IN_DIM), D_MODEL),
        'ssd_conv_w': jax.random.uniform(next(ks), (no, SSD_CONV, SSD_CONV_DIM), f32, -0.5, 0.5),
        'ssd_conv_b': 0.01 * jax.random.normal(next(ks), (no, SSD_CONV_DIM), f32),
        'ssd_dt_bias': ssd_dt_bias,
        'ssd_a_log': ssd_a_log,
        'ssd_d': gain((no, SSD_HEADS), 0.1),
        'ssd_norm': gain((no, SSD_D_INNER)),
        'ssd_w_out': w((no, SSD_D_INNER, D_MODEL), SSD_D_INNER),
        'moe_router': w((no, D_MODEL, N_EXPERTS), D_MODEL),
        'moe_w_gate': w((no, N_EXPERTS, D_MODEL, D_FF_EXPERT), D_MODEL),
        'moe_w_up': w((no, N_EXPERTS, D_MODEL, D_FF_EXPERT), D_MODEL),
        'moe_w_down': w((no, N_EXPERTS, D_FF_EXPERT, D_MODEL), D_FF_EXPERT),
    }


def reference(x, mem, positions, mix_norm, xa_norm, mem_norm, ffn_norm, xa_wq, xa_wkv,
              xa_q_norm, xa_k_norm, xa_wo, hy_w_in, hy_q_norm, hy_k_norm, pool_w, pool_scale,
              hy_w_out, ffn_w_gate, ffn_w_up, ffn_w_down, ssd_w_in, ssd_conv_w, ssd_conv_b,
              ssd_dt_bias, ssd_a_log, ssd_d, ssd_norm, ssd_w_out, moe_router, moe_w_gate,
              moe_w_up, moe_w_down):
    h = x
    for layer in range(DEPTH):
        j = layer // 2
        hn = _rmsnorm(h, mix_norm[layer])
        if layer % 2 == 0:
            h = h + _attn_pool_mixer(hn, positions, hy_w_in[j], hy_q_norm[j], hy_k_norm[j],
                                     pool_w[j], pool_scale[j], hy_w_out[j])
        else:
            h = h + _ssd_mixer(hn, ssd_w_in[j], ssd_conv_w[j], ssd_conv_b[j], ssd_dt_bias[j],
                               ssd_a_log[j], ssd_d[j], ssd_norm[j], ssd_w_out[j])
        h = h + _memory_cross_attention(_rmsnorm(h, xa_norm[layer]),
                                        _rmsnorm(mem, mem_norm[layer]), xa_wq[layer],
                                        xa_wkv[layer], xa_q_norm[layer], xa_k_norm[layer],
                                        xa_wo[layer])
        hn = _rmsnorm(h, ffn_norm[layer])
        if layer % 2 == 0:
            h = h + _swiglu(hn, ffn_w_gate[j], ffn_w_up[j], ffn_w_down[j])
        else:
            h = h + _moe_swiglu(hn, moe_router[j], moe_w_gate[j], moe_w_up[j], moe_w_down[j])
    return h
```

```python
from concourse.bass_utils import run_bass_kernel_spmd
import numpy as np
import concourse.bass as bass
import concourse.mybir as mybir
from contextlib import ExitStack

F32 = mybir.dt.float32
BF16 = mybir.dt.bfloat16
I32 = mybir.dt.int32
AF = mybir.ActivationFunctionType
ALU = mybir.AluOpType
AX = mybir.AxisListType


class Res:
    __slots__ = ("w", "r", "name")

    def __init__(self, name=""):
        self.w = None
        self.r = {}
        self.name = name


class KB:
    RING = {"sp": 16, "pool": 12, "act": 4}

    def __init__(self, nc, es):
        self.nc = nc
        self.es = es
        self.eng = {"pe": nc.tensor, "dve": nc.vector, "act": nc.scalar,
                    "pool": nc.gpsimd, "sp": nc.sync}
        self.sems = {}
        for e in ("pe", "dve", "act", "pool"):
            self.sems[e] = es.enter_context(nc.semaphore("c_" + e))
        self.cnt = {e: 0 for e in ("pe", "dve", "act", "pool")}
        self.ring = {}
        self.ring_i = {}
        for q, n in self.RING.items():
            self.ring[q] = []
            for i in range(n):
                key = f"d_{q}{i}"
                self.sems[key] = es.enter_context(nc.semaphore(key))
                self.ring[q].append(key)
            self.ring_i[q] = 0
        self.seen = {e: {} for e in self.eng}
        self.n_inst = 0
        self.n_wait = 0
        self.out_tokens = []

    def sb(self, name, shape, dtype):
        self.uid = getattr(self, "uid", 0) + 1
        t = self.es.enter_context(self.nc.sbuf_tensor(f"s{self.uid}_" + name, list(shape), dtype))
        return t, Res(name)

    def ps(self, name, shape, dtype):
        t = self.es.enter_context(self.nc.psum_tensor("p_" + name, list(shape), dtype))
        return t, Res(name)

    def _waits(self, e, R, W):
        need = {}
        for r in R:
            if r.w is not None:
                k, v = r.w
                if need.get(k, 0) < v:
                    need[k] = v
        for w in W:
            if w.w is not None:
                k, v = w.w
                if need.get(k, 0) < v:
                    need[k] = v
            for k, v in w.r.items():
                if need.get(k, 0) < v:
                    need[k] = v
        seen = self.seen[e]
        eng = self.eng[e]
        for k, v in need.items():
            if e == "pe" and k == "pe":
                continue
            if seen.get(k, 0) >= v:
                continue
            eng.wait_ge(self.sems[k], v)
            seen[k] = v
            self.n_wait += 1

    def _commit(self, tok, R, W):
        k, v = tok
        for r in R:
            if r.r.get(k, 0) < v:
                r.r[k] = v
        for w in W:
            w.w = tok
            w.r = {}

    def I(self, e, fn, R=(), W=(), inc=True):
        self._waits(e, R, W)
        ins = fn(self.eng[e])
        self.n_inst += 1
        if inc:
            self.cnt[e] += 1
            ins.then_inc(self.sems[e], 1)
            tok = (e, self.cnt[e])
        else:
            tok = (e, self.cnt[e] + 1)
        self._commit(tok, R, W)
        return tok

    def dma(self, q, out, in_, R=(), W=(), **kw):
        e = q
        i = self.ring_i[q]
        n = len(self.ring[q])
        key = self.ring[q][i % n]
        prev = 16 * (i // n)
        eng = self.eng[e]
        if prev > 0 and self.seen[e].get(key, 0) < prev:
            eng.wait_ge(self.sems[key], prev)
            self.seen[e][key] = prev
        self._waits(e, R, W)
        ins = eng.dma_start(out=out, in_=in_, **kw)
        ins.then_inc(self.sems[key], 16)
        self.ring_i[q] = i + 1
        self.n_inst += 1
        tok = (key, prev + 16)
        self._commit(tok, R, W)
        return tok

    def finish(self, toks):
        eng = self.eng["sp"]
        for k, v in toks:
            eng.wait_ge(self.sems[k], v)


def kb_barrier(self):
    for e, eng in self.eng.items():
        for x in ("pe", "dve", "act", "pool"):
            v = self.cnt[x]
            if v > 0 and self.seen[e].get(x, 0) < v and not (e == x):
                eng.wait_ge(self.sems[x], v)
                self.seen[e][x] = v
        for q in self.ring:
            n = len(self.ring[q])
            tot = self.ring_i[q]
            for j in range(min(n, tot)):
                idx = tot - 1 - j
                key = self.ring[q][idx % n]
                v = 16 * (idx // n + 1)
                if self.seen[e].get(key, 0) < v:
                    eng.wait_ge(self.sems[key], v)
                    self.seen[e][key] = v


KB.barrier = kb_barrier


import numpy as np

S = 4096
D = 1024
T = 512
NT = S // T
EPS = 1e-6
NEG = -30000.0

CST = {}
_c = 0
def _col(name, n):
    global _c
    CST[name] = _c
    _c += n
for _n in ("mix0", "mix1", "xa0", "xa1", "mem0", "mem1", "ffn0", "ffn1"):
    _col(_n, 8)
_col("hyq", 1); _col("hyk", 1); _col("pscale", 4)
_col("xaq0", 2); _col("xaq1", 2); _col("xak0", 2); _col("xak1", 2)
_col("invf", 1)
_col("convw", 96); _col("convb", 24); _col("ssdn", 16)
_col("rcnt", 64)
NCST = _c

CM = {"ident": 0, "ones": 128, "bones": 256, "rot": 384, "tri01": 512, "upper": 640}
NCM = 768


def host_consts(inp):
    c = np.zeros((128, NCST), np.float32)
    def put(name, v):
        v = np.asarray(v, np.float32).reshape(-1, 128).T
        c[:, CST[name]:CST[name] + v.shape[1]] = v
    for l in range(2):
        put(f"mix{l}", inp["mix_norm"][l]); put(f"xa{l}", inp["xa_norm"][l])
        put(f"mem{l}", inp["mem_norm"][l]); put(f"ffn{l}", inp["ffn_norm"][l])
        put(f"xaq{l}", inp["xa_q_norm"][l]); put(f"xak{l}", inp["xa_k_norm"][l])
    put("hyq", np.tile(inp["hy_q_norm"][0], 2)); put("hyk", np.tile(inp["hy_k_norm"][0], 2))
    put("pscale", inp["pool_scale"][0])
    invf = (10000.0 ** (-np.arange(32, dtype=np.float32) / 32)).astype(np.float32)
    put("invf", np.tile(invf, 4))
    cw = inp["ssd_conv_w"][0]
    cwl = np.zeros((128, 96), np.float32)
    for ch in range(24):
        for i in range(4):
            cwl[:, 4 * ch + i] = cw[i, ch * 128:(ch + 1) * 128]
    c[:, CST["convw"]:CST["convw"] + 96] = cwl
    put("convb", inp["ssd_conv_b"][0]); put("ssdn", inp["ssd_norm"][0])
    rc = np.zeros((128, 64), np.float32)
    for g, w in enumerate((2, 4, 8, 16)):
        rc[:, g * 16:(g + 1) * 16] = 1.0 / np.minimum(np.arange(16) + 1, w)
    c[:, CST["rcnt"]:CST["rcnt"] + 64] = rc
    m = np.zeros((128, NCM), np.float32)
    m[:, 0:128] = np.eye(128)
    m[:, 128:256] = 1.0
    for p in range(128):
        for q in range(128):
            if p // 64 == q // 64:
                m[p, 256 + q] = 1.0
    for p in range(128):
        if p % 64 < 32:
            m[p + 32, 384 + p] = -1.0
        else:
            m[p - 32, 384 + p] = 1.0
    qi = np.arange(128)[:, None]; ki = np.arange(128)[None, :]
    m[:, 512:640] = (ki <= qi).astype(np.float32)
    m[:, 640:768] = (qi <= ki).astype(np.float32)
    return c, m


def host_rowc(inp):
    r = np.zeros((128, 96), np.float32)
    r[:, 0:32] = inp["ssd_dt_bias"][0][None, :]
    r[:, 32:64] = inp["ssd_a_log"][0][None, :]
    r[:, 64:96] = inp["ssd_d"][0][None, :]
    return r


class Ctx:
    pass


def setup_common(k, nc, cst_d, cm_d):
    C = Ctx()
    C.cst, C.cst_r = k.sb("cst", [128, NCST], F32)
    C.cmf, C.cmf_r = k.sb("cmf", [128, NCM], F32)
    C.cmb, C.cmb_r = k.sb("cmb", [128, NCM], BF16)
    C.trib, C.trib_r = k.sb("trib", [128, 128], F32)
    k.dma("sp", C.cst[:], cst_d[:, :], W=[C.cst_r])
    k.dma("sp", C.cmf[:], cm_d[:, :], W=[C.cmf_r])
    k.I("dve", lambda e: e.tensor_copy(out=C.cmb[:], in_=C.cmf[:]), R=[C.cmf_r], W=[C.cmb_r])
    k.I("dve", lambda e: e.tensor_scalar(out=C.trib[:], in0=C.cmf[:, 512:640], scalar1=-1.0, scalar2=-NEG,
                                          op0=ALU.add, op1=ALU.mult), R=[C.cmf_r], W=[C.trib_r])
    C.P = []
    for i in range(7):
        C.P.append(k.ps(f"P{i}", [128, 512], F32))
    C.PT = k.ps("PTb", [128, 1024], BF16)
    return C


def col(C, name, i=0):
    c = CST[name] + i
    return C.cst[:, c:c + 1]


def cmb(C, name, rows=128):
    o = CM[name]
    return C.cmb[0:rows, o:o + 128]


def load_w_bf16(k, dst, dst_r, src, nk, ncols, q="pool"):
    for c in range(nk):
        for c0 in range(0, ncols, 1024):
            c1 = min(ncols, c0 + 1024)
            k.dma(q, dst[:, c, c0:c1], src[c * 128:(c + 1) * 128, c0:c1], W=[dst_r])


def load_w_staged(k, dst, dst_r, src, nk, ncols, stg, cnt=[0]):
    for c0 in range(0, ncols, 512):
        c1 = min(ncols, c0 + 512)
        for c in range(nk):
            st, st_r = stg[cnt[0] % len(stg)]
            k.dma("sp", st[:, 0:c1 - c0], src[c * 128:(c + 1) * 128, c0:c1], W=[st_r])
            if cnt[0] % 2 == 0:
                k.I("dve", lambda e: e.tensor_copy(out=dst[:, c, c0:c1], in_=st[:, 0:c1 - c0]), R=[st_r], W=[dst_r])
            else:
                k.I("act", lambda e: e.copy(out=dst[:, c, c0:c1], in_=st[:, 0:c1 - c0]), R=[st_r], W=[dst_r])
            cnt[0] += 1


def cast_w(k, nc, src, rows, cols, name):
    dst = nc.dram_tensor(name, [rows, cols], BF16, kind="Internal").ap()
    r = Res(name)
    for r0 in range(0, rows, 128):
        for c0 in range(0, cols, 1024):
            c1 = min(cols, c0 + 1024)
            k.dma("pool", dst[r0:r0 + 128, c0:c1], src[r0:r0 + 128, c0:c1], W=[r])
    return dst, r


def load_bf(k, dst, dst_r, bf, c0, c1, nk=None):
    src, src_r = bf
    v = src.rearrange("(c p) f -> p c f", p=128)
    if nk is not None:
        v = v[:, 0:nk, :]
    k.dma("sp", dst[:, :, 0:c1 - c0], v[:, :, c0:c1], R=[src_r], W=[dst_r])


def rmsnorm_T(k, C, h, h_r, gname, hn, hn_r, sq, sq_r, rstd, rstd_r, PM, n=T):
    pm, pm_r = PM
    ns = len(sq_r)
    for c in range(8):
        k.I("act", lambda e: e.activation(out=sq[:, c % ns, 0:n], in_=h[:, c, 0:n], func=AF.Square),
            R=[h_r], W=[sq_r[c % ns]])
        k.I("pe", lambda e: e.matmul(pm[:, 0:n], cmb(C, "ones"), sq[:, c % ns, 0:n], start=(c == 0), stop=(c == 7)),
            R=[sq_r[c % ns], C.cmb_r], W=[pm_r])
    k.I("act", lambda e: e.activation(out=rstd[:, 0:n], in_=pm[:, 0:n], func=AF.Sqrt, bias=EPS, scale=1.0 / D),
        R=[pm_r], W=[rstd_r])
    k.I("dve", lambda e: e.reciprocal(out=rstd[:, 0:n], in_=rstd[:, 0:n]), R=[rstd_r], W=[rstd_r])
    for c in range(8):
        k.I("dve", lambda e: e.scalar_tensor_tensor(out=hn[:, c, 0:n], in0=h[:, c, 0:n], scalar=col(C, gname, c),
                                                    in1=rstd[:, 0:n], op0=ALU.mult, op1=ALU.mult),
            R=[h_r, rstd_r, C.cst_r], W=[hn_r])


def g1_moba(k, nc, C, es, xT, h1, positions, w_in_d, w_out_d, pool_w_d, after_loads=None):
    (PA, PB, PS0, PS1, PO0, PO1, PM) = C.P
    PTt, PT_r = C.PT
    win, win_r = k.sb("win", [128, 8, 2048], BF16)
    wout, wout_r = k.sb("wout", [128, 8, 1024], BF16)
    poolw, poolw_r = k.sb("poolw", [128, 4, 128], BF16)
    KT, KT_r = k.sb("KT", [128, 4, S], BF16)
    V, V_r = k.sb("V", [128, 32, 512], BF16)
    kmT, kmT_r = k.sb("kmT", [128, 4, 32], BF16)
    h, h_r = k.sb("h", [128, 8, T], F32)
    hn, hn_r = k.sb("hn", [128, 8, T], BF16)
    sq, _ = k.sb("sq", [128, 2, T], BF16)
    sq_r = [Res() for _ in range(2)]
    rstd, rstd_r = k.sb("rstd", [128, T], F32)
    qT, qT_r = k.sb("qT", [128, 4, T], BF16)
    catT, catT_r = k.sb("catT", [128, 8, T], BF16)
    qTz, qTz_r = k.sb("qTz", [128, 8, T], BF16)
    cos, cos_r = k.sb("cos", [128, T], F32)
    sin, sin_r = k.sb("sin", [128, T], F32)
    posi, posi_r = k.sb("posi", [128, T], I32)
    wk = [k.sb(f"wk{i}", [128, 528], F32) for i in range(6)]
    qnb, qnb_r = k.sb("qnb", [128, T], BF16)
    sqb, sqb_r = k.sb("sqb", [128, T], BF16)
    uext = [k.sb(f"uext{g}", [128, 528], F32) for g in range(4)]
    pooled, pooled_r = k.sb("pooled", [128, T], BF16)
    gsb, gsb_r = k.sb("gsb", [128, 8, 16], F32)
    biasq4, _ = k.sb("biasq4", [128, 4, 8, 16], F32)
    biasq4_r = [Res() for _ in range(4)]
    PMq_r = [Res() for _ in range(4)]
    PTv = [(PTt, PT_r), (PA[0][:, :].bitcast(BF16), PA[1])]
    m8, m8_r = k.sb("m8", [128, 8, 8], F32)
    Pb = [k.sb(f"Pb{i}", [128, T], BF16) for i in range(2)]
    PTs = [k.sb(f"PTs{i}", [128, T], BF16) for i in range(2)]
    rs = [k.sb(f"rs{i}", [128, 32], F32) for i in range(4)]
    rinv, rinv_r = k.sb("rinv", [128, 8], F32)
    otok, otok_r = k.sb("otok", [128, 512], BF16)
    dsm, dsm_r = k.sb("dsm", [128, 128], F32)

    for g in range(4):
        k.I("dve", lambda e: e.memset(uext[g][0][:, 0:16], 0.0), W=[uext[g][1]])
    k.I("dve", lambda e: e.memset(kmT[:], 0.0), W=[kmT_r])
    k.I("dve", lambda e: e.memset(qTz[:], 0.0), W=[qTz_r])
    stg = wk[0:4]
    load_w_staged(k, win, win_r, w_in_d, 8, 2048, stg)
    load_w_staged(k, wout, wout_r, w_out_d, 8, 1024, stg)
    for g in range(4):
        k.dma("pool", poolw[:, g, :], pool_w_d[g, :, :], W=[poolw_r])
    if after_loads is not None:
        after_loads()

    twopi = float(2 * np.pi)
    pcount = 0
    for t in range(NT):
        t0 = t * T
        for c in range(8):
            k.dma("sp", h[:, c, :], xT[c, :, t0:t0 + T], W=[h_r])
        k.dma("sp", posi[:], positions[t0:t0 + T].partition_broadcast(128), W=[posi_r])
        (a0, a0r), (a1, a1r), (a2, a2r) = wk[0], wk[1], wk[2]
        k.I("dve", lambda e: e.tensor_copy(out=a0[:, 0:T], in_=posi[:]), R=[posi_r], W=[a0r])
        k.I("dve", lambda e: e.tensor_scalar(out=a0[:, 0:T], in0=a0[:, 0:T], scalar1=col(C, "invf"), scalar2=1.0 / twopi,
                                              op0=ALU.mult, op1=ALU.mult), R=[a0r, C.cst_r], W=[a0r])
        for (dst, dst_r, off) in ((sin, sin_r, 0.5), (cos, cos_r, 0.75)):
            k.I("dve", lambda e: e.tensor_scalar(out=a1[:, 0:T], in0=a0[:, 0:T], scalar1=off, scalar2=None, op0=ALU.add),
                R=[a0r], W=[a1r])
            k.I("dve", lambda e: e.tensor_copy(out=posi[:], in_=a1[:, 0:T]), R=[a1r], W=[posi_r])
            k.I("dve", lambda e: e.tensor_copy(out=a2[:, 0:T], in_=posi[:]), R=[posi_r], W=[a2r])
            k.I("dve", lambda e: e.tensor_tensor(out=a1[:, 0:T], in0=a1[:, 0:T], in1=a2[:, 0:T], op=ALU.subtract),
                R=[a1r, a2r], W=[a1r])
            k.I("dve", lambda e: e.tensor_scalar(out=a2[:, 0:T], in0=a1[:, 0:T], scalar1=0.0, scalar2=None, op0=ALU.is_lt),
                R=[a1r], W=[a2r])
            k.I("dve", lambda e: e.tensor_tensor(out=a1[:, 0:T], in0=a1[:, 0:T], in1=a2[:, 0:T], op=ALU.add),
                R=[a1r, a2r], W=[a1r])
            k.I("act", lambda e: e.activation(out=dst[:], in_=a1[:, 0:T], func=AF.Sin, bias=-float(np.pi), scale=twopi),
                R=[a1r], W=[dst_r])
        rmsnorm_T(k, C, h, h_r, "mix0", hn, hn_r, sq, sq_r, rstd, rstd_r, PM)
        for c in range(8):
            pp, pp_r = (PA, PB)[c % 2]
            for kc in range(8):
                k.I("pe", lambda e: e.matmul(pp[:], win[:, kc, c * 128:(c + 1) * 128], hn[:, kc, :],
                                             start=(kc == 0), stop=(kc == 7)),
                    R=[win_r, hn_r], W=[pp_r], inc=(kc == 7))
            isq = c < 4
            gname = "hyq" if isq else "hyk"
            (qn, qn_r), (t1, t1_r), (t2, t2_r), (r2, r2_r) = wk[0], wk[1], wk[2], wk[3]
            k.I("act", lambda e: e.activation(out=sqb[:], in_=pp[:], func=AF.Square), R=[pp_r], W=[sqb_r])
            k.I("act", lambda e: e.activation(out=qn[:, 0:T], in_=pp[:], func=AF.Copy, scale=col(C, gname)),
                R=[pp_r, C.cst_r], W=[qn_r])
            k.I("pe", lambda e: e.matmul(PM[0][:], cmb(C, "bones"), sqb[:], start=True, stop=True),
                R=[sqb_r, C.cmb_r], W=[PM[1]])
            k.I("act", lambda e: e.activation(out=r2[:, 0:T], in_=PM[0][:], func=AF.Sqrt, bias=EPS, scale=1.0 / 64),
                R=[PM[1]], W=[r2_r])
            k.I("dve", lambda e: e.reciprocal(out=r2[:, 0:T], in_=r2[:, 0:T]), R=[r2_r], W=[r2_r])
            k.I("dve", lambda e: e.tensor_copy(out=qnb[:], in_=qn[:, 0:T]), R=[qn_r], W=[qnb_r])
            k.I("pe", lambda e: e.matmul(PM[0][:], cmb(C, "rot"), qnb[:], start=True, stop=True),
                R=[qnb_r, C.cmb_r], W=[PM[1]])
            k.I("dve", lambda e: e.tensor_tensor(out=t2[:, 0:T], in0=PM[0][:], in1=sin[:], op=ALU.mult),
                R=[PM[1], sin_r], W=[t2_r])
            k.I("dve", lambda e: e.tensor_tensor(out=t1[:, 0:T], in0=qn[:, 0:T], in1=cos[:], op=ALU.mult),
                R=[qn_r, cos_r], W=[t1_r])
            k.I("dve", lambda e: e.tensor_tensor(out=t1[:, 0:T], in0=t1[:, 0:T], in1=t2[:, 0:T], op=ALU.add),
                R=[t1_r, t2_r], W=[t1_r])
            if isq:
                k.I("dve", lambda e: e.tensor_tensor(out=qT[:, c, :], in0=t1[:, 0:T], in1=r2[:, 0:T], op=ALU.mult),
                    R=[t1_r, r2_r], W=[qT_r])
                k.I("act", lambda e: e.copy(out=qTz[0:64, 2 * c, :], in_=qT[0:64, c, :]), R=[qT_r], W=[qTz_r])
                k.I("act", lambda e: e.copy(out=qTz[64:128, 2 * c + 1, :], in_=qT[64:128, c, :]), R=[qT_r], W=[qTz_r])
            else:
                kc4 = c - 4
                k.I("dve", lambda e: e.tensor_tensor(out=t2[:, 0:T], in0=t1[:, 0:T], in1=r2[:, 0:T], op=ALU.mult),
                    R=[t1_r, r2_r], W=[t2_r])
                k.I("act", lambda e: e.copy(out=KT[:, kc4, t0:t0 + T], in_=t2[:, 0:T]), R=[t2_r], W=[KT_r])
                k.I("dve", lambda e: e.tensor_reduce(out=r2[:, 0:2], in_=t2[:, 0:T].rearrange("p (b x) -> p b x", b=2),
                                                      axis=AX.X, op=ALU.add), R=[t2_r], W=[r2_r])
                k.I("act", lambda e: e.mul(out=kmT[0:64, kc4, 2 * t:2 * t + 2], in_=r2[0:64, 0:2], mul=1.0 / 256),
                    R=[r2_r], W=[kmT_r])
                k.I("act", lambda e: e.mul(out=kmT[64:128, kc4, 16 + 2 * t:16 + 2 * t + 2], in_=r2[64:128, 0:2], mul=1.0 / 256),
                    R=[r2_r], W=[kmT_r])
        for qs in range(4):
            pp, pp_r = (PA, PB)[qs % 2]
            for kc in range(8):
                k.I("pe", lambda e: e.matmul(pp[:], hn[:, kc, qs * 128:(qs + 1) * 128], win[:, kc, 1024:1536],
                                             start=(kc == 0), stop=(kc == 7)),
                    R=[win_r, hn_r], W=[pp_r], inc=(kc == 7))
            k.I("act", lambda e: e.copy(out=V[:, 4 * t + qs, :], in_=pp[:]), R=[pp_r], W=[V_r])
        for g in range(4):
            w = 2 << g
            pp, pp_r = (PA, PB)[g % 2]
            ue, ue_r = uext[g]
            for kc in range(8):
                k.I("pe", lambda e: e.matmul(pp[:], win[:, kc, (12 + g) * 128:(13 + g) * 128], hn[:, kc, :],
                                             start=(kc == 0), stop=(kc == 7)),
                    R=[win_r, hn_r], W=[pp_r], inc=(kc == 7))
            k.I("act", lambda e: e.copy(out=ue[:, 16:528], in_=pp[:]), R=[pp_r], W=[ue_r])
            cur, cur_r = ue, ue_r
            step = 1
            bufs = [wk[4], wk[5]]
            bi = 0
            lo = 0
            while step < w:
                lo += step
                nxt, nxt_r = bufs[bi]
                bi ^= 1
                k.I("dve", lambda e: e.tensor_tensor(out=nxt[:, lo:528], in0=cur[:, lo:528], in1=cur[:, lo - step:528 - step],
                                                       op=ALU.add), R=[cur_r], W=[nxt_r])
                cur, cur_r = nxt, nxt_r
                step *= 2
            (pf, pf_r) = wk[3]
            k.I("dve", lambda e: e.scalar_tensor_tensor(out=pf[:, 0:T], in0=cur[:, 16:528], scalar=1.0 / w, in1=ue[:, 16:528],
                                                        op0=ALU.mult, op1=ALU.subtract), R=[cur_r, ue_r], W=[pf_r])
            if t == 0:
                rc = C.cst[:, CST["rcnt"] + 16 * g: CST["rcnt"] + 16 * g + 16]
                k.I("dve", lambda e: e.tensor_tensor(out=pf[:, 0:16], in0=cur[:, 16:32], in1=rc, op=ALU.mult),
                    R=[cur_r, C.cst_r], W=[pf_r])
                k.I("dve", lambda e: e.tensor_tensor(out=pf[:, 0:16], in0=pf[:, 0:16], in1=ue[:, 16:32], op=ALU.subtract),
                    R=[ue_r], W=[pf_r])
            k.I("act", lambda e: e.copy(out=pooled[:], in_=pf[:, 0:T]), R=[pf_r], W=[pooled_r])
            k.I("act", lambda e: e.copy(out=ue[:, 0:16], in_=ue[:, 512:528]), R=[cur_r], W=[ue_r])
            k.I("pe", lambda e: e.matmul(PM[0][:], poolw[:, g, :], pooled[:], start=True, stop=True),
                R=[poolw_r, pooled_r], W=[PM[1]])
            k.I("act", lambda e: e.activation(out=catT[:, 4 + g, :], in_=PM[0][:], func=AF.Copy, scale=col(C, "pscale", g)),
                R=[PM[1], C.cst_r], W=[catT_r])
        for q_ in range(4):
            PMq_r[q_].w = PM[1].w
            PMq_r[q_].r = dict(PM[1].r)
        for qs in range(4):
            gq = 4 * t + qs
            own = gq // 2
            q0 = qs * 128
            bq = biasq4[:, qs]
            if own > 0:
                for c4 in range(4):
                    k.I("pe", lambda e: e.matmul(PM[0][:, qs * 128 + c4 * 32:qs * 128 + (c4 + 1) * 32], qT[:, c4, q0:q0 + 128],
                                                 kmT[:, c4, :], start=True, stop=True),
                        R=[qT_r, kmT_r], W=[PM[1]], inc=(c4 == 3))
                k.I("dve", lambda e: e.memset(gsb[:], -1e30), W=[gsb_r])
                k.I("dve", lambda e: e.tensor_copy(out=gsb[:, :, 0:own],
                                                    in_=PM[0][:, qs * 128:(qs + 1) * 128].rearrange("p (h j) -> p h j", h=8)[:, :, 0:own]),
                    R=[PM[1]], W=[gsb_r])
                for hh in range(8):
                    k.I("dve", lambda e: e.max(out=m8[:, hh, :], in_=gsb[:, hh, :]), R=[gsb_r], W=[m8_r])
                k.I("dve", lambda e: e.tensor_tensor(out=bq[:, :, 0:own], in0=gsb[:, :, 0:own],
                                                      in1=m8[:, :, 2:3].to_broadcast([128, 8, own]), op=ALU.is_lt),
                    R=[gsb_r, m8_r], W=[biasq4_r[qs]])
                k.I("dve", lambda e: e.tensor_scalar(out=bq[:, :, 0:own], in0=bq[:, :, 0:own], scalar1=NEG, scalar2=None,
                                                      op0=ALU.mult), R=[biasq4_r[qs]], W=[biasq4_r[qs]])
            k.I("dve", lambda e: e.memset(bq[:, :, own:own + 1], 0.0), W=[biasq4_r[qs]])
        items = []
        for qs in range(4):
            gq = 4 * t + qs
            ngrp = gq // 4 + 1
            for hh in range(8):
                for kg in range(ngrp):
                    items.append((qs, hh, kg, kg == 0, kg == ngrp - 1, hh == 7 and kg == ngrp - 1))
        nrs_of = {}

        def stA(i):
            qs, hh, kg, fh, lh, lq = items[i]
            gq = 4 * t + qs
            kt_lo = kg * 4
            kt_hi = min(kt_lo + 4, gq + 1)
            nk = kt_hi - kt_lo
            PSx, PSx_r = (PS0, PS1)[i % 2]
            k.I("pe", lambda e: e.matmul(PSx[:, 0:nk * 128], qTz[:, hh, qs * 128:(qs + 1) * 128],
                                         KT[:, hh // 2, kt_lo * 128:kt_hi * 128], start=True, stop=True),
                R=[qTz_r, KT_r], W=[PSx_r])

        def stB(i):
            qs, hh, kg, fh, lh, lq = items[i]
            gq = 4 * t + qs
            kt_lo = kg * 4
            kt_hi = min(kt_lo + 4, gq + 1)
            PSx, PSx_r = (PS0, PS1)[i % 2]
            Pbx, Pbx_r = Pb[i % 2]
            rsx, rsx_r = rs[hh % 4]
            nrs = 0 if fh else nrs_of[(qs, hh)]
            kt = kt_lo
            while kt < kt_hi:
                j = kt // 2
                c0 = (kt - kt_lo) * 128
                if kt == gq:
                    k.I("act", lambda e: e.activation(out=Pbx[:, c0:c0 + 128], in_=PSx[:, c0:c0 + 128], func=AF.Exp, scale=0.125),
                        R=[PSx_r], W=[Pbx_r])
                    k.I("dve", lambda e: e.scalar_tensor_tensor(out=Pbx[:, c0:c0 + 128], in0=Pbx[:, c0:c0 + 128], scalar=1.0,
                                                                in1=cmb(C, "tri01"), op0=ALU.mult, op1=ALU.mult,
                                                                accum_out=rsx[:, nrs:nrs + 1]),
                        R=[C.cmb_r], W=[Pbx_r, rsx_r])
                    nrs += 1
                    kt += 1
                else:
                    n = 2 if (kt % 2 == 0 and kt + 1 < kt_hi and kt + 1 != gq) else 1
                    k.I("act", lambda e: e.activation(out=Pbx[:, c0:c0 + n * 128], in_=PSx[:, c0:c0 + n * 128],
                                                      func=AF.Exp, scale=0.125, bias=biasq4[:, qs, hh, j:j + 1],
                                                      accum_out=rsx[:, nrs:nrs + 1]),
                        R=[PSx_r, biasq4_r[qs]], W=[Pbx_r, rsx_r])
                    nrs += 1
                    kt += n
            nrs_of[(qs, hh)] = nrs

        def stC(i):
            qs, hh, kg, fh, lh, lq = items[i]
            gq = 4 * t + qs
            kt_lo = kg * 4
            nk = min(kt_lo + 4, gq + 1) - kt_lo
            Pbx, Pbx_r = Pb[i % 2]
            ptv, ptv_r = PTv[i % 2]
            for ii in range(nk):
                k.I("pe", lambda e: e.transpose(ptv[:, ii * 128:(ii + 1) * 128],
                                                Pbx[:, ii * 128:(ii + 1) * 128], cmb(C, "ident")),
                    R=[Pbx_r, C.cmb_r], W=[ptv_r], inc=(ii == nk - 1))
            PTx, PTx_r = PTs[i % 2]
            k.I("dve", lambda e: e.tensor_copy(out=PTx[:, 0:nk * 128], in_=ptv[:, 0:nk * 128]),
                R=[ptv_r], W=[PTx_r])

        def stE(i):
            qs, hh, kg, fh, lh, lq = items[i]
            gq = 4 * t + qs
            kt_lo = kg * 4
            nk = min(kt_lo + 4, gq + 1) - kt_lo
            PTx, PTx_r = PTs[i % 2]
            PO, PO_r = (PO0, PO1)[qs % 2]
            for ii in range(nk):
                ktt = kt_lo + ii
                k.I("pe", lambda e: e.matmul(PO[:, hh * 64:(hh + 1) * 64], PTx[:, ii * 128:(ii + 1) * 128],
                                             V[:, ktt, hh * 64:(hh + 1) * 64], start=(ktt == 0), stop=(ktt == gq)),
                    R=[PTx_r, V_r], W=[PO_r], inc=(ii == nk - 1))
            if lh:
                rsx, rsx_r = rs[hh % 4]
                nrs = nrs_of[(qs, hh)]
                k.I("dve", lambda e: e.tensor_reduce(out=rinv[:, hh:hh + 1], in_=rsx[:, 0:nrs], axis=AX.X, op=ALU.add),
                    R=[rsx_r], W=[rinv_r])
            if lq:
                q0 = qs * 128
                k.I("dve", lambda e: e.reciprocal(out=rinv[:], in_=rinv[:]), R=[rinv_r], W=[rinv_r])
                k.I("dve", lambda e: e.tensor_tensor(out=otok[:].rearrange("p (h d) -> p h d", h=8),
                                                      in0=PO[:].rearrange("p (h d) -> p h d", h=8),
                                                      in1=rinv[:].rearrange("p (h o) -> p h o", o=1).to_broadcast([128, 8, 64]),
                                                      op=ALU.mult), R=[PO_r, rinv_r], W=[otok_r])
                for c in range(4):
                    k.I("pe", lambda e: e.transpose(PTt[:, c * 128:(c + 1) * 128], otok[:, c * 128:(c + 1) * 128], cmb(C, "ident")),
                        R=[otok_r, C.cmb_r], W=[PT_r], inc=(c == 3))
                k.I("act", lambda e: e.copy(out=catT[:, 0:4, q0:q0 + 128], in_=PTt[:, 0:512].rearrange("p (c q) -> p c q", c=4)),
                    R=[PT_r], W=[catT_r])

        nit = len(items)
        for i in range(nit + 2):
            if i < nit:
                stA(i)
                stB(i)
            if 0 <= i - 1 < nit:
                stC(i - 1)
            if 0 <= i - 2 < nit:
                stE(i - 2)
        for dc in range(8):
            pp, pp_r = (PA, PB)[dc % 2]
            for kc in range(8):
                k.I("pe", lambda e: e.matmul(pp[:], wout[:, kc, dc * 128:(dc + 1) * 128], catT[:, kc, :],
                                             start=(kc == 0), stop=(kc == 7)),
                    R=[wout_r, catT_r], W=[pp_r], inc=(kc == 7))
            k.I("dve", lambda e: e.tensor_tensor(out=h[:, dc, :], in0=pp[:], in1=h[:, dc, :], op=ALU.add),
                R=[pp_r, h_r], W=[h_r])
        for c in range(8):
            k.dma("sp", h1[c, :, t0:t0 + T], h[:, c, :], R=[h_r])


def g_xattn(k, nc, C, h_in, h_out, memT_d, wq_d, wkv_d, wo_d, layer, bf=None):
    (PA, PB, PS0, PS1, PO0, PO1, PM) = C.P
    L = str(layer)
    wq, wq_r = k.sb("xwq", [128, 8, 1024], BF16)
    wo, wo_r = k.sb("xwo", [128, 8, 1024], BF16)
    wkv, wkv_r = k.sb("xwkv", [128, 8, 2048], BF16)
    memf, memf_r = k.sb("memf", [128, 8, 256], F32)
    memn, memn_r = k.sb("memn", [128, 8, 256], BF16)
    KxT, KxT_r = k.sb("KxT", [128, 8, 256], BF16)
    Vx, Vx_r = k.sb("Vx", [128, 2, 1024], BF16)
    h, h_r = k.sb("xh", [128, 8, T], F32)
    hn, hn_r = k.sb("xhn", [128, 8, T], BF16)
    sq, _ = k.sb("xsq", [128, 2, T], BF16)
    sq_r = [Res(), Res()]
    rstd, rstd_r = k.sb("xrstd", [128, T], F32)
    qx, qx_r = k.sb("qx", [128, 8, T], BF16)
    oT, oT_r = k.sb("oT", [128, 8, T], BF16)
    PTb_ = [k.sb(f"xPT{i}", [128, T], BF16) for i in range(2)]
    rden, rden_r = k.sb("rden", [128, T], F32)
    r2, r2_r = k.sb("xr2", [128, T], F32)

    if bf is None:
        load_w_bf16(k, wkv, wkv_r, wkv_d, 8, 2048)
        load_w_bf16(k, wq, wq_r, wq_d, 8, 1024)
        load_w_bf16(k, wo, wo_r, wo_d, 8, 1024)
    else:
        load_bf(k, wkv, wkv_r, bf["wkv"], 0, 2048)
        load_bf(k, wq, wq_r, bf["wq"], 0, 1024)
        load_bf(k, wo, wo_r, bf["wo"], 0, 1024)
    for c in range(8):
        k.dma("sp", memf[:, c, :], memT_d[c, :, :], W=[memf_r])
    rmsnorm_T(k, C, memf, memf_r, "mem" + L, memn, memn_r, sq, sq_r, rstd, rstd_r, PM, n=256)
    for hh in range(4):
        for cc in range(2):
            pp, pp_r = (PA, PB)[cc]
            fc = 2 * hh + cc
            for kc in range(8):
                k.I("pe", lambda e: e.matmul(pp[:, 0:256], wkv[:, kc, fc * 128:(fc + 1) * 128], memn[:, kc, :],
                                             start=(kc == 0), stop=(kc == 7)), R=[wkv_r, memn_r], W=[pp_r], inc=(kc == 7))
            k.I("act", lambda e: e.activation(out=sq[:, cc, 0:256], in_=pp[:, 0:256], func=AF.Square), R=[pp_r], W=[sq_r[cc]])
            k.I("pe", lambda e: e.matmul(PM[0][:, 0:256], cmb(C, "ones"), sq[:, cc, 0:256], start=(cc == 0), stop=(cc == 1)),
                R=[sq_r[cc], C.cmb_r], W=[PM[1]])
        k.I("act", lambda e: e.activation(out=r2[:, 0:256], in_=PM[0][:, 0:256], func=AF.Sqrt, bias=EPS, scale=1.0 / 256),
            R=[PM[1]], W=[r2_r])
        k.I("dve", lambda e: e.reciprocal(out=r2[:, 0:256], in_=r2[:, 0:256]), R=[r2_r], W=[r2_r])
        for cc in range(2):
            pp, pp_r = (PA, PB)[cc]
            k.I("dve", lambda e: e.scalar_tensor_tensor(out=KxT[:, 2 * hh + cc, :], in0=pp[:, 0:256], scalar=col(C, "xak" + L, cc),
                                                        in1=r2[:, 0:256], op0=ALU.mult, op1=ALU.mult),
                R=[pp_r, r2_r, C.cst_r], W=[KxT_r])
    for mt in range(2):
        for half in range(2):
            pp, pp_r = (PA, PB)[half]
            for kc in range(8):
                k.I("pe", lambda e: e.matmul(pp[:], memn[:, kc, mt * 128:(mt + 1) * 128],
                                             wkv[:, kc, 1024 + half * 512:1024 + (half + 1) * 512],
                                             start=(kc == 0), stop=(kc == 7)), R=[wkv_r, memn_r], W=[pp_r], inc=(kc == 7))
            k.I("act", lambda e: e.copy(out=Vx[:, mt, half * 512:(half + 1) * 512], in_=pp[:]), R=[pp_r], W=[Vx_r])

    hB = [(h, h_r), k.sb("xh2", [128, 8, T], F32)]
    hnB = [(hn, hn_r), k.sb("xhn2", [128, 8, T], BF16)]
    oTB = [(oT, oT_r), k.sb("oT2", [128, 8, T], BF16)]
    sqh = [[k.sb(f"xsqh{i}{c}", [128, T], BF16) for c in range(2)] for i in range(2)]
    r2B = [(r2, r2_r), k.sb("xr2b", [128, T], F32)]
    rdB = [(rden, rden_r), k.sb("rden2", [128, T], F32)]
    PTB = [PTb_, [k.sb(f"xPTb{i}", [128, T], BF16) for i in range(2)]]
    PD = (C.PT[0][:, :].bitcast(F32), C.PT[1])
    N = NT * 4

    def S0(t):
        h_, h_r_ = hB[t % 2]
        hn_, hn_r_ = hnB[t % 2]
        for c in range(8):
            k.dma("sp", h_[:, c, :], h_in[c, :, t * T:(t + 1) * T], W=[h_r_])
        rmsnorm_T(k, C, h_, h_r_, "xa" + L, hn_, hn_r_, sq, sq_r, rstd, rstd_r, PM)

    def S1(i):
        t, hh = divmod(i, 4)
        hn_, hn_r_ = hnB[t % 2]
        for cc in range(2):
            pp, pp_r = (PA, PB)[cc]
            fc = 2 * hh + cc
            s_, s_r = sqh[i % 2][cc]
            for kc in range(8):
                k.I("pe", lambda e: e.matmul(pp[:], wq[:, kc, fc * 128:(fc + 1) * 128], hn_[:, kc, :],
                                             start=(kc == 0), stop=(kc == 7)), R=[wq_r, hn_r_], W=[pp_r], inc=(kc == 7))
            k.I("act", lambda e: e.activation(out=s_[:], in_=pp[:], func=AF.Square), R=[pp_r], W=[s_r])
            k.I("pe", lambda e: e.matmul(PM[0][:], cmb(C, "ones"), s_[:], start=(cc == 0), stop=(cc == 1)),
                R=[s_r, C.cmb_r], W=[PM[1]])

    def S2(i):
        t, hh = divmod(i, 4)
        r_, r_r = r2B[i % 2]
        k.I("act", lambda e: e.activation(out=r_[:], in_=PM[0][:], func=AF.Sqrt, bias=EPS, scale=1.0 / 256),
            R=[PM[1]], W=[r_r])
        k.I("dve", lambda e: e.reciprocal(out=r_[:], in_=r_[:]), R=[r_r], W=[r_r])
        for cc in range(2):
            pp, pp_r = (PA, PB)[cc]
            k.I("dve", lambda e: e.scalar_tensor_tensor(out=qx[:, 2 * hh + cc, :], in0=pp[:], scalar=col(C, "xaq" + L, cc),
                                                        in1=r_[:], op0=ALU.mult, op1=ALU.mult),
                R=[pp_r, r_r, C.cst_r], W=[qx_r])

    def S3(i):
        t, hh = divmod(i, 4)
        for mt in range(2):
            ps_, ps_r = (PS0, PS1)[mt]
            pb, pb_r = PTB[i % 2][mt]
            for cc in range(2):
                k.I("pe", lambda e: e.matmul(ps_[:], KxT[:, 2 * hh + cc, mt * 128:(mt + 1) * 128], qx[:, 2 * hh + cc, :],
                                             start=(cc == 0), stop=(cc == 1)), R=[KxT_r, qx_r], W=[ps_r], inc=(cc == 1))
            k.I("act", lambda e: e.activation(out=pb[:], in_=ps_[:], func=AF.Exp, scale=1.0 / 16), R=[ps_r], W=[pb_r])

    def S4(i):
        t, hh = divmod(i, 4)
        oT_, oT_r_ = oTB[t % 2]
        rd, rd_r = rdB[i % 2]
        for mt in range(2):
            pb, pb_r = PTB[i % 2][mt]
            k.I("pe", lambda e: e.matmul(PD[0][:, 0:512], cmb(C, "ones"), pb[:], start=(mt == 0), stop=(mt == 1)),
                R=[pb_r, C.cmb_r], W=[PD[1]], inc=(mt == 1))
        k.I("dve", lambda e: e.reciprocal(out=rd[:], in_=PD[0][:, 0:512]), R=[PD[1]], W=[rd_r])
        for cc in range(2):
            po, po_r = (PO0, PO1)[cc]
            for mt in range(2):
                pb, pb_r = PTB[i % 2][mt]
                k.I("pe", lambda e: e.matmul(po[:], Vx[:, mt, (2 * hh + cc) * 128:(2 * hh + cc + 1) * 128], pb[:],
                                             start=(mt == 0), stop=(mt == 1)), R=[Vx_r, pb_r], W=[po_r], inc=(mt == 1))
            k.I("dve", lambda e: e.tensor_tensor(out=oT_[:, 2 * hh + cc, :], in0=po[:], in1=rd[:], op=ALU.mult),
                R=[po_r, rd_r], W=[oT_r_])
        if hh == 3:
            h_, h_r_ = hB[t % 2]
            for dc in range(8):
                pp, pp_r = (PO0, PO1)[dc % 2]
                for kc in range(8):
                    k.I("pe", lambda e: e.matmul(pp[:], wo[:, kc, dc * 128:(dc + 1) * 128], oT_[:, kc, :],
                                                 start=(kc == 0), stop=(kc == 7)), R=[wo_r, oT_r_], W=[pp_r], inc=(kc == 7))
                k.I("dve", lambda e: e.tensor_tensor(out=h_[:, dc, :], in0=pp[:], in1=h_[:, dc, :], op=ALU.add),
                    R=[pp_r, h_r_], W=[h_r_])
            for c in range(8):
                k.dma("sp", h_out[c, :, t * T:(t + 1) * T], h_[:, c, :], R=[h_r_])

    for i in range(N + 2):
        if i < N:
            if i % 4 == 0:
                S0(i // 4)
            S1(i)
            S2(i)
        if 0 <= i - 1 < N:
            S3(i - 1)
        if 0 <= i - 2 < N:
            S4(i - 2)


def g_ffn(k, nc, C, h_in, h_out, wg_d, wu_d, wd_d, bf=None):
    (PA, PB, PS0, PS1, PO0, PO1, PM) = C.P
    NF = 22
    wg, wg_r = k.sb("fwg", [128, 8, 2816], BF16)
    wu, wu_r = k.sb("fwu", [128, 8, 2816], BF16)
    wd, wd_r = k.sb("fwd", [128, NF, 1024], BF16)
    h, h_r = k.sb("fh", [128, 8, T], F32)
    hn, hn_r = k.sb("fhn", [128, 8, T], BF16)
    sq, _ = k.sb("fsq", [128, 2, T], BF16)
    sq_r = [Res(), Res()]
    rstd, rstd_r = k.sb("frstd", [128, T], F32)
    act, act_r = k.sb("fact", [128, NF, T], BF16)
    sg = [k.sb(f"fsg{i}", [128, T], BF16) for i in range(2)]
    if bf is None:
        load_w_bf16(k, wg, wg_r, wg_d, 8, 2816)
        load_w_bf16(k, wu, wu_r, wu_d, 8, 2816)
        load_w_bf16(k, wd, wd_r, wd_d, NF, 1024)
    else:
        for c0 in range(0, 2816, 704):
            k.dma("sp", wg[:, :, c0:c0 + 704], bf["wg"][0].rearrange("(c p) f -> p c f", p=128)[:, :, c0:c0 + 704],
                  R=[bf["wg"][1]], W=[wg_r])
            k.dma("sp", wu[:, :, c0:c0 + 704], bf["wu"][0].rearrange("(c p) f -> p c f", p=128)[:, :, c0:c0 + 704],
                  R=[bf["wu"][1]], W=[wu_r])
        for f0 in range(0, NF, 11):
            k.dma("sp", wd[:, f0:f0 + 11, :], bf["wd"][0].rearrange("(c p) f -> p c f", p=128)[:, f0:f0 + 11, :],
                  R=[bf["wd"][1]], W=[wd_r])
    hB = [(h, h_r), (h, h_r)]
    hnB = [(hn, hn_r), k.sb("fhn2", [128, 8, T], BF16)]
    rst = [k.sb(f"frst{i}", [128, T], F32) for i in range(2)]

    def prologue(t):
        h_, h_r_ = hB[t % 2]
        hn_, hn_r_ = hnB[t % 2]
        for c in range(8):
            k.dma("sp", h_[:, c, :], h_in[c, :, t * T:(t + 1) * T], W=[h_r_])
        rmsnorm_T(k, C, h_, h_r_, "ffn0", hn_, hn_r_, sq, sq_r, rstd, rstd_r, PM)

    prologue(0)
    for t in range(NT):
        t0 = t * T
        h_, h_r_ = hB[t % 2]
        hn_, hn_r_ = hnB[t % 2]
        for fc in range(NF):
            (pg, pg_r), (pu, pu_r) = ((PA, PB), (PS0, PS1))[fc % 2]
            for kc in range(8):
                k.I("pe", lambda e: e.matmul(pg[:], wg[:, kc, fc * 128:(fc + 1) * 128], hn_[:, kc, :],
                                             start=(kc == 0), stop=(kc == 7)), R=[wg_r, hn_r_], W=[pg_r], inc=(kc == 7))
            for kc in range(8):
                k.I("pe", lambda e: e.matmul(pu[:], wu[:, kc, fc * 128:(fc + 1) * 128], hn_[:, kc, :],
                                             start=(kc == 0), stop=(kc == 7)), R=[wu_r, hn_r_], W=[pu_r], inc=(kc == 7))
            s_, s_r = sg[fc % 2]
            k.I("act", lambda e: e.activation(out=s_[:], in_=pg[:], func=AF.Silu), R=[pg_r], W=[s_r])
            k.I("dve", lambda e: e.tensor_tensor(out=act[:, fc, :], in0=pu[:], in1=s_[:], op=ALU.mult),
                R=[pu_r, s_r], W=[act_r])
            if fc == NF // 2 and t + 1 < NT:
                prologue(t + 1)
        for dc in range(8):
            pp, pp_r = (PO0, PO1)[dc % 2]
            for fc in range(NF):
                k.I("pe", lambda e: e.matmul(pp[:], wd[:, fc, dc * 128:(dc + 1) * 128], act[:, fc, :],
                                             start=(fc == 0), stop=(fc == NF - 1)), R=[wd_r, act_r], W=[pp_r], inc=(fc == NF - 1))
            r_, r_r = rst[dc % 2]
            k.dma("sp", r_[:], h_in[dc, :, t0:t0 + T], W=[r_r])
            k.I("dve", lambda e: e.tensor_tensor(out=r_[:], in0=pp[:], in1=r_[:], op=ALU.add),
                R=[pp_r, r_r], W=[r_r])
            k.dma("sp", h_out[dc, :, t0:t0 + T], r_[:], R=[r_r])


def g_moe(k, nc, C, h_in, h_out, wr_d, wg_d, wu_d, wd_d, n_exp=8):
    (PA, PB, PS0, PS1, PO0, PO1, PM) = C.P
    SUP = min(2048, NT * T)
    NSUP = (NT * T) // SUP
    NSB = SUP // 128
    NTT = SUP // T
    hs, hs_r = k.sb("mhs", [128, 8, SUP], F32)
    hn, hn_r = k.sb("mhn", [128, 8, SUP], BF16)
    gbc, gbc_r = k.sb("mgbc", [128, 8, SUP], BF16)
    wgs = [k.sb(f"mwg{i}", [128, 8, 512], BF16) for i in range(2)]
    wus = [k.sb(f"mwu{i}", [128, 8, 512], BF16) for i in range(2)]
    wds = [k.sb(f"mwd{i}", [128, 4, 1024], BF16) for i in range(2)]
    act = [k.sb(f"mact{i}", [128, 4, T], BF16) for i in range(2)]
    sg = [k.sb(f"msg{i}", [128, T], BF16) for i in range(2)]
    sg2 = [k.sb(f"msg2{i}", [128, T], BF16) for i in range(2)]
    sq, _ = k.sb("msq", [128, 2, T], BF16)
    sq_r = [Res(), Res()]
    rstd, rstd_r = k.sb("mrstd", [128, T], F32)
    wr, wr_r = k.sb("mwr", [128, 8, 8], BF16)
    Lg, Lg_r = k.sb("mL", [128, NSB, 8], F32)
    Eg, Eg_r = k.sb("mE", [128, NSB, 8], F32)
    Sg, Sg_r = k.sb("mS", [128, NSB, 8], F32)
    m8, m8_r = k.sb("mm8", [128, NSB, 8], F32)
    rd, rd_r = k.sb("mrd", [128, NSB], F32)
    Gb, Gb_r = k.sb("mGb", [128, NSB, 8], BF16)
    Gbig, Gbig_r = k.sb("mGbig", [128, 128], BF16)
    for kc in range(8):
        k.dma("pool", wr[:, kc, :], wr_d[kc * 128:(kc + 1) * 128, :], W=[wr_r])
    wcount = 0
    for sp_ in range(NSUP):
        s0 = sp_ * SUP
        for c in range(8):
            k.dma("sp", hs[:, c, :], h_in[c, :, s0:s0 + SUP], W=[hs_r])
        for tt in range(NTT):
            hv = hs[:, :, tt * T:(tt + 1) * T]
            hnv = hn[:, :, tt * T:(tt + 1) * T]
            rmsnorm_T(k, C, hv, hs_r, "ffn1", hnv, hn_r, sq, sq_r, rstd, rstd_r, PM)
        for sb_ in range(NSB):
            for kc in range(8):
                k.I("pe", lambda e: e.matmul(PM[0][:, sb_ * 8:(sb_ + 1) * 8], hn[:, kc, sb_ * 128:(sb_ + 1) * 128], wr[:, kc, :],
                                             start=(kc == 0), stop=(kc == 7)), R=[hn_r, wr_r], W=[PM[1]],
                    inc=(kc == 7 and sb_ == NSB - 1))
        k.I("dve", lambda e: e.tensor_copy(out=Lg[:], in_=PM[0][:, 0:NSB * 8].rearrange("p (s e) -> p s e", e=8)),
            R=[PM[1]], W=[Lg_r])
        for sb_ in range(NSB):
            k.I("dve", lambda e: e.max(out=m8[:, sb_, :], in_=Lg[:, sb_, :]), R=[Lg_r], W=[m8_r])
        k.I("dve", lambda e: e.tensor_tensor(out=Eg[:], in0=Lg[:], in1=m8[:, :, 0:1].to_broadcast([128, NSB, 8]), op=ALU.subtract),
            R=[Lg_r, m8_r], W=[Eg_r])
        k.I("act", lambda e: e.activation(out=Eg[:], in_=Eg[:], func=AF.Exp), R=[Eg_r], W=[Eg_r])
        k.I("dve", lambda e: e.tensor_tensor(out=Sg[:], in0=Lg[:], in1=m8[:, :, 1:2].to_broadcast([128, NSB, 8]), op=ALU.is_ge),
            R=[Lg_r, m8_r], W=[Sg_r])
        k.I("dve", lambda e: e.tensor_tensor(out=Eg[:], in0=Eg[:], in1=Sg[:], op=ALU.mult), R=[Eg_r, Sg_r], W=[Eg_r])
        k.I("dve", lambda e: e.tensor_tensor(out=rd[:], in0=m8[:, :, 1], in1=m8[:, :, 0], op=ALU.subtract), R=[m8_r], W=[rd_r])
        k.I("act", lambda e: e.activation(out=rd[:], in_=rd[:], func=AF.Exp), R=[rd_r], W=[rd_r])
        k.I("dve", lambda e: e.tensor_scalar(out=rd[:], in0=rd[:], scalar1=1.0, scalar2=None, op0=ALU.add), R=[rd_r], W=[rd_r])
        k.I("dve", lambda e: e.reciprocal(out=rd[:], in_=rd[:]), R=[rd_r], W=[rd_r])
        k.I("dve", lambda e: e.tensor_tensor(out=Gb[:], in0=Eg[:], in1=rd[:].rearrange("p (s o) -> p s o", o=1).to_broadcast([128, NSB, 8]),
                                              op=ALU.mult), R=[Eg_r, rd_r], W=[Gb_r])
        for ex in range(n_exp):
            for sb_ in range(NSB):
                k.I("dve", lambda e: e.tensor_copy(out=Gbig[:], in_=Gb[:, sb_, ex:ex + 1].to_broadcast([128, 128])),
                    R=[Gb_r], W=[Gbig_r])
                pp, pp_r = (PA, PB)[(sb_ // 4) % 2]
                k.I("pe", lambda e: e.matmul(pp[:, (sb_ % 4) * 128:(sb_ % 4 + 1) * 128], Gbig[:], cmb(C, "ident"), start=True, stop=True),
                    R=[Gbig_r, C.cmb_r], W=[pp_r])
                if sb_ % 4 == 3:
                    k.I("act", lambda e: e.copy(out=gbc[:, ex, (sb_ - 3) * 128:(sb_ + 1) * 128], in_=pp[:]), R=[pp_r], W=[gbc_r])
        pend = None

        def gu(ex, wset, tt, icnt):
            (wg_, wg_r), (wu_, wu_r), (wd_, wd_r) = wset
            a_, a_r = act[icnt % 2]
            tsl = slice(tt * T, (tt + 1) * T)
            for fc in range(4):
                (pg, pg_r), (pu, pu_r) = ((PA, PB), (PS0, PS1))[fc % 2]
                for kc in range(8):
                    k.I("pe", lambda e: e.matmul(pg[:], wg_[:, kc, fc * 128:(fc + 1) * 128], hn[:, kc, tsl],
                                                 start=(kc == 0), stop=(kc == 7)), R=[wg_r, hn_r], W=[pg_r], inc=(kc == 7))
                for kc in range(8):
                    k.I("pe", lambda e: e.matmul(pu[:], wu_[:, kc, fc * 128:(fc + 1) * 128], hn[:, kc, tsl],
                                                 start=(kc == 0), stop=(kc == 7)), R=[wu_r, hn_r], W=[pu_r], inc=(kc == 7))
                s_, s_r = sg[fc % 2]
                s2, s2_r = sg2[fc % 2]
                k.I("act", lambda e: e.activation(out=s_[:], in_=pg[:], func=AF.Silu), R=[pg_r], W=[s_r])
                k.I("dve", lambda e: e.tensor_tensor(out=s2[:], in0=s_[:], in1=gbc[:, ex, tsl], op=ALU.mult),
                    R=[s_r, gbc_r], W=[s2_r])
                k.I("dve", lambda e: e.tensor_tensor(out=a_[:, fc, :], in0=pu[:], in1=s2[:], op=ALU.mult),
                    R=[pu_r, s2_r], W=[a_r])

        def down(wset, tt, icnt):
            (wd_, wd_r) = wset[2]
            a_, a_r = act[icnt % 2]
            tsl = slice(tt * T, (tt + 1) * T)
            for dc in range(8):
                pp, pp_r = (PO0, PO1)[dc % 2]
                for fc in range(4):
                    k.I("pe", lambda e: e.matmul(pp[:], wd_[:, fc, dc * 128:(dc + 1) * 128], a_[:, fc, :],
                                                 start=(fc == 0), stop=(fc == 3)), R=[wd_r, a_r], W=[pp_r], inc=(fc == 3))
                k.I("dve", lambda e: e.tensor_tensor(out=hs[:, dc, tsl], in0=pp[:], in1=hs[:, dc, tsl], op=ALU.add),
                    R=[pp_r, hs_r], W=[hs_r])

        icnt = 0
        for ex in range(n_exp):
            for fg in range(7):
                b = wcount % 2
                wcount += 1
                wset = (wgs[b], wus[b], wds[b])
                (wg_, wg_r), (wu_, wu_r), (wd_, wd_r) = wset
                f0 = fg * 512
                for kc in range(8):
                    k.dma("pool", wg_[:, kc, :], wg_d[ex, kc * 128:(kc + 1) * 128, f0:f0 + 512], W=[wg_r])
                    k.dma("pool", wu_[:, kc, :], wu_d[ex, kc * 128:(kc + 1) * 128, f0:f0 + 512], W=[wu_r])
                for fc in range(4):
                    k.dma("pool", wd_[:, fc, :], wd_d[ex, f0 + fc * 128:f0 + (fc + 1) * 128, :], W=[wd_r])
                for tt in range(NTT):
                    gu(ex, wset, tt, icnt)
                    if pend is not None:
                        down(*pend)
                    pend = (wset, tt, icnt)
                    icnt += 1
        down(*pend)
        pend = None
        for c in range(8):
            k.dma("sp", h_out[c, :, s0:s0 + SUP], hs[:, c, :], R=[hs_r])


def cmf(C, name):
    o = CM[name]
    return C.cmf[:, o:o + 128]


def g_ssd(k, nc, C, h_in, h_out, rowc_d, w_in_d, w_out_d, bf=None):
    (PA, PB, PS0, PS1, PY, PZ, PM) = C.P
    PTt, PT_r = C.PT
    wout, wout_r = k.sb("swout", [128, 16, 1024], BF16)
    wdt, wdt_r = k.sb("swdt", [128, 8, 32], BF16)
    NWB = 2 if bf is None else 3
    wblk = [k.sb(f"swb{i}", [128, 8, 512], BF16) for i in range(NWB)]
    rowc, rowc_r = k.sb("rowc", [128, 96], F32)
    abc, abc_r = k.sb("sabc", [128, 32], F32)
    h, h_r = k.sb("sh", [128, 8, T], F32)
    hn, hn_r = k.sb("shn", [128, 8, T], BF16)
    sq, _ = k.sb("ssq", [128, 2, T], BF16)
    sq_r = [Res(), Res()]
    rstd, rstd_r = k.sb("srstd", [128, T], F32)
    xr = [k.sb(f"sxr{i}", [128, 515], F32) for i in range(2)]
    acc = [k.sb(f"sacc{i}", [128, T], F32) for i in range(2)]
    halo, halo_r = k.sb("shalo", [128, 24, 3], F32)
    xbcT, xbcT_r = k.sb("sxbcT", [128, 24, T], BF16)
    zs, zs_r = k.sb("szs", [128, 4, 2048], BF16)
    dt, dt_r = k.sb("sdt", [128, 4, 32], F32)
    da, da_r = k.sb("sda", [128, 4, 32], F32)
    xtok, xtok_r = k.sb("sxtok", [128, 2048], BF16)
    btok, btok_r = k.sb("sbtok", [128, 512], BF16)
    acum, acum_r = k.sb("sacum", [128, 64], F32)
    ea, ea_r = k.sb("sea", [128, 32], F32)
    nacum, nacum_r = k.sb("snacum", [128, 32], F32)
    te, te_r = k.sb("ste", [128, 32], F32)
    cd, cd_r = k.sb("scd", [128, 32], F32)
    seg = [k.sb(f"sseg{i}", [128, 512], F32) for i in range(2)]
    mt_ = [k.sb(f"smt{i}", [128, 512], BF16) for i in range(2)]
    cbm, cbm_r = k.sb("scbm", [128, 4, 128], F32)
    xdt, xdt_r = k.sb("sxdt", [128, 2048], BF16)
    xw, xw_r = k.sb("sxw", [128, 2048], BF16)
    state, state_r = k.sb("sstate", [128, 2048], F32)
    stb, stb_r = k.sb("sstb", [128, 2048], BF16)
    tmp = [k.sb(f"stmp{i}", [128, 512], F32) for i in range(2)]
    tmp2 = [k.sb(f"stmq{i}", [128, 512], F32) for i in range(2)]
    yz, yz_r = k.sb("syz", [128, 512], F32)
    junk, junk_r = k.sb("sjunk", [128, 512], BF16)
    ssq, ssq_r = k.sb("sssq", [128, 4], F32)
    yn, yn_r = k.sb("syn", [128, 2048], BF16)
    ynT, ynT_r = k.sb("synT", [128, 16, T], BF16)

    if bf is None:
        load_w_bf16(k, wout, wout_r, w_out_d, 16, 1024)
    else:
        load_bf(k, wout, wout_r, bf["wout"], 0, 1024)
    for kc in range(8):
        k.dma("pool", wdt[:, kc, :], w_in_d[kc * 128:(kc + 1) * 128, 5120:5152], W=[wdt_r])
    k.dma("sp", rowc[:], rowc_d[:, :], W=[rowc_r])
    k.I("act", lambda e: e.activation(out=abc[:], in_=rowc[:, 32:64], func=AF.Exp), R=[rowc_r], W=[abc_r])
    k.I("dve", lambda e: e.tensor_scalar(out=abc[:], in0=abc[:], scalar1=-1.0, scalar2=None, op0=ALU.mult), R=[abc_r], W=[abc_r])
    for kc in range(16):
        k.I("dve", lambda e: e.tensor_scalar(out=wout[:, kc, :], in0=wout[:, kc, :], scalar1=col(C, "ssdn", kc), scalar2=None,
                                              op0=ALU.mult), R=[wout_r, C.cst_r], W=[wout_r])
    k.I("pool", lambda e: e.memset(halo[:], 0.0), W=[halo_r])
    k.I("pool", lambda e: e.memset(state[:], 0.0), W=[state_r])
    k.I("pool", lambda e: e.memset(stb[:], 0.0), W=[stb_r])
    wc = 0
    hcount = 0
    for t in range(NT):
        t0 = t * T
        for c in range(8):
            k.dma("sp", h[:, c, :], h_in[c, :, t0:t0 + T], W=[h_r])
        rmsnorm_T(k, C, h, h_r, "mix1", hn, hn_r, sq, sq_r, rstd, rstd_r, PM)
        for zb in range(4):
            wb, wb_r = wblk[wc % NWB]
            wc += 1
            if bf is None:
                for kc in range(8):
                    k.dma("pool", wb[:, kc, :], w_in_d[kc * 128:(kc + 1) * 128, zb * 512:(zb + 1) * 512], W=[wb_r])
            else:
                load_bf(k, wb, wb_r, bf["win"], zb * 512, (zb + 1) * 512)
            for qs in range(4):
                pp, pp_r = (PA, PB)[qs % 2]
                for kc in range(8):
                    k.I("pe", lambda e: e.matmul(pp[:], hn[:, kc, qs * 128:(qs + 1) * 128], wb[:, kc, :],
                                                 start=(kc == 0), stop=(kc == 7)), R=[hn_r, wb_r], W=[pp_r], inc=(kc == 7))
                k.I("act", lambda e: e.activation(out=zs[:, qs, zb * 512:(zb + 1) * 512], in_=pp[:], func=AF.Silu),
                    R=[pp_r], W=[zs_r])
        for xb in range(6):
            wb, wb_r = wblk[wc % NWB]
            wc += 1
            if bf is None:
                for kc in range(8):
                    k.dma("pool", wb[:, kc, :], w_in_d[kc * 128:(kc + 1) * 128, 2048 + xb * 512:2048 + (xb + 1) * 512], W=[wb_r])
            else:
                load_bf(k, wb, wb_r, bf["win"], 2048 + xb * 512, 2048 + (xb + 1) * 512)
            for c4 in range(4):
                ch = xb * 4 + c4
                pp, pp_r = (PA, PB)[c4 % 2]
                x_, x_r = xr[c4 % 2]
                a_, a_r = acc[c4 % 2]
                for kc in range(8):
                    k.I("pe", lambda e: e.matmul(pp[:], wb[:, kc, c4 * 128:(c4 + 1) * 128], hn[:, kc, :],
                                                 start=(kc == 0), stop=(kc == 7)), R=[hn_r, wb_r], W=[pp_r], inc=(kc == 7))
                k.I("act", lambda e: e.copy(out=x_[:, 3:515], in_=pp[:]), R=[pp_r], W=[x_r])
                k.I("act", lambda e: e.copy(out=x_[:, 0:3], in_=halo[:, ch, :]), R=[halo_r], W=[x_r])
                k.I("act", lambda e: e.copy(out=halo[:, ch, :], in_=x_[:, 512:515]), R=[x_r], W=[halo_r])
                cw = CST["convw"] + 4 * ch
                k.I("dve", lambda e: e.tensor_scalar(out=a_[:], in0=x_[:, 0:512], scalar1=C.cst[:, cw:cw + 1], scalar2=None, op0=ALU.mult),
                    R=[x_r, C.cst_r], W=[a_r])
                for i in range(1, 4):
                    k.I("dve", lambda e: e.scalar_tensor_tensor(out=a_[:], in0=x_[:, i:i + 512], scalar=C.cst[:, cw + i:cw + i + 1],
                                                                in1=a_[:], op0=ALU.mult, op1=ALU.add),
                        R=[x_r, C.cst_r], W=[a_r])
                k.I("act", lambda e: e.activation(out=xbcT[:, ch, :], in_=a_[:], func=AF.Silu, bias=col(C, "convb", ch)),
                    R=[a_r, C.cst_r], W=[xbcT_r])
        for qs in range(4):
            for kc in range(8):
                k.I("pe", lambda e: e.matmul(PM[0][:, qs * 32:(qs + 1) * 32], hn[:, kc, qs * 128:(qs + 1) * 128], wdt[:, kc, :],
                                             start=(kc == 0), stop=(kc == 7)), R=[hn_r, wdt_r], W=[PM[1]],
                    inc=(kc == 7 and qs == 3))
        k.I("dve", lambda e: e.tensor_tensor(out=dt[:], in0=PM[0][:, 0:128].rearrange("p (q h) -> p q h", q=4),
                                              in1=rowc[:, 0:32].rearrange("p (o h) -> p o h", o=1).to_broadcast([128, 4, 32]),
                                              op=ALU.add), R=[PM[1], rowc_r], W=[dt_r])
        k.I("act", lambda e: e.activation(out=dt[:], in_=dt[:], func=AF.Exp), R=[dt_r], W=[dt_r])
        k.I("act", lambda e: e.activation(out=dt[:], in_=dt[:], func=AF.Ln, bias=1.0), R=[dt_r], W=[dt_r])
        k.I("dve", lambda e: e.tensor_tensor(out=da[:], in0=dt[:],
                                              in1=abc[:].rearrange("p (o h) -> p o h", o=1).to_broadcast([128, 4, 32]), op=ALU.mult),
            R=[dt_r, abc_r], W=[da_r])
        def ch_pre(qs):
            cs = slice(qs * 128, (qs + 1) * 128)
            first = (t == 0 and qs == 0)
            for half in range(2):
                for j in range(8):
                    k.I("pe", lambda e: e.transpose(PTt[:, j * 128:(j + 1) * 128], xbcT[:, half * 8 + j, cs], cmb(C, "ident")),
                        R=[xbcT_r, C.cmb_r], W=[PT_r], inc=(j == 7))
                k.I("act", lambda e: e.copy(out=xtok[:, half * 1024:(half + 1) * 1024], in_=PTt[:]), R=[PT_r], W=[xtok_r])
            for j in range(4):
                k.I("pe", lambda e: e.transpose(PTt[:, j * 128:(j + 1) * 128], xbcT[:, 16 + j, cs], cmb(C, "ident")),
                    R=[xbcT_r, C.cmb_r], W=[PT_r], inc=(j == 3))
            k.I("act", lambda e: e.copy(out=btok[:], in_=PTt[:, 0:512]), R=[PT_r], W=[btok_r])
            k.I("pe", lambda e: e.matmul(PM[0][:, 0:32], cmf(C, "upper"), da[:, qs, :], start=True, stop=True),
                R=[da_r, C.cmf_r], W=[PM[1]])
            k.I("pe", lambda e: e.matmul(PM[0][:, 32:64], cmf(C, "ones"), da[:, qs, :], start=True, stop=True),
                R=[da_r, C.cmf_r], W=[PM[1]])
            k.I("act", lambda e: e.copy(out=acum[:], in_=PM[0][:, 0:64]), R=[PM[1]], W=[acum_r])
            k.I("act", lambda e: e.activation(out=ea[:], in_=acum[:, 0:32], func=AF.Exp), R=[acum_r], W=[ea_r])
            k.I("act", lambda e: e.mul(out=nacum[:], in_=acum[:, 0:32], mul=-1.0), R=[acum_r], W=[nacum_r])
            k.I("act", lambda e: e.activation(out=cd[:], in_=acum[:, 32:64], func=AF.Exp), R=[acum_r], W=[cd_r])
            k.I("dve", lambda e: e.tensor_tensor(out=te[:], in0=acum[:, 32:64], in1=acum[:, 0:32], op=ALU.subtract),
                R=[acum_r], W=[te_r])
            k.I("act", lambda e: e.activation(out=te[:], in_=te[:], func=AF.Exp), R=[te_r], W=[te_r])
            for g in range(4):
                pp, pp_r = (PA, PB)[g % 2]
                k.I("pe", lambda e: e.matmul(pp[:, 0:128], xbcT[:, 16 + g, cs], xbcT[:, 20 + g, cs], start=True, stop=True),
                    R=[xbcT_r], W=[pp_r])
                k.I("dve", lambda e: e.tensor_tensor(out=cbm[:, g, :], in0=pp[:, 0:128], in1=cmf(C, "upper"), op=ALU.mult),
                    R=[pp_r, C.cmf_r], W=[cbm_r])
            k.I("dve", lambda e: e.tensor_tensor(out=xdt[:].rearrange("p (h d) -> p h d", h=32),
                                                  in0=xtok[:].rearrange("p (h d) -> p h d", h=32),
                                                  in1=dt[:, qs, :].rearrange("p (h o) -> p h o", o=1).to_broadcast([128, 32, 64]),
                                                  op=ALU.mult), R=[xtok_r, dt_r], W=[xdt_r])
            k.I("dve", lambda e: e.tensor_tensor(out=xw[:].rearrange("p (h d) -> p h d", h=32),
                                                  in0=xdt[:].rearrange("p (h d) -> p h d", h=32),
                                                  in1=te[:].rearrange("p (h o) -> p h o", o=1).to_broadcast([128, 32, 64]),
                                                  op=ALU.mult), R=[xdt_r, te_r], W=[xw_r])
        def ch_heads(qs):
            cs = slice(qs * 128, (qs + 1) * 128)
            PYs = [PY, PA]
            PZs = [PZ, PB]

            def s1(q):
                ps_, ps_r = (PS0, PS1)[q % 2]
                for i in range(4):
                    hh = 4 * q + i
                    k.I("pe", lambda e: e.matmul(ps_[:, i * 128:(i + 1) * 128], da[:, qs, hh:hh + 1].to_broadcast([128, 128]),
                                                 cmf(C, "upper"), start=True, stop=True), R=[da_r, C.cmf_r], W=[ps_r], inc=(i == 3))

            def s2(q):
                sg_, sg_r = seg[q % 2]
                ps_, ps_r = (PS0, PS1)[q % 2]
                k.I("dve", lambda e: e.tensor_tensor(out=sg_[:].rearrange("p (h t) -> p h t", h=4),
                                                      in0=ps_[:].rearrange("p (h t) -> p h t", h=4),
                                                      in1=acum[:, 4 * q:4 * q + 4].rearrange("p (h o) -> p h o", o=1).to_broadcast([128, 4, 128]),
                                                      op=ALU.subtract), R=[ps_r, acum_r], W=[sg_r])
                k.I("act", lambda e: e.activation(out=sg_[:], in_=sg_[:], func=AF.Relu, scale=-1.0), R=[sg_r], W=[sg_r])
                k.I("act", lambda e: e.activation(out=sg_[:], in_=sg_[:], func=AF.Exp, scale=-1.0), R=[sg_r], W=[sg_r])

            def s3(q):
                g = q // 2
                gs = slice(g * 512, (g + 1) * 512)
                sg_, sg_r = seg[q % 2]
                m_, m_r = mt_[q % 2]
                py, py_r = PYs[g % 2]
                pz, pz_r = PZs[g % 2]
                k.I("dve", lambda e: e.tensor_tensor(out=m_[:].rearrange("p (h t) -> p h t", h=4),
                                                      in0=sg_[:].rearrange("p (h t) -> p h t", h=4),
                                                      in1=cbm[:, g:g + 1, :].to_broadcast([128, 4, 128]), op=ALU.mult),
                    R=[sg_r, cbm_r], W=[m_r])
                for i in range(4):
                    hh = 4 * q + i
                    hl = hh % 8
                    k.I("pe", lambda e: e.matmul(py[:, hl * 64:(hl + 1) * 64], m_[:, i * 128:(i + 1) * 128], xdt[:, hh * 64:(hh + 1) * 64],
                                                 start=True, stop=True), R=[m_r, xdt_r], W=[py_r], inc=(i == 3))
                if q % 2 == 0:
                    return
                k.I("pe", lambda e: e.matmul(pz[:], xbcT[:, 20 + g, cs], stb[:, gs], start=True, stop=True),
                    R=[xbcT_r, stb_r], W=[pz_r])
                t1_, t1_r = tmp[g % 2]
                t2_, t2_r = tmp2[g % 2]
                k.I("dve", lambda e: e.tensor_tensor(out=t1_[:].rearrange("p (h d) -> p h d", h=8),
                                                      in0=pz[:].rearrange("p (h d) -> p h d", h=8),
                                                      in1=ea[:, g * 8:(g + 1) * 8].rearrange("p (h o) -> p h o", o=1).to_broadcast([128, 8, 64]),
                                                      op=ALU.mult), R=[pz_r, ea_r], W=[t1_r])
                k.I("dve", lambda e: e.tensor_tensor(out=t1_[:], in0=py[:], in1=t1_[:], op=ALU.add), R=[py_r, t1_r], W=[t1_r])
                k.I("dve", lambda e: e.tensor_tensor(out=t2_[:].rearrange("p (h d) -> p h d", h=8),
                                                      in0=xtok[:, gs].rearrange("p (h d) -> p h d", h=8),
                                                      in1=rowc[:, 64 + g * 8:64 + (g + 1) * 8].rearrange("p (h o) -> p h o", o=1).to_broadcast([128, 8, 64]),
                                                      op=ALU.mult), R=[xtok_r, rowc_r], W=[t2_r])
                k.I("dve", lambda e: e.tensor_tensor(out=t1_[:], in0=t1_[:], in1=t2_[:], op=ALU.add), R=[t1_r, t2_r], W=[t1_r])
                k.I("dve", lambda e: e.tensor_tensor(out=t2_[:], in0=t1_[:], in1=zs[:, qs, gs], op=ALU.mult), R=[t1_r, zs_r], W=[t2_r])
                k.I("act", lambda e: e.activation(out=junk[:], in_=t2_[:], func=AF.Square, accum_out=ssq[:, g:g + 1]),
                    R=[t2_r], W=[junk_r, ssq_r])
                k.I("act", lambda e: e.activation(out=ssq[:, g:g + 1], in_=ssq[:, g:g + 1], func=AF.Ln, bias=EPS, scale=1.0 / 512),
                    R=[ssq_r], W=[ssq_r])
                k.I("act", lambda e: e.activation(out=ssq[:, g:g + 1], in_=ssq[:, g:g + 1], func=AF.Exp, scale=-0.5),
                    R=[ssq_r], W=[ssq_r])
                k.I("act", lambda e: e.activation(out=yn[:, gs], in_=t2_[:], func=AF.Copy, scale=ssq[:, g:g + 1]),
                    R=[t2_r, ssq_r], W=[yn_r])
                k.I("pe", lambda e: e.matmul(pz[:], btok[:, g * 128:(g + 1) * 128], xw[:, gs], start=True, stop=True),
                    R=[btok_r, xw_r], W=[pz_r])
                k.I("pool", lambda e: e.tensor_tensor(out=state[:, gs].rearrange("p (h d) -> p h d", h=8),
                                                       in0=state[:, gs].rearrange("p (h d) -> p h d", h=8),
                                                       in1=cd[:, g * 8:(g + 1) * 8].rearrange("p (h o) -> p h o", o=1).to_broadcast([128, 8, 64]),
                                                       op=ALU.mult), R=[cd_r], W=[state_r])
                k.I("dve", lambda e: e.tensor_tensor(out=state[:, gs], in0=pz[:], in1=state[:, gs], op=ALU.add),
                    R=[pz_r], W=[state_r])
                k.I("act", lambda e: e.copy(out=stb[:, gs], in_=state[:, gs]), R=[state_r], W=[stb_r])

            for j in range(8 + 2):
                if j < 8:
                    s1(j)
                if 0 <= j - 1 < 8:
                    s2(j - 1)
                if 0 <= j - 2 < 8:
                    s3(j - 2)
        def ch_post(qs):
            cs = slice(qs * 128, (qs + 1) * 128)
            for half in range(2):
                for j in range(8):
                    k.I("pe", lambda e: e.transpose(PTt[:, j * 128:(j + 1) * 128], yn[:, (half * 8 + j) * 128:(half * 8 + j + 1) * 128],
                                                    cmb(C, "ident")), R=[yn_r, C.cmb_r], W=[PT_r], inc=(j == 7))
                k.I("act", lambda e: e.copy(out=ynT[:, half * 8:(half + 1) * 8, cs], in_=PTt[:].rearrange("p (c q) -> p c q", c=8)),
                    R=[PT_r], W=[ynT_r])
        ch_pre(0)
        for qs in range(4):
            ch_heads(qs)
            if qs + 1 < 4:
                ch_pre(qs + 1)
            ch_post(qs)
        for dc in range(8):
            pp, pp_r = (PA, PB)[dc % 2]
            for kc in range(16):
                k.I("pe", lambda e: e.matmul(pp[:], wout[:, kc, dc * 128:(dc + 1) * 128], ynT[:, kc, :],
                                             start=(kc == 0), stop=(kc == 15)), R=[wout_r, ynT_r], W=[pp_r], inc=(kc == 15))
            k.I("dve", lambda e: e.tensor_tensor(out=h[:, dc, :], in0=pp[:], in1=h[:, dc, :], op=ALU.add),
                R=[pp_r, h_r], W=[h_r])
        for c in range(8):
            k.dma("sp", h_out[c, :, t0:t0 + T], h[:, c, :], R=[h_r])


_CACHE = {}


def build_program():
    nc = bass.Bass("TRN2", target_bir_lowering=False)
    def din(name, shape, dt=F32):
        return nc.dram_tensor(name, list(shape), dt, kind="ExternalInput").ap()
    xT = din("xT", [8, 128, S]); memT = din("memT", [8, 128, 256]); pos = din("pos", [S], I32)
    cst_d = din("cst", [128, NCST]); cm_d = din("cm", [128, NCM]); rowc_d = din("rowc", [128, 96])
    hy_w_in = din("hy_w_in", [1024, 2048]); hy_w_out = din("hy_w_out", [1024, 1024]); pool_w = din("pool_w", [4, 128, 128])
    xa_wq = [din(f"xa_wq{l}", [1024, 1024]) for l in range(2)]
    xa_wkv = [din(f"xa_wkv{l}", [1024, 2048]) for l in range(2)]
    xa_wo = [din(f"xa_wo{l}", [1024, 1024]) for l in range(2)]
    ffn_wg = din("ffn_wg", [1024, 2816]); ffn_wu = din("ffn_wu", [1024, 2816]); ffn_wd = din("ffn_wd", [2816, 1024])
    ssd_w_in = din("ssd_w_in", [1024, 5152]); ssd_w_out = din("ssd_w_out", [2048, 1024])
    moe_wr = din("moe_wr", [1024, 8]); moe_wg = din("moe_wg", [8, 1024, 3584]); moe_wu = din("moe_wu", [8, 1024, 3584])
    moe_wd = din("moe_wd", [8, 3584, 1024])
    hs_ = [nc.dram_tensor(f"hscr{i}", [8, 128, S], F32, kind="Internal").ap() for i in range(5)]
    outT = nc.dram_tensor("outT", [8, 128, S], F32, kind="ExternalOutput").ap()
    with ExitStack() as es:
        k = KB(nc, es)
        C = setup_common(k, nc, cst_d, cm_d)
        def group(fn):
            with ExitStack() as ges:
                k.es = ges
                fn()
                k.barrier()
        BFW = {}

        def casts():
            for l in range(2):
                BFW[f"wq{l}"] = cast_w(k, nc, xa_wq[l], 1024, 1024, f"bf_wq{l}")
                BFW[f"wkv{l}"] = cast_w(k, nc, xa_wkv[l], 1024, 2048, f"bf_wkv{l}")
                BFW[f"wo{l}"] = cast_w(k, nc, xa_wo[l], 1024, 1024, f"bf_wo{l}")
                if l == 0:
                    BFW["wg"] = cast_w(k, nc, ffn_wg, 1024, 2816, "bf_wg")
                    BFW["wu"] = cast_w(k, nc, ffn_wu, 1024, 2816, "bf_wu")
                    BFW["wd"] = cast_w(k, nc, ffn_wd, 2816, 1024, "bf_wd")
                    BFW["win"] = cast_w(k, nc, ssd_w_in, 1024, 5152, "bf_win")
                    BFW["wout"] = cast_w(k, nc, ssd_w_out, 2048, 1024, "bf_wout")

        def xbf(l):
            return {"wq": BFW[f"wq{l}"], "wkv": BFW[f"wkv{l}"], "wo": BFW[f"wo{l}"]}

        group(lambda: g1_moba(k, nc, C, es, xT, hs_[0], pos, hy_w_in, hy_w_out, pool_w, after_loads=casts))
        group(lambda: g_xattn(k, nc, C, hs_[0], hs_[1], memT, xa_wq[0], xa_wkv[0], xa_wo[0], 0, bf=xbf(0)))
        group(lambda: g_ffn(k, nc, C, hs_[1], hs_[2], ffn_wg, ffn_wu, ffn_wd, bf=BFW))
        group(lambda: g_ssd(k, nc, C, hs_[2], hs_[3], rowc_d, ssd_w_in, ssd_w_out, bf=BFW))
        group(lambda: g_xattn(k, nc, C, hs_[3], hs_[4], memT, xa_wq[1], xa_wkv[1], xa_wo[1], 1, bf=xbf(1)))
        group(lambda: g_moe(k, nc, C, hs_[4], outT, moe_wr, moe_wg, moe_wu, moe_wd))
    return nc


def kernel(**inp):
    inp = {k_: np.asarray(v) for k_, v in inp.items()}
    if "nc" not in _CACHE:
        _CACHE["nc"] = build_program()
    nc = _CACHE["nc"]
    c, m = host_consts(inp)
    rowc = host_rowc(inp)
    f32 = lambda a: np.ascontiguousarray(a, dtype=np.float32)
    shared = {
        "pos": np.ascontiguousarray(inp["positions"], dtype=np.int32), "cst": c, "cm": m, "rowc": rowc,
        "hy_w_in": f32(inp["hy_w_in"][0]), "hy_w_out": f32(inp["hy_w_out"][0]), "pool_w": f32(inp["pool_w"][0]),
        "ffn_wg": f32(inp["ffn_w_gate"][0]), "ffn_wu": f32(inp["ffn_w_up"][0]), "ffn_wd": f32(inp["ffn_w_down"][0]),
        "ssd_w_in": f32(inp["ssd_w_in"][0]), "ssd_w_out": f32(inp["ssd_w_out"][0]),
        "moe_wr": f32(inp["moe_router"][0]), "moe_wg": f32(inp["moe_w_gate"][0]), "moe_wu": f32(inp["moe_w_up"][0]),
        "moe_wd": f32(inp["moe_w_down"][0]),
    }
    for l in range(2):
        shared[f"xa_wq{l}"] = f32(inp["xa_wq"][l]); shared[f"xa_wkv{l}"] = f32(inp["xa_wkv"][l]); shared[f"xa_wo{l}"] = f32(inp["xa_wo"][l])
    in_maps = []
    for b in range(8):
        mp = dict(shared)
        mp["xT"] = np.ascontiguousarray(inp["x"][b].T, dtype=np.float32).reshape(8, 128, S)
        mp["memT"] = np.ascontiguousarray(inp["mem"][b].T, dtype=np.float32).reshape(8, 128, 256)
        in_maps.append(mp)
    res = run_bass_kernel_spmd(nc, in_maps, core_ids=list(range(8)))
    out = np.empty((8, S, D), np.float32)
    for b in range(8):
        out[b] = np.asarray(res.results[b]["outT"]).reshape(D, S).T
    return out
```

```python
from concourse.bass_utils import run_bass_kernel_spmd
import numpy as np
import concourse.bass as bass
import concourse.mybir as mybir
from contextlib import ExitStack

F32 = mybir.dt.float32
BF16 = mybir.dt.bfloat16
I32 = mybir.dt.int32
AF = mybir.ActivationFunctionType
ALU = mybir.AluOpType
AX = mybir.AxisListType


class Res:
    __slots__ = ("w", "r", "name")

    def __init__(self, name=""):
        self.w = None
        self.r = {}
        self.name = name


class KB:
    RING = {"sp": 16, "pool": 12, "act": 4}

    def __init__(self, nc, es):
        self.nc = nc
        self.es = es
        self.eng = {"pe": nc.tensor, "dve": nc.vector, "act": nc.scalar,
                    "pool": nc.gpsimd, "sp": nc.sync}
        self.sems = {}
        for e in ("pe", "dve", "act", "pool"):
            self.sems[e] = es.enter_context(nc.semaphore("c_" + e))
        self.cnt = {e: 0 for e in ("pe", "dve", "act", "pool")}
        self.ring = {}
        self.ring_i = {}
        for q, n in self.RING.items():
            self.ring[q] = []
            for i in range(n):
                key = f"d_{q}{i}"
                self.sems[key] = es.enter_context(nc.semaphore(key))
                self.ring[q].append(key)
            self.ring_i[q] = 0
        self.seen = {e: {} for e in self.eng}
        self.n_inst = 0
        self.n_wait = 0
        self.out_tokens = []

    def sb(self, name, shape, dtype):
        self.uid = getattr(self, "uid", 0) + 1
        t = self.es.enter_context(self.nc.sbuf_tensor(f"s{self.uid}_" + name, list(shape), dtype))
        return t, Res(name)

    def ps(self, name, shape, dtype):
        t = self.es.enter_context(self.nc.psum_tensor("p_" + name, list(shape), dtype))
        return t, Res(name)

    def _waits(self, e, R, W):
        need = {}
        for r in R:
            if r.w is not None:
                k, v = r.w
                if need.get(k, 0) < v:
                    need[k] = v
        for w in W:
            if w.w is not None:
                k, v = w.w
                if need.get(k, 0) < v:
                    need[k] = v
            for k, v in w.r.items():
                if need.get(k, 0) < v:
                    need[k] = v
        seen = self.seen[e]
        eng = self.eng[e]
        for k, v in need.items():
            if e == "pe" and k == "pe":
                continue
            if seen.get(k, 0) >= v:
                continue
            eng.wait_ge(self.sems[k], v)
            seen[k] = v
            self.n_wait += 1

    def _commit(self, tok, R, W):
        k, v = tok
        for r in R:
            if r.r.get(k, 0) < v:
                r.r[k] = v
        for w in W:
            w.w = tok
            w.r = {}

    def I(self, e, fn, R=(), W=(), inc=True):
        self._waits(e, R, W)
        ins = fn(self.eng[e])
        self.n_inst += 1
        if inc:
            self.cnt[e] += 1
            ins.then_inc(self.sems[e], 1)
            tok = (e, self.cnt[e])
        else:
            tok = (e, self.cnt[e] + 1)
        self._commit(tok, R, W)
        return tok

    def dma(self, q, out, in_, R=(), W=(), **kw):
        e = q
        i = self.ring_i[q]
        n = len(self.ring[q])
        key = self.ring[q][i % n]
        prev = 16 * (i // n)
        eng = self.eng[e]
        if prev > 0 and self.seen[e].get(key, 0) < prev:
            eng.wait_ge(self.sems[key], prev)
            self.seen[e][key] = prev
        self._waits(e, R, W)
        ins = eng.dma_start(out=out, in_=in_, **kw)
        ins.then_inc(self.sems[key], 16)
        self.ring_i[q] = i + 1
        self.n_inst += 1
        tok = (key, prev + 16)
        self._commit(tok, R, W)
        return tok

    def finish(self, toks):
        eng = self.eng["sp"]
        for k, v in toks:
            eng.wait_ge(self.sems[k], v)


def kb_barrier(self):
    for e, eng in self.eng.items():
        for x in ("pe", "dve", "act", "pool"):
            v = self.cnt[x]
            if v > 0 and self.seen[e].get(x, 0) < v and not (e == x):
                eng.wait_ge(self.sems[x], v)
                self.seen[e][x] = v
        for q in self.ring:
            n = len(self.ring[q])
            tot = self.ring_i[q]
            for j in range(min(n, tot)):
                idx = tot - 1 - j
                key = self.ring[q][idx % n]
                v = 16 * (idx // n + 1)
                if self.seen[e].get(key, 0) < v:
                    eng.wait_ge(self.sems[key], v)
                    self.seen[e][key] = v


KB.barrier = kb_barrier


import numpy as np

S = 4096
D = 1024
T = 512
NT = S // T
EPS = 1e-6
NEG = -30000.0

CST = {}
_c = 0
def _col(name, n):
    global _c
    CST[name] = _c
    _c += n
for _n in ("mix0", "mix1", "xa0", "xa1", "mem0", "mem1", "ffn0", "ffn1"):
    _col(_n, 8)
_col("hyq", 1); _col("hyk", 1); _col("pscale", 4)
_col("xaq0", 2); _col("xaq1", 2); _col("xak0", 2); _col("xak1", 2)
_col("invf", 1)
_col("convw", 96); _col("convb", 24); _col("ssdn", 16)
_col("rcnt", 64)
NCST = _c

CM = {"ident": 0, "ones": 128, "bones": 256, "rot": 384, "tri01": 512, "upper": 640}
NCM = 768


def host_consts(inp):
    c = np.zeros((128, NCST), np.float32)
    def put(name, v):
        v = np.asarray(v, np.float32).reshape(-1, 128).T
        c[:, CST[name]:CST[name] + v.shape[1]] = v
    for l in range(2):
        put(f"mix{l}", inp["mix_norm"][l]); put(f"xa{l}", inp["xa_norm"][l])
        put(f"mem{l}", inp["mem_norm"][l]); put(f"ffn{l}", inp["ffn_norm"][l])
        put(f"xaq{l}", inp["xa_q_norm"][l]); put(f"xak{l}", inp["xa_k_norm"][l])
    put("hyq", np.tile(inp["hy_q_norm"][0], 2)); put("hyk", np.tile(inp["hy_k_norm"][0], 2))
    put("pscale", inp["pool_scale"][0])
    invf = (10000.0 ** (-np.arange(32, dtype=np.float32) / 32)).astype(np.float32)
    put("invf", np.tile(invf, 4))
    cw = inp["ssd_conv_w"][0]
    cwl = np.zeros((128, 96), np.float32)
    for ch in range(24):
        for i in range(4):
            cwl[:, 4 * ch + i] = cw[i, ch * 128:(ch + 1) * 128]
    c[:, CST["convw"]:CST["convw"] + 96] = cwl
    put("convb", inp["ssd_conv_b"][0]); put("ssdn", inp["ssd_norm"][0])
    rc = np.zeros((128, 64), np.float32)
    for g, w in enumerate((2, 4, 8, 16)):
        rc[:, g * 16:(g + 1) * 16] = 1.0 / np.minimum(np.arange(16) + 1, w)
    c[:, CST["rcnt"]:CST["rcnt"] + 64] = rc
    m = np.zeros((128, NCM), np.float32)
    m[:, 0:128] = np.eye(128)
    m[:, 128:256] = 1.0
    for p in range(128):
        for q in range(128):
            if p // 64 == q // 64:
                m[p, 256 + q] = 1.0
    for p in range(128):
        if p % 64 < 32:
            m[p + 32, 384 + p] = -1.0
        else:
            m[p - 32, 384 + p] = 1.0
    qi = np.arange(128)[:, None]; ki = np.arange(128)[None, :]
    m[:, 512:640] = (ki <= qi).astype(np.float32)
    m[:, 640:768] = (qi <= ki).astype(np.float32)
    return c, m


def host_rowc(inp):
    r = np.zeros((128, 96), np.float32)
    r[:, 0:32] = inp["ssd_dt_bias"][0][None, :]
    r[:, 32:64] = inp["ssd_a_log"][0][None, :]
    r[:, 64:96] = inp["ssd_d"][0][None, :]
    return r


class Ctx:
    pass


def setup_common(k, nc, cst_d, cm_d):
    C = Ctx()
    C.cst, C.cst_r = k.sb("cst", [128, NCST], F32)
    C.cmf, C.cmf_r = k.sb("cmf", [128, NCM], F32)
    C.cmb, C.cmb_r = k.sb("cmb", [128, NCM], BF16)
    C.trib, C.trib_r = k.sb("trib", [128, 128], F32)
    k.dma("sp", C.cst[:], cst_d[:, :], W=[C.cst_r])
    k.dma("sp", C.cmf[:], cm_d[:, :], W=[C.cmf_r])
    k.I("dve", lambda e: e.tensor_copy(out=C.cmb[:], in_=C.cmf[:]), R=[C.cmf_r], W=[C.cmb_r])
    k.I("dve", lambda e: e.tensor_scalar(out=C.trib[:], in0=C.cmf[:, 512:640], scalar1=-1.0, scalar2=-NEG,
                                          op0=ALU.add, op1=ALU.mult), R=[C.cmf_r], W=[C.trib_r])
    C.P = []
    for i in range(7):
        C.P.append(k.ps(f"P{i}", [128, 512], F32))
    C.PT = k.ps("PTb", [128, 1024], BF16)
    return C


def col(C, name, i=0):
    c = CST[name] + i
    return C.cst[:, c:c + 1]


def cmb(C, name, rows=128):
    o = CM[name]
    return C.cmb[0:rows, o:o + 128]


def load_w_bf16(k, dst, dst_r, src, nk, ncols, q="pool"):
    for c in range(nk):
        for c0 in range(0, ncols, 1024):
            c1 = min(ncols, c0 + 1024)
            k.dma(q, dst[:, c, c0:c1], src[c * 128:(c + 1) * 128, c0:c1], W=[dst_r])


def load_w_staged(k, dst, dst_r, src, nk, ncols, stg, cnt=[0]):
    for c0 in range(0, ncols, 512):
        c1 = min(ncols, c0 + 512)
        for c in range(nk):
            st, st_r = stg[cnt[0] % len(stg)]
            k.dma("sp", st[:, 0:c1 - c0], src[c * 128:(c + 1) * 128, c0:c1], W=[st_r])
            if cnt[0] % 2 == 0:
                k.I("dve", lambda e: e.tensor_copy(out=dst[:, c, c0:c1], in_=st[:, 0:c1 - c0]), R=[st_r], W=[dst_r])
            else:
                k.I("act", lambda e: e.copy(out=dst[:, c, c0:c1], in_=st[:, 0:c1 - c0]), R=[st_r], W=[dst_r])
            cnt[0] += 1


def cast_w(k, nc, src, rows, cols, name):
    dst = nc.dram_tensor(name, [rows, cols], BF16, kind="Internal").ap()
    r = Res(name)
    for r0 in range(0, rows, 128):
        for c0 in range(0, cols, 1024):
            c1 = min(cols, c0 + 1024)
            k.dma("pool", dst[r0:r0 + 128, c0:c1], src[r0:r0 + 128, c0:c1], W=[r])
    return dst, r


def load_bf(k, dst, dst_r, bf, c0, c1, nk=None):
    src, src_r = bf
    v = src.rearrange("(c p) f -> p c f", p=128)
    if nk is not None:
        v = v[:, 0:nk, :]
    k.dma("sp", dst[:, :, 0:c1 - c0], v[:, :, c0:c1], R=[src_r], W=[dst_r])


def rmsnorm_T(k, C, h, h_r, gname, hn, hn_r, sq, sq_r, rstd, rstd_r, PM, n=T):
    pm, pm_r = PM
    ns = len(sq_r)
    for c in range(8):
        k.I("act", lambda e: e.activation(out=sq[:, c % ns, 0:n], in_=h[:, c, 0:n], func=AF.Square),
            R=[h_r], W=[sq_r[c % ns]])
        k.I("pe", lambda e: e.matmul(pm[:, 0:n], cmb(C, "ones"), sq[:, c % ns, 0:n], start=(c == 0), stop=(c == 7)),
            R=[sq_r[c % ns], C.cmb_r], W=[pm_r])
    k.I("act", lambda e: e.activation(out=rstd[:, 0:n], in_=pm[:, 0:n], func=AF.Sqrt, bias=EPS, scale=1.0 / D),
        R=[pm_r], W=[rstd_r])
    k.I("dve", lambda e: e.reciprocal(out=rstd[:, 0:n], in_=rstd[:, 0:n]), R=[rstd_r], W=[rstd_r])
    for c in range(8):
        k.I("dve", lambda e: e.scalar_tensor_tensor(out=hn[:, c, 0:n], in0=h[:, c, 0:n], scalar=col(C, gname, c),
                                                    in1=rstd[:, 0:n], op0=ALU.mult, op1=ALU.mult),
            R=[h_r, rstd_r, C.cst_r], W=[hn_r])


def g1_moba(k, nc, C, es, xT, h1, positions, w_in_d, w_out_d, pool_w_d, after_loads=None):
    (PA, PB, PS0, PS1, PO0, PO1, PM) = C.P
    PTt, PT_r = C.PT
    win, win_r = k.sb("win", [128, 8, 2048], BF16)
    wout, wout_r = k.sb("wout", [128, 8, 1024], BF16)
    poolw, poolw_r = k.sb("poolw", [128, 4, 128], BF16)
    KT, KT_r = k.sb("KT", [128, 4, S], BF16)
    V, V_r = k.sb("V", [128, 32, 512], BF16)
    kmT, kmT_r = k.sb("kmT", [128, 4, 32], BF16)
    h, h_r = k.sb("h", [128, 8, T], F32)
    hn, hn_r = k.sb("hn", [128, 8, T], BF16)
    sq, _ = k.sb("sq", [128, 2, T], BF16)
    sq_r = [Res() for _ in range(2)]
    rstd, rstd_r = k.sb("rstd", [128, T], F32)
    qT, qT_r = k.sb("qT", [128, 4, T], BF16)
    catT, catT_r = k.sb("catT", [128, 8, T], BF16)
    qTz, qTz_r = k.sb("qTz", [128, 8, T], BF16)
    cos, cos_r = k.sb("cos", [128, T], F32)
    sin, sin_r = k.sb("sin", [128, T], F32)
    posi, posi_r = k.sb("posi", [128, T], I32)
    wk = [k.sb(f"wk{i}", [128, 528], F32) for i in range(6)]
    qnb, qnb_r = k.sb("qnb", [128, T], BF16)
    sqb, sqb_r = k.sb("sqb", [128, T], BF16)
    uext = [k.sb(f"uext{g}", [128, 528], F32) for g in range(4)]
    pooled, pooled_r = k.sb("pooled", [128, T], BF16)
    gsb, gsb_r = k.sb("gsb", [128, 8, 16], F32)
    biasq4, _ = k.sb("biasq4", [128, 4, 8, 16], F32)
    biasq4_r = [Res() for _ in range(4)]
    PMq_r = [Res() for _ in range(4)]
    PTv = [(PTt, PT_r), (PA[0][:, :].bitcast(BF16), PA[1])]
    m8, m8_r = k.sb("m8", [128, 8, 8], F32)
    Pb = [k.sb(f"Pb{i}", [128, T], BF16) for i in range(2)]
    PTs = [k.sb(f"PTs{i}", [128, T], BF16) for i in range(2)]
    rs = [k.sb(f"rs{i}", [128, 32], F32) for i in range(4)]
    rinv, rinv_r = k.sb("rinv", [128, 8], F32)
    otok, otok_r = k.sb("otok", [128, 512], BF16)
    dsm, dsm_r = k.sb("dsm", [128, 128], F32)

    for g in range(4):
        k.I("dve", lambda e: e.memset(uext[g][0][:, 0:16], 0.0), W=[uext[g][1]])
    k.I("dve", lambda e: e.memset(kmT[:], 0.0), W=[kmT_r])
    k.I("dve", lambda e: e.memset(qTz[:], 0.0), W=[qTz_r])
    stg = wk[0:4]
    load_w_staged(k, win, win_r, w_in_d, 8, 2048, stg)
    load_w_staged(k, wout, wout_r, w_out_d, 8, 1024, stg)
    for g in range(4):
        k.dma("pool", poolw[:, g, :], pool_w_d[g, :, :], W=[poolw_r])
    if after_loads is not None:
        after_loads()

    twopi = float(2 * np.pi)
    pcount = 0
    for t in range(NT):
        t0 = t * T
        for c in range(8):
            k.dma("sp", h[:, c, :], xT[c, :, t0:t0 + T], W=[h_r])
        k.dma("sp", posi[:], positions[t0:t0 + T].partition_broadcast(128), W=[posi_r])
        (a0, a0r), (a1, a1r), (a2, a2r) = wk[0], wk[1], wk[2]
        k.I("dve", lambda e: e.tensor_copy(out=a0[:, 0:T], in_=posi[:]), R=[posi_r], W=[a0r])
        k.I("dve", lambda e: e.tensor_scalar(out=a0[:, 0:T], in0=a0[:, 0:T], scalar1=col(C, "invf"), scalar2=1.0 / twopi,
                                              op0=ALU.mult, op1=ALU.mult), R=[a0r, C.cst_r], W=[a0r])
        for (dst, dst_r, off) in ((sin, sin_r, 0.5), (cos, cos_r, 0.75)):
            k.I("dve", lambda e: e.tensor_scalar(out=a1[:, 0:T], in0=a0[:, 0:T], scalar1=off, scalar2=None, op0=ALU.add),
                R=[a0r], W=[a1r])
            k.I("dve", lambda e: e.tensor_copy(out=posi[:], in_=a1[:, 0:T]), R=[a1r], W=[posi_r])
            k.I("dve", lambda e: e.tensor_copy(out=a2[:, 0:T], in_=posi[:]), R=[posi_r], W=[a2r])
            k.I("dve", lambda e: e.tensor_tensor(out=a1[:, 0:T], in0=a1[:, 0:T], in1=a2[:, 0:T], op=ALU.subtract),
                R=[a1r, a2r], W=[a1r])
            k.I("dve", lambda e: e.tensor_scalar(out=a2[:, 0:T], in0=a1[:, 0:T], scalar1=0.0, scalar2=None, op0=ALU.is_lt),
                R=[a1r], W=[a2r])
            k.I("dve", lambda e: e.tensor_tensor(out=a1[:, 0:T], in0=a1[:, 0:T], in1=a2[:, 0:T], op=ALU.add),
                R=[a1r, a2r], W=[a1r])
            k.I("act", lambda e: e.activation(out=dst[:], in_=a1[:, 0:T], func=AF.Sin, bias=-float(np.pi), scale=twopi),
                R=[a1r], W=[dst_r])
        rmsnorm_T(k, C, h, h_r, "mix0", hn, hn_r, sq, sq_r, rstd, rstd_r, PM)
        for c in range(8):
            pp, pp_r = (PA, PB)[c % 2]
            for kc in range(8):
                k.I("pe", lambda e: e.matmul(pp[:], win[:, kc, c * 128:(c + 1) * 128], hn[:, kc, :],
                                             start=(kc == 0), stop=(kc == 7)),
                    R=[win_r, hn_r], W=[pp_r], inc=(kc == 7))
            isq = c < 4
            gname = "hyq" if isq else "hyk"
            (qn, qn_r), (t1, t1_r), (t2, t2_r), (r2, r2_r) = wk[0], wk[1], wk[2], wk[3]
            k.I("act", lambda e: e.activation(out=sqb[:], in_=pp[:], func=AF.Square), R=[pp_r], W=[sqb_r])
            k.I("act", lambda e: e.activation(out=qn[:, 0:T], in_=pp[:], func=AF.Copy, scale=col(C, gname)),
                R=[pp_r, C.cst_r], W=[qn_r])
            k.I("pe", lambda e: e.matmul(PM[0][:], cmb(C, "bones"), sqb[:], start=True, stop=True),
                R=[sqb_r, C.cmb_r], W=[PM[1]])
            k.I("act", lambda e: e.activation(out=r2[:, 0:T], in_=PM[0][:], func=AF.Sqrt, bias=EPS, scale=1.0 / 64),
                R=[PM[1]], W=[r2_r])
            k.I("dve", lambda e: e.reciprocal(out=r2[:, 0:T], in_=r2[:, 0:T]), R=[r2_r], W=[r2_r])
            k.I("dve", lambda e: e.tensor_copy(out=qnb[:], in_=qn[:, 0:T]), R=[qn_r], W=[qnb_r])
            k.I("pe", lambda e: e.matmul(PM[0][:], cmb(C, "rot"), qnb[:], start=True, stop=True),
                R=[qnb_r, C.cmb_r], W=[PM[1]])
            k.I("dve", lambda e: e.tensor_tensor(out=t2[:, 0:T], in0=PM[0][:], in1=sin[:], op=ALU.mult),
                R=[PM[1], sin_r], W=[t2_r])
            k.I("dve", lambda e: e.tensor_tensor(out=t1[:, 0:T], in0=qn[:, 0:T], in1=cos[:], op=ALU.mult),
                R=[qn_r, cos_r], W=[t1_r])
            k.I("dve", lambda e: e.tensor_tensor(out=t1[:, 0:T], in0=t1[:, 0:T], in1=t2[:, 0:T], op=ALU.add),
                R=[t1_r, t2_r], W=[t1_r])
            if isq:
                k.I("dve", lambda e: e.tensor_tensor(out=qT[:, c, :], in0=t1[:, 0:T], in1=r2[:, 0:T], op=ALU.mult),
                    R=[t1_r, r2_r], W=[qT_r])
                k.I("act", lambda e: e.copy(out=qTz[0:64, 2 * c, :], in_=qT[0:64, c, :]), R=[qT_r], W=[qTz_r])
                k.I("act", lambda e: e.copy(out=qTz[64:128, 2 * c + 1, :], in_=qT[64:128, c, :]), R=[qT_r], W=[qTz_r])
            else:
                kc4 = c - 4
                k.I("dve", lambda e: e.tensor_tensor(out=t2[:, 0:T], in0=t1[:, 0:T], in1=r2[:, 0:T], op=ALU.mult),
                    R=[t1_r, r2_r], W=[t2_r])
                k.I("act", lambda e: e.copy(out=KT[:, kc4, t0:t0 + T], in_=t2[:, 0:T]), R=[t2_r], W=[KT_r])
                k.I("dve", lambda e: e.tensor_reduce(out=r2[:, 0:2], in_=t2[:, 0:T].rearrange("p (b x) -> p b x", b=2),
                                                      axis=AX.X, op=ALU.add), R=[t2_r], W=[r2_r])
                k.I("act", lambda e: e.mul(out=kmT[0:64, kc4, 2 * t:2 * t + 2], in_=r2[0:64, 0:2], mul=1.0 / 256),
                    R=[r2_r], W=[kmT_r])
                k.I("act", lambda e: e.mul(out=kmT[64:128, kc4, 16 + 2 * t:16 + 2 * t + 2], in_=r2[64:128, 0:2], mul=1.0 / 256),
                    R=[r2_r], W=[kmT_r])
        for qs in range(4):
            pp, pp_r = (PA, PB)[qs % 2]
            for kc in range(8):
                k.I("pe", lambda e: e.matmul(pp[:], hn[:, kc, qs * 128:(qs + 1) * 128], win[:, kc, 1024:1536],
                                             start=(kc == 0), stop=(kc == 7)),
                    R=[win_r, hn_r], W=[pp_r], inc=(kc == 7))
            k.I("act", lambda e: e.copy(out=V[:, 4 * t + qs, :], in_=pp[:]), R=[pp_r], W=[V_r])
        for g in range(4):
            w = 2 << g
            pp, pp_r = (PA, PB)[g % 2]
            ue, ue_r = uext[g]
            for kc in range(8):
                k.I("pe", lambda e: e.matmul(pp[:], win[:, kc, (12 + g) * 128:(13 + g) * 128], hn[:, kc, :],
                                             start=(kc == 0), stop=(kc == 7)),
                    R=[win_r, hn_r], W=[pp_r], inc=(kc == 7))
            k.I("act", lambda e: e.copy(out=ue[:, 16:528], in_=pp[:]), R=[pp_r], W=[ue_r])
            cur, cur_r = ue, ue_r
            step = 1
            bufs = [wk[4], wk[5]]
            bi = 0
            lo = 0
            while step < w:
                lo += step
                nxt, nxt_r = bufs[bi]
                bi ^= 1
                k.I("dve", lambda e: e.tensor_tensor(out=nxt[:, lo:528], in0=cur[:, lo:528], in1=cur[:, lo - step:528 - step],
                                                       op=ALU.add), R=[cur_r], W=[nxt_r])
                cur, cur_r = nxt, nxt_r
                step *= 2
            (pf, pf_r) = wk[3]
            k.I("dve", lambda e: e.scalar_tensor_tensor(out=pf[:, 0:T], in0=cur[:, 16:528], scalar=1.0 / w, in1=ue[:, 16:528],
                                                        op0=ALU.mult, op1=ALU.subtract), R=[cur_r, ue_r], W=[pf_r])
            if t == 0:
                rc = C.cst[:, CST["rcnt"] + 16 * g: CST["rcnt"] + 16 * g + 16]
                k.I("dve", lambda e: e.tensor_tensor(out=pf[:, 0:16], in0=cur[:, 16:32], in1=rc, op=ALU.mult),
                    R=[cur_r, C.cst_r], W=[pf_r])
                k.I("dve", lambda e: e.tensor_tensor(out=pf[:, 0:16], in0=pf[:, 0:16], in1=ue[:, 16:32], op=ALU.subtract),
                    R=[ue_r], W=[pf_r])
            k.I("act", lambda e: e.copy(out=pooled[:], in_=pf[:, 0:T]), R=[pf_r], W=[pooled_r])
            k.I("act", lambda e: e.copy(out=ue[:, 0:16], in_=ue[:, 512:528]), R=[cur_r], W=[ue_r])
            k.I("pe", lambda e: e.matmul(PM[0][:], poolw[:, g, :], pooled[:], start=True, stop=True),
                R=[poolw_r, pooled_r], W=[PM[1]])
            k.I("act", lambda e: e.activation(out=catT[:, 4 + g, :], in_=PM[0][:], func=AF.Copy, scale=col(C, "pscale", g)),
                R=[PM[1], C.cst_r], W=[catT_r])
        for q_ in range(4):
            PMq_r[q_].w = PM[1].w
            PMq_r[q_].r = dict(PM[1].r)
        for qs in range(4):
            gq = 4 * t + qs
            own = gq // 2
            q0 = qs * 128
            bq = biasq4[:, qs]
            if own > 0:
                for c4 in range(4):
                    k.I("pe", lambda e: e.matmul(PM[0][:, qs * 128 + c4 * 32:qs * 128 + (c4 + 1) * 32], qT[:, c4, q0:q0 + 128],
                                                 kmT[:, c4, :], start=True, stop=True),
                        R=[qT_r, kmT_r], W=[PM[1]], inc=(c4 == 3))
                k.I("dve", lambda e: e.memset(gsb[:], -1e30), W=[gsb_r])
                k.I("dve", lambda e: e.tensor_copy(out=gsb[:, :, 0:own],
                                                    in_=PM[0][:, qs * 128:(qs + 1) * 128].rearrange("p (h j) -> p h j", h=8)[:, :, 0:own]),
                    R=[PM[1]], W=[gsb_r])
                for hh in range(8):
                    k.I("dve", lambda e: e.max(out=m8[:, hh, :], in_=gsb[:, hh, :]), R=[gsb_r], W=[m8_r])
                k.I("dve", lambda e: e.tensor_tensor(out=bq[:, :, 0:own], in0=gsb[:, :, 0:own],
                                                      in1=m8[:, :, 2:3].to_broadcast([128, 8, own]), op=ALU.is_lt),
                    R=[gsb_r, m8_r], W=[biasq4_r[qs]])
                k.I("dve", lambda e: e.tensor_scalar(out=bq[:, :, 0:own], in0=bq[:, :, 0:own], scalar1=NEG, scalar2=None,
                                                      op0=ALU.mult), R=[biasq4_r[qs]], W=[biasq4_r[qs]])
            k.I("dve", lambda e: e.memset(bq[:, :, own:own + 1], 0.0), W=[biasq4_r[qs]])
        items = []
        for qs in range(4):
            gq = 4 * t + qs
            ngrp = gq // 4 + 1
            for hh in range(8):
                for kg in range(ngrp):
                    items.append((qs, hh, kg, kg == 0, kg == ngrp - 1, hh == 7 and kg == ngrp - 1))
        nrs_of = {}

        def stA(i):
            qs, hh, kg, fh, lh, lq = items[i]
            gq = 4 * t + qs
            kt_lo = kg * 4
            kt_hi = min(kt_lo + 4, gq + 1)
            nk = kt_hi - kt_lo
            PSx, PSx_r = (PS0, PS1)[i % 2]
            k.I("pe", lambda e: e.matmul(PSx[:, 0:nk * 128], qTz[:, hh, qs * 128:(qs + 1) * 128],
                                         KT[:, hh // 2, kt_lo * 128:kt_hi * 128], start=True, stop=True),
                R=[qTz_r, KT_r], W=[PSx_r])

        def stB(i):
            qs, hh, kg, fh, lh, lq = items[i]
            gq = 4 * t + qs
            kt_lo = kg * 4
            kt_hi = min(kt_lo + 4, gq + 1)
            PSx, PSx_r = (PS0, PS1)[i % 2]
            Pbx, Pbx_r = Pb[i % 2]
            rsx, rsx_r = rs[hh % 4]
            nrs = 0 if fh else nrs_of[(qs, hh)]
            kt = kt_lo
            while kt < kt_hi:
                j = kt // 2
                c0 = (kt - kt_lo) * 128
                if kt == gq:
                    k.I("act", lambda e: e.activation(out=Pbx[:, c0:c0 + 128], in_=PSx[:, c0:c0 + 128], func=AF.Exp, scale=0.125),
                        R=[PSx_r], W=[Pbx_r])
                    k.I("dve", lambda e: e.scalar_tensor_tensor(out=Pbx[:, c0:c0 + 128], in0=Pbx[:, c0:c0 + 128], scalar=1.0,
                                                                in1=cmb(C, "tri01"), op0=ALU.mult, op1=ALU.mult,
                                                                accum_out=rsx[:, nrs:nrs + 1]),
                        R=[C.cmb_r], W=[Pbx_r, rsx_r])
                    nrs += 1
                    kt += 1
                else:
                    n = 2 if (kt % 2 == 0 and kt + 1 < kt_hi and kt + 1 != gq) else 1
                    k.I("act", lambda e: e.activation(out=Pbx[:, c0:c0 + n * 128], in_=PSx[:, c0:c0 + n * 128],
                                                      func=AF.Exp, scale=0.125, bias=biasq4[:, qs, hh, j:j + 1],
                                                      accum_out=rsx[:, nrs:nrs + 1]),
                        R=[PSx_r, biasq4_r[qs]], W=[Pbx_r, rsx_r])
                    nrs += 1
                    kt += n
            nrs_of[(qs, hh)] = nrs

        def stC(i):
            qs, hh, kg, fh, lh, lq = items[i]
            gq = 4 * t + qs
            kt_lo = kg * 4
            nk = min(kt_lo + 4, gq + 1) - kt_lo
            Pbx, Pbx_r = Pb[i % 2]
            ptv, ptv_r = PTv[i % 2]
            for ii in range(nk):
                k.I("pe", lambda e: e.transpose(ptv[:, ii * 128:(ii + 1) * 128],
                                                Pbx[:, ii * 128:(ii + 1) * 128], cmb(C, "ident")),
                    R=[Pbx_r, C.cmb_r], W=[ptv_r], inc=(ii == nk - 1))
            PTx, PTx_r = PTs[i % 2]
            k.I("dve", lambda e: e.tensor_copy(out=PTx[:, 0:nk * 128], in_=ptv[:, 0:nk * 128]),
                R=[ptv_r], W=[PTx_r])

        def stE(i):
            qs, hh, kg, fh, lh, lq = items[i]
            gq = 4 * t + qs
            kt_lo = kg * 4
            nk = min(kt_lo + 4, gq + 1) - kt_lo
            PTx, PTx_r = PTs[i % 2]
            PO, PO_r = (PO0, PO1)[qs % 2]
            for ii in range(nk):
                ktt = kt_lo + ii
                k.I("pe", lambda e: e.matmul(PO[:, hh * 64:(hh + 1) * 64], PTx[:, ii * 128:(ii + 1) * 128],
                                             V[:, ktt, hh * 64:(hh + 1) * 64], start=(ktt == 0), stop=(ktt == gq)),
                    R=[PTx_r, V_r], W=[PO_r], inc=(ii == nk - 1))
            if lh:
                rsx, rsx_r = rs[hh % 4]
                nrs = nrs_of[(qs, hh)]
                k.I("dve", lambda e: e.tensor_reduce(out=rinv[:, hh:hh + 1], in_=rsx[:, 0:nrs], axis=AX.X, op=ALU.add),
                    R=[rsx_r], W=[rinv_r])
            if lq:
                q0 = qs * 128
                k.I("dve", lambda e: e.reciprocal(out=rinv[:], in_=rinv[:]), R=[rinv_r], W=[rinv_r])
                k.I("dve", lambda e: e.tensor_tensor(out=otok[:].rearrange("p (h d) -> p h d", h=8),
                                                      in0=PO[:].rearrange("p (h d) -> p h d", h=8),
                                                      in1=rinv[:].rearrange("p (h o) -> p h o", o=1).to_broadcast([128, 8, 64]),
                                                      op=ALU.mult), R=[PO_r, rinv_r], W=[otok_r])
                for c in range(4):
                    k.I("pe", lambda e: e.transpose(PTt[:, c * 128:(c + 1) * 128], otok[:, c * 128:(c + 1) * 128], cmb(C, "ident")),
                        R=[otok_r, C.cmb_r], W=[PT_r], inc=(c == 3))
                k.I("act", lambda e: e.copy(out=catT[:, 0:4, q0:q0 + 128], in_=PTt[:, 0:512].rearrange("p (c q) -> p c q", c=4)),
                    R=[PT_r], W=[catT_r])

        nit = len(items)
        for i in range(nit + 2):
            if i < nit:
                stA(i)
                stB(i)
            if 0 <= i - 1 < nit:
                stC(i - 1)
            if 0 <= i - 2 < nit:
                stE(i - 2)
        for dc in range(8):
            pp, pp_r = (PA, PB)[dc % 2]
            for kc in range(8):
                k.I("pe", lambda e: e.matmul(pp[:], wout[:, kc, dc * 128:(dc + 1) * 128], catT[:, kc, :],
                                             start=(kc == 0), stop=(kc == 7)),
                    R=[wout_r, catT_r], W=[pp_r], inc=(kc == 7))
            k.I("dve", lambda e: e.tensor_tensor(out=h[:, dc, :], in0=pp[:], in1=h[:, dc, :], op=ALU.add),
                R=[pp_r, h_r], W=[h_r])
        for c in range(8):
            k.dma("sp", h1[c, :, t0:t0 + T], h[:, c, :], R=[h_r])


def g_xattn(k, nc, C, h_in, h_out, memT_d, wq_d, wkv_d, wo_d, layer, bf=None):
    (PA, PB, PS0, PS1, PO0, PO1, PM) = C.P
    L = str(layer)
    wq, wq_r = k.sb("xwq", [128, 8, 1024], BF16)
    wo, wo_r = k.sb("xwo", [128, 8, 1024], BF16)
    wkv, wkv_r = k.sb("xwkv", [128, 8, 2048], BF16)
    memf, memf_r = k.sb("memf", [128, 8, 256], F32)
    memn, memn_r = k.sb("memn", [128, 8, 256], BF16)
    KxT, KxT_r = k.sb("KxT", [128, 8, 256], BF16)
    Vx, Vx_r = k.sb("Vx", [128, 2, 1024], BF16)
    h, h_r = k.sb("xh", [128, 8, T], F32)
    hn, hn_r = k.sb("xhn", [128, 8, T], BF16)
    sq, _ = k.sb("xsq", [128, 2, T], BF16)
    sq_r = [Res(), Res()]
    rstd, rstd_r = k.sb("xrstd", [128, T], F32)
    qx, qx_r = k.sb("qx", [128, 8, T], BF16)
    oT, oT_r = k.sb("oT", [128, 8, T], BF16)
    PTb_ = [k.sb(f"xPT{i}", [128, T], BF16) for i in range(2)]
    rden, rden_r = k.sb("rden", [128, T], F32)
    r2, r2_r = k.sb("xr2", [128, T], F32)

    if bf is None:
        load_w_bf16(k, wkv, wkv_r, wkv_d, 8, 2048)
        load_w_bf16(k, wq, wq_r, wq_d, 8, 1024)
        load_w_bf16(k, wo, wo_r, wo_d, 8, 1024)
    else:
        load_bf(k, wkv, wkv_r, bf["wkv"], 0, 2048)
        load_bf(k, wq, wq_r, bf["wq"], 0, 1024)
        load_bf(k, wo, wo_r, bf["wo"], 0, 1024)
    for c in range(8):
        k.dma("sp", memf[:, c, :], memT_d[c, :, :], W=[memf_r])
    rmsnorm_T(k, C, memf, memf_r, "mem" + L, memn, memn_r, sq, sq_r, rstd, rstd_r, PM, n=256)
    for hh in range(4):
        for cc in range(2):
            pp, pp_r = (PA, PB)[cc]
            fc = 2 * hh + cc
            for kc in range(8):
                k.I("pe", lambda e: e.matmul(pp[:, 0:256], wkv[:, kc, fc * 128:(fc + 1) * 128], memn[:, kc, :],
                                             start=(kc == 0), stop=(kc == 7)), R=[wkv_r, memn_r], W=[pp_r], inc=(kc == 7))
            k.I("act", lambda e: e.activation(out=sq[:, cc, 0:256], in_=pp[:, 0:256], func=AF.Square), R=[pp_r], W=[sq_r[cc]])
            k.I("pe", lambda e: e.matmul(PM[0][:, 0:256], cmb(C, "ones"), sq[:, cc, 0:256], start=(cc == 0), stop=(cc == 1)),
                R=[sq_r[cc], C.cmb_r], W=[PM[1]])
        k.I("act", lambda e: e.activation(out=r2[:, 0:256], in_=PM[0][:, 0:256], func=AF.Sqrt, bias=EPS, scale=1.0 / 256),
            R=[PM[1]], W=[r2_r])
        k.I("dve", lambda e: e.reciprocal(out=r2[:, 0:256], in_=r2[:, 0:256]), R=[r2_r], W=[r2_r])
        for cc in range(2):
            pp, pp_r = (PA, PB)[cc]
            k.I("dve", lambda e: e.scalar_tensor_tensor(out=KxT[:, 2 * hh + cc, :], in0=pp[:, 0:256], scalar=col(C, "xak" + L, cc),
                                                        in1=r2[:, 0:256], op0=ALU.mult, op1=ALU.mult),
                R=[pp_r, r2_r, C.cst_r], W=[KxT_r])
    for mt in range(2):
        for half in range(2):
            pp, pp_r = (PA, PB)[half]
            for kc in range(8):
                k.I("pe", lambda e: e.matmul(pp[:], memn[:, kc, mt * 128:(mt + 1) * 128],
                                             wkv[:, kc, 1024 + half * 512:1024 + (half + 1) * 512],
                                             start=(kc == 0), stop=(kc == 7)), R=[wkv_r, memn_r], W=[pp_r], inc=(kc == 7))
            k.I("act", lambda e: e.copy(out=Vx[:, mt, half * 512:(half + 1) * 512], in_=pp[:]), R=[pp_r], W=[Vx_r])

    hB = [(h, h_r), k.sb("xh2", [128, 8, T], F32)]
    hnB = [(hn, hn_r), k.sb("xhn2", [128, 8, T], BF16)]
    oTB = [(oT, oT_r), k.sb("oT2", [128, 8, T], BF16)]
    sqh = [[k.sb(f"xsqh{i}{c}", [128, T], BF16) for c in range(2)] for i in range(2)]
    r2B = [(r2, r2_r), k.sb("xr2b", [128, T], F32)]
    rdB = [(rden, rden_r), k.sb("rden2", [128, T], F32)]
    PTB = [PTb_, [k.sb(f"xPTb{i}", [128, T], BF16) for i in range(2)]]
    PD = (C.PT[0][:, :].bitcast(F32), C.PT[1])
    N = NT * 4

    def S0(t):
        h_, h_r_ = hB[t % 2]
        hn_, hn_r_ = hnB[t % 2]
        for c in range(8):
            k.dma("sp", h_[:, c, :], h_in[c, :, t * T:(t + 1) * T], W=[h_r_])
        rmsnorm_T(k, C, h_, h_r_, "xa" + L, hn_, hn_r_, sq, sq_r, rstd, rstd_r, PM)

    def S1(i):
        t, hh = divmod(i, 4)
        hn_, hn_r_ = hnB[t % 2]
        for cc in range(2):
            pp, pp_r = (PA, PB)[cc]
            fc = 2 * hh + cc
            s_, s_r = sqh[i % 2][cc]
            for kc in range(8):
                k.I("pe", lambda e: e.matmul(pp[:], wq[:, kc, fc * 128:(fc + 1) * 128], hn_[:, kc, :],
                                             start=(kc == 0), stop=(kc == 7)), R=[wq_r, hn_r_], W=[pp_r], inc=(kc == 7))
            k.I("act", lambda e: e.activation(out=s_[:], in_=pp[:], func=AF.Square), R=[pp_r], W=[s_r])
            k.I("pe", lambda e: e.matmul(PM[0][:], cmb(C, "ones"), s_[:], start=(cc == 0), stop=(cc == 1)),
                R=[s_r, C.cmb_r], W=[PM[1]])

    def S2(i):
        t, hh = divmod(i, 4)
        r_, r_r = r2B[i % 2]
        k.I("act", lambda e: e.activation(out=r_[:], in_=PM[0][:], func=AF.Sqrt, bias=EPS, scale=1.0 / 256),
            R=[PM[1]], W=[r_r])
        k.I("dve", lambda e: e.reciprocal(out=r_[:], in_=r_[:]), R=[r_r], W=[r_r])
        for cc in range(2):
            pp, pp_r = (PA, PB)[cc]
            k.I("dve", lambda e: e.scalar_tensor_tensor(out=qx[:, 2 * hh + cc, :], in0=pp[:], scalar=col(C, "xaq" + L, cc),
                                                        in1=r_[:], op0=ALU.mult, op1=ALU.mult),
                R=[pp_r, r_r, C.cst_r], W=[qx_r])

    def S3(i):
        t, hh = divmod(i, 4)
        for mt in range(2):
            ps_, ps_r = (PS0, PS1)[mt]
            pb, pb_r = PTB[i % 2][mt]
            for cc in range(2):
                k.I("pe", lambda e: e.matmul(ps_[:], KxT[:, 2 * hh + cc, mt * 128:(mt + 1) * 128], qx[:, 2 * hh + cc, :],
                                             start=(cc == 0), stop=(cc == 1)), R=[KxT_r, qx_r], W=[ps_r], inc=(cc == 1))
            k.I("act", lambda e: e.activation(out=pb[:], in_=ps_[:], func=AF.Exp, scale=1.0 / 16), R=[ps_r], W=[pb_r])

    def S4(i):
        t, hh = divmod(i, 4)
        oT_, oT_r_ = oTB[t % 2]
        rd, rd_r = rdB[i % 2]
        for mt in range(2):
            pb, pb_r = PTB[i % 2][mt]
            k.I("pe", lambda e: e.matmul(PD[0][:, 0:512], cmb(C, "ones"), pb[:], start=(mt == 0), stop=(mt == 1)),
                R=[pb_r, C.cmb_r], W=[PD[1]], inc=(mt == 1))
        k.I("dve", lambda e: e.reciprocal(out=rd[:], in_=PD[0][:, 0:512]), R=[PD[1]], W=[rd_r])
        for cc in range(2):
            po, po_r = (PO0, PO1)[cc]
            for mt in range(2):
                pb, pb_r = PTB[i % 2][mt]
                k.I("pe", lambda e: e.matmul(po[:], Vx[:, mt, (2 * hh + cc) * 128:(2 * hh + cc + 1) * 128], pb[:],
                                             start=(mt == 0), stop=(mt == 1)), R=[Vx_r, pb_r], W=[po_r], inc=(mt == 1))
            k.I("dve", lambda e: e.tensor_tensor(out=oT_[:, 2 * hh + cc, :], in0=po[:], in1=rd[:], op=ALU.mult),
                R=[po_r, rd_r], W=[oT_r_])
        if hh == 3:
            h_, h_r_ = hB[t % 2]
            for dc in range(8):
                pp, pp_r = (PO0, PO1)[dc % 2]
                for kc in range(8):
                    k.I("pe", lambda e: e.matmul(pp[:], wo[:, kc, dc * 128:(dc + 1) * 128], oT_[:, kc, :],
                                                 start=(kc == 0), stop=(kc == 7)), R=[wo_r, oT_r_], W=[pp_r], inc=(kc == 7))
                k.I("dve", lambda e: e.tensor_tensor(out=h_[:, dc, :], in0=pp[:], in1=h_[:, dc, :], op=ALU.add),
                    R=[pp_r, h_r_], W=[h_r_])
            for c in range(8):
                k.dma("sp", h_out[c, :, t * T:(t + 1) * T], h_[:, c, :], R=[h_r_])

    for i in range(N + 2):
        if i < N:
            if i % 4 == 0:
                S0(i // 4)
            S1(i)
            S2(i)
        if 0 <= i - 1 < N:
            S3(i - 1)
        if 0 <= i - 2 < N:
            S4(i - 2)


def g_ffn(k, nc, C, h_in, h_out, wg_d, wu_d, wd_d, bf=None):
    (PA, PB, PS0, PS1, PO0, PO1, PM) = C.P
    NF = 22
    wg, wg_r = k.sb("fwg", [128, 8, 2816], BF16)
    wu, wu_r = k.sb("fwu", [128, 8, 2816], BF16)
    wd, wd_r = k.sb("fwd", [128, NF, 1024], BF16)
    h, h_r = k.sb("fh", [128, 8, T], F32)
    hn, hn_r = k.sb("fhn", [128, 8, T], BF16)
    sq, _ = k.sb("fsq", [128, 2, T], BF16)
    sq_r = [Res(), Res()]
    rstd, rstd_r = k.sb("frstd", [128, T], F32)
    act, act_r = k.sb("fact", [128, NF, T], BF16)
    sg = [k.sb(f"fsg{i}", [128, T], BF16) for i in range(2)]
    if bf is None:
        load_w_bf16(k, wg, wg_r, wg_d, 8, 2816)
        load_w_bf16(k, wu, wu_r, wu_d, 8, 2816)
        load_w_bf16(k, wd, wd_r, wd_d, NF, 1024)
    else:
        for c0 in range(0, 2816, 704):
            k.dma("sp", wg[:, :, c0:c0 + 704], bf["wg"][0].rearrange("(c p) f -> p c f", p=128)[:, :, c0:c0 + 704],
                  R=[bf["wg"][1]], W=[wg_r])
            k.dma("sp", wu[:, :, c0:c0 + 704], bf["wu"][0].rearrange("(c p) f -> p c f", p=128)[:, :, c0:c0 + 704],
                  R=[bf["wu"][1]], W=[wu_r])
        for f0 in range(0, NF, 11):
            k.dma("sp", wd[:, f0:f0 + 11, :], bf["wd"][0].rearrange("(c p) f -> p c f", p=128)[:, f0:f0 + 11, :],
                  R=[bf["wd"][1]], W=[wd_r])
    hB = [(h, h_r), (h, h_r)]
    hnB = [(hn, hn_r), k.sb("fhn2", [128, 8, T], BF16)]
    rst = [k.sb(f"frst{i}", [128, T], F32) for i in range(2)]

    def prologue(t):
        h_, h_r_ = hB[t % 2]
        hn_, hn_r_ = hnB[t % 2]
        for c in range(8):
            k.dma("sp", h_[:, c, :], h_in[c, :, t * T:(t + 1) * T], W=[h_r_])
        rmsnorm_T(k, C, h_, h_r_, "ffn0", hn_, hn_r_, sq, sq_r, rstd, rstd_r, PM)

    prologue(0)
    for t in range(NT):
        t0 = t * T
        h_, h_r_ = hB[t % 2]
        hn_, hn_r_ = hnB[t % 2]
        for fc in range(NF):
            (pg, pg_r), (pu, pu_r) = ((PA, PB), (PS0, PS1))[fc % 2]
            for kc in range(8):
                k.I("pe", lambda e: e.matmul(pg[:], wg[:, kc, fc * 128:(fc + 1) * 128], hn_[:, kc, :],
                                             start=(kc == 0), stop=(kc == 7)), R=[wg_r, hn_r_], W=[pg_r], inc=(kc == 7))
            for kc in range(8):
                k.I("pe", lambda e: e.matmul(pu[:], wu[:, kc, fc * 128:(fc + 1) * 128], hn_[:, kc, :],
                                             start=(kc == 0), stop=(kc == 7)), R=[wu_r, hn_r_], W=[pu_r], inc=(kc == 7))
            s_, s_r = sg[fc % 2]
            k.I("act", lambda e: e.activation(out=s_[:], in_=pg[:], func=AF.Silu), R=[pg_r], W=[s_r])
            k.I("dve", lambda e: e.tensor_tensor(out=act[:, fc, :], in0=pu[:], in1=s_[:], op=ALU.mult),
                R=[pu_r, s_r], W=[act_r])
            if fc == NF // 2 and t + 1 < NT:
                prologue(t + 1)
        for dc in range(8):
            pp, pp_r = (PO0, PO1)[dc % 2]
            for fc in range(NF):
                k.I("pe", lambda e: e.matmul(pp[:], wd[:, fc, dc * 128:(dc + 1) * 128], act[:, fc, :],
                                             start=(fc == 0), stop=(fc == NF - 1)), R=[wd_r, act_r], W=[pp_r], inc=(fc == NF - 1))
            r_, r_r = rst[dc % 2]
            k.dma("sp", r_[:], h_in[dc, :, t0:t0 + T], W=[r_r])
            k.I("dve", lambda e: e.tensor_tensor(out=r_[:], in0=pp[:], in1=r_[:], op=ALU.add),
                R=[pp_r, r_r], W=[r_r])
            k.dma("sp", h_out[dc, :, t0:t0 + T], r_[:], R=[r_r])


def g_moe(k, nc, C, h_in, h_out, wr_d, wg_d, wu_d, wd_d, n_exp=8):
    (PA, PB, PS0, PS1, PO0, PO1, PM) = C.P
    SUP = min(2048, NT * T)
    NSUP = (NT * T) // SUP
    NSB = SUP // 128
    NTT = SUP // T
    hs, hs_r = k.sb("mhs", [128, 8, SUP], F32)
    hn, hn_r = k.sb("mhn", [128, 8, SUP], BF16)
    gbc, gbc_r = k.sb("mgbc", [128, 8, SUP], BF16)
    wgs = [k.sb(f"mwg{i}", [128, 8, 512], BF16) for i in range(2)]
    wus = [k.sb(f"mwu{i}", [128, 8, 512], BF16) for i in range(2)]
    wds = [k.sb(f"mwd{i}", [128, 4, 1024], BF16) for i in range(2)]
    act = [k.sb(f"mact{i}", [128, 4, T], BF16) for i in range(2)]
    sg = [k.sb(f"msg{i}", [128, T], BF16) for i in range(2)]
    sg2 = [k.sb(f"msg2{i}", [128, T], BF16) for i in range(2)]
    sq, _ = k.sb("msq", [128, 2, T], BF16)
    sq_r = [Res(), Res()]
    rstd, rstd_r = k.sb("mrstd", [128, T], F32)
    wr, wr_r = k.sb("mwr", [128, 8, 8], BF16)
    Lg, Lg_r = k.sb("mL", [128, NSB, 8], F32)
    Eg, Eg_r = k.sb("mE", [128, NSB, 8], F32)
    Sg, Sg_r = k.sb("mS", [128, NSB, 8], F32)
    m8, m8_r = k.sb("mm8", [128, NSB, 8], F32)
    rd, rd_r = k.sb("mrd", [128, NSB], F32)
    Gb, Gb_r = k.sb("mGb", [128, NSB, 8], BF16)
    Gbig, Gbig_r = k.sb("mGbig", [128, 128], BF16)
    for kc in range(8):
        k.dma("pool", wr[:, kc, :], wr_d[kc * 128:(kc + 1) * 128, :], W=[wr_r])
    wcount = 0
    for sp_ in range(NSUP):
        s0 = sp_ * SUP
        for c in range(8):
            k.dma("sp", hs[:, c, :], h_in[c, :, s0:s0 + SUP], W=[hs_r])
        for tt in range(NTT):
            hv = hs[:, :, tt * T:(tt + 1) * T]
            hnv = hn[:, :, tt * T:(tt + 1) * T]
            rmsnorm_T(k, C, hv, hs_r, "ffn1", hnv, hn_r, sq, sq_r, rstd, rstd_r, PM)
        for sb_ in range(NSB):
            for kc in range(8):
                k.I("pe", lambda e: e.matmul(PM[0][:, sb_ * 8:(sb_ + 1) * 8], hn[:, kc, sb_ * 128:(sb_ + 1) * 128], wr[:, kc, :],
                                             start=(kc == 0), stop=(kc == 7)), R=[hn_r, wr_r], W=[PM[1]],
                    inc=(kc == 7 and sb_ == NSB - 1))
        k.I("dve", lambda e: e.tensor_copy(out=Lg[:], in_=PM[0][:, 0:NSB * 8].rearrange("p (s e) -> p s e", e=8)),
            R=[PM[1]], W=[Lg_r])
        for sb_ in range(NSB):
            k.I("dve", lambda e: e.max(out=m8[:, sb_, :], in_=Lg[:, sb_, :]), R=[Lg_r], W=[m8_r])
        k.I("dve", lambda e: e.tensor_tensor(out=Eg[:], in0=Lg[:], in1=m8[:, :, 0:1].to_broadcast([128, NSB, 8]), op=ALU.subtract),
            R=[Lg_r, m8_r], W=[Eg_r])
        k.I("act", lambda e: e.activation(out=Eg[:], in_=Eg[:], func=AF.Exp), R=[Eg_r], W=[Eg_r])
        k.I("dve", lambda e: e.tensor_tensor(out=Sg[:], in0=Lg[:], in1=m8[:, :, 1:2].to_broadcast([128, NSB, 8]), op=ALU.is_ge),
            R=[Lg_r, m8_r], W=[Sg_r])
        k.I("dve", lambda e: e.tensor_tensor(out=Eg[:], in0=Eg[:], in1=Sg[:], op=ALU.mult), R=[Eg_r, Sg_r], W=[Eg_r])
        k.I("dve", lambda e: e.tensor_tensor(out=rd[:], in0=m8[:, :, 1], in1=m8[:, :, 0], op=ALU.subtract), R=[m8_r], W=[rd_r])
        k.I("act", lambda e: e.activation(out=rd[:], in_=rd[:], func=AF.Exp), R=[rd_r], W=[rd_r])
        k.I("dve", lambda e: e.tensor_scalar(out=rd[:], in0=rd[:], scalar1=1.0, scalar2=None, op0=ALU.add), R=[rd_r], W=[rd_r])
        k.I("dve", lambda e: e.reciprocal(out=rd[:], in_=rd[:]), R=[rd_r], W=[rd_r])
        k.I("dve", lambda e: e.tensor_tensor(out=Gb[:], in0=Eg[:], in1=rd[:].rearrange("p (s o) -> p s o", o=1).to_broadcast([128, NSB, 8]),
                                              op=ALU.mult), R=[Eg_r, rd_r], W=[Gb_r])
        for ex in range(n_exp):
            for sb_ in range(NSB):
                pp, pp_r = (PA, PB)[(sb_ // 4) % 2]
                k.I("pe", lambda e: e.matmul(pp[:, (sb_ % 4) * 128:(sb_ % 4 + 1) * 128],
                                             Gb[:, sb_, ex:ex + 1].to_broadcast([128, 128]), cmb(C, "ident"), start=True, stop=True),
                    R=[Gb_r, C.cmb_r], W=[pp_r], inc=(sb_ % 4 == 3))
                if sb_ % 4 == 3:
                    k.I("act", lambda e: e.copy(out=gbc[:, ex, (sb_ - 3) * 128:(sb_ + 1) * 128], in_=pp[:]), R=[pp_r], W=[gbc_r])
        pend = None

        def gu(ex, wset, tt, icnt):
            (wg_, wg_r), (wu_, wu_r), (wd_, wd_r) = wset
            a_, a_r = act[icnt % 2]
            tsl = slice(tt * T, (tt + 1) * T)
            for fc in range(4):
                (pg, pg_r), (pu, pu_r) = ((PA, PB), (PS0, PS1))[fc % 2]
                for kc in range(8):
                    k.I("pe", lambda e: e.matmul(pg[:], wg_[:, kc, fc * 128:(fc + 1) * 128], hn[:, kc, tsl],
                                                 start=(kc == 0), stop=(kc == 7)), R=[wg_r, hn_r], W=[pg_r], inc=(kc == 7))
                for kc in range(8):
                    k.I("pe", lambda e: e.matmul(pu[:], wu_[:, kc, fc * 128:(fc + 1) * 128], hn[:, kc, tsl],
                                                 start=(kc == 0), stop=(kc == 7)), R=[wu_r, hn_r], W=[pu_r], inc=(kc == 7))
                s_, s_r = sg[fc % 2]
                s2, s2_r = sg2[fc % 2]
                k.I("act", lambda e: e.activation(out=s_[:], in_=pg[:], func=AF.Silu), R=[pg_r], W=[s_r])
                k.I("dve", lambda e: e.tensor_tensor(out=s2[:], in0=s_[:], in1=gbc[:, ex, tsl], op=ALU.mult),
                    R=[s_r, gbc_r], W=[s2_r])
                k.I("dve", lambda e: e.tensor_tensor(out=a_[:, fc, :], in0=pu[:], in1=s2[:], op=ALU.mult),
                    R=[pu_r, s2_r], W=[a_r])

        def down(wset, tt, icnt):
            (wd_, wd_r) = wset[2]
            a_, a_r = act[icnt % 2]
            tsl = slice(tt * T, (tt + 1) * T)
            for dc in range(8):
                pp, pp_r = (PO0, PO1)[dc % 2]
                for fc in range(4):
                    k.I("pe", lambda e: e.matmul(pp[:], wd_[:, fc, dc * 128:(dc + 1) * 128], a_[:, fc, :],
                                                 start=(fc == 0), stop=(fc == 3)), R=[wd_r, a_r], W=[pp_r], inc=(fc == 3))
                k.I("dve", lambda e: e.tensor_tensor(out=hs[:, dc, tsl], in0=pp[:], in1=hs[:, dc, tsl], op=ALU.add),
                    R=[pp_r, hs_r], W=[hs_r])

        icnt = 0
        for ex in range(n_exp):
            for fg in range(7):
                b = wcount % 2
                wcount += 1
                wset = (wgs[b], wus[b], wds[b])
                (wg_, wg_r), (wu_, wu_r), (wd_, wd_r) = wset
                f0 = fg * 512
                for kc in range(8):
                    k.dma("pool", wg_[:, kc, :], wg_d[ex, kc * 128:(kc + 1) * 128, f0:f0 + 512], W=[wg_r])
                    k.dma("pool", wu_[:, kc, :], wu_d[ex, kc * 128:(kc + 1) * 128, f0:f0 + 512], W=[wu_r])
                for fc in range(4):
                    k.dma("pool", wd_[:, fc, :], wd_d[ex, f0 + fc * 128:f0 + (fc + 1) * 128, :], W=[wd_r])
                for tt in range(NTT):
                    gu(ex, wset, tt, icnt)
                    if pend is not None:
                        down(*pend)
                    pend = (wset, tt, icnt)
                    icnt += 1
        down(*pend)
        pend = None
        for c in range(8):
            k.dma("sp", h_out[c, :, s0:s0 + SUP], hs[:, c, :], R=[hs_r])


def cmf(C, name):
    o = CM[name]
    return C.cmf[:, o:o + 128]


def g_ssd(k, nc, C, h_in, h_out, rowc_d, w_in_d, w_out_d, bf=None):
    (PA, PB, PS0, PS1, PY, PZ, PM) = C.P
    PTt, PT_r = C.PT
    wout, wout_r = k.sb("swout", [128, 16, 1024], BF16)
    wdt, wdt_r = k.sb("swdt", [128, 8, 32], BF16)
    NWB = 2 if bf is None else 3
    wblk = [k.sb(f"swb{i}", [128, 8, 512], BF16) for i in range(NWB)]
    rowc, rowc_r = k.sb("rowc", [128, 96], F32)
    abc, abc_r = k.sb("sabc", [128, 32], F32)
    h, h_r = k.sb("sh", [128, 8, T], F32)
    hn, hn_r = k.sb("shn", [128, 8, T], BF16)
    sq, _ = k.sb("ssq", [128, 2, T], BF16)
    sq_r = [Res(), Res()]
    rstd, rstd_r = k.sb("srstd", [128, T], F32)
    xr = [k.sb(f"sxr{i}", [128, 515], F32) for i in range(2)]
    acc = [k.sb(f"sacc{i}", [128, T], F32) for i in range(2)]
    halo, halo_r = k.sb("shalo", [128, 24, 3], F32)
    xbcT, xbcT_r = k.sb("sxbcT", [128, 24, T], BF16)
    zs, zs_r = k.sb("szs", [128, 4, 2048], BF16)
    dt, dt_r = k.sb("sdt", [128, 4, 32], F32)
    da, da_r = k.sb("sda", [128, 4, 32], F32)
    xtok, xtok_r = k.sb("sxtok", [128, 2048], BF16)
    btok, btok_r = k.sb("sbtok", [128, 512], BF16)
    acum, acum_r = k.sb("sacum", [128, 64], F32)
    ea, ea_r = k.sb("sea", [128, 32], F32)
    nacum, nacum_r = k.sb("snacum", [128, 32], F32)
    te, te_r = k.sb("ste", [128, 32], F32)
    cd, cd_r = k.sb("scd", [128, 32], F32)
    seg = [k.sb(f"sseg{i}", [128, 512], F32) for i in range(2)]
    mt_ = [k.sb(f"smt{i}", [128, 512], BF16) for i in range(2)]
    cbm, cbm_r = k.sb("scbm", [128, 4, 128], F32)
    xdt, xdt_r = k.sb("sxdt", [128, 2048], BF16)
    xw, xw_r = k.sb("sxw", [128, 2048], BF16)
    state, state_r = k.sb("sstate", [128, 2048], F32)
    stb, stb_r = k.sb("sstb", [128, 2048], BF16)
    tmp = [k.sb(f"stmp{i}", [128, 512], F32) for i in range(2)]
    tmp2 = [k.sb(f"stmq{i}", [128, 512], F32) for i in range(2)]
    yz, yz_r = k.sb("syz", [128, 512], F32)
    junk, junk_r = k.sb("sjunk", [128, 512], BF16)
    ssq, ssq_r = k.sb("sssq", [128, 4], F32)
    yn, yn_r = k.sb("syn", [128, 2048], BF16)
    ynT, ynT_r = k.sb("synT", [128, 16, T], BF16)

    if bf is None:
        load_w_bf16(k, wout, wout_r, w_out_d, 16, 1024)
    else:
        load_bf(k, wout, wout_r, bf["wout"], 0, 1024)
    for kc in range(8):
        k.dma("pool", wdt[:, kc, :], w_in_d[kc * 128:(kc + 1) * 128, 5120:5152], W=[wdt_r])
    k.dma("sp", rowc[:], rowc_d[:, :], W=[rowc_r])
    k.I("act", lambda e: e.activation(out=abc[:], in_=rowc[:, 32:64], func=AF.Exp), R=[rowc_r], W=[abc_r])
    k.I("dve", lambda e: e.tensor_scalar(out=abc[:], in0=abc[:], scalar1=-1.0, scalar2=None, op0=ALU.mult), R=[abc_r], W=[abc_r])
    for kc in range(16):
        k.I("dve", lambda e: e.tensor_scalar(out=wout[:, kc, :], in0=wout[:, kc, :], scalar1=col(C, "ssdn", kc), scalar2=None,
                                              op0=ALU.mult), R=[wout_r, C.cst_r], W=[wout_r])
    k.I("pool", lambda e: e.memset(halo[:], 0.0), W=[halo_r])
    k.I("pool", lambda e: e.memset(state[:], 0.0), W=[state_r])
    k.I("pool", lambda e: e.memset(stb[:], 0.0), W=[stb_r])
    wc = 0
    hcount = 0
    for t in range(NT):
        t0 = t * T
        for c in range(8):
            k.dma("sp", h[:, c, :], h_in[c, :, t0:t0 + T], W=[h_r])
        rmsnorm_T(k, C, h, h_r, "mix1", hn, hn_r, sq, sq_r, rstd, rstd_r, PM)
        for zb in range(4):
            wb, wb_r = wblk[wc % NWB]
            wc += 1
            if bf is None:
                for kc in range(8):
                    k.dma("pool", wb[:, kc, :], w_in_d[kc * 128:(kc + 1) * 128, zb * 512:(zb + 1) * 512], W=[wb_r])
            else:
                load_bf(k, wb, wb_r, bf["win"], zb * 512, (zb + 1) * 512)
            for qs in range(4):
                pp, pp_r = (PA, PB)[qs % 2]
                for kc in range(8):
                    k.I("pe", lambda e: e.matmul(pp[:], hn[:, kc, qs * 128:(qs + 1) * 128], wb[:, kc, :],
                                                 start=(kc == 0), stop=(kc == 7)), R=[hn_r, wb_r], W=[pp_r], inc=(kc == 7))
                k.I("act", lambda e: e.activation(out=zs[:, qs, zb * 512:(zb + 1) * 512], in_=pp[:], func=AF.Silu),
                    R=[pp_r], W=[zs_r])
        for xb in range(6):
            wb, wb_r = wblk[wc % NWB]
            wc += 1
            if bf is None:
                for kc in range(8):
                    k.dma("pool", wb[:, kc, :], w_in_d[kc * 128:(kc + 1) * 128, 2048 + xb * 512:2048 + (xb + 1) * 512], W=[wb_r])
            else:
                load_bf(k, wb, wb_r, bf["win"], 2048 + xb * 512, 2048 + (xb + 1) * 512)
            for c4 in range(4):
                ch = xb * 4 + c4
                pp, pp_r = (PA, PB)[c4 % 2]
                x_, x_r = xr[c4 % 2]
                a_, a_r = acc[c4 % 2]
                for kc in range(8):
                    k.I("pe", lambda e: e.matmul(pp[:], wb[:, kc, c4 * 128:(c4 + 1) * 128], hn[:, kc, :],
                                                 start=(kc == 0), stop=(kc == 7)), R=[hn_r, wb_r], W=[pp_r], inc=(kc == 7))
                k.I("act", lambda e: e.copy(out=x_[:, 3:515], in_=pp[:]), R=[pp_r], W=[x_r])
                k.I("act", lambda e: e.copy(out=x_[:, 0:3], in_=halo[:, ch, :]), R=[halo_r], W=[x_r])
                k.I("act", lambda e: e.copy(out=halo[:, ch, :], in_=x_[:, 512:515]), R=[x_r], W=[halo_r])
                cw = CST["convw"] + 4 * ch
                k.I("dve", lambda e: e.tensor_scalar(out=a_[:], in0=x_[:, 0:512], scalar1=C.cst[:, cw:cw + 1], scalar2=None, op0=ALU.mult),
                    R=[x_r, C.cst_r], W=[a_r])
                for i in range(1, 4):
                    k.I("dve", lambda e: e.scalar_tensor_tensor(out=a_[:], in0=x_[:, i:i + 512], scalar=C.cst[:, cw + i:cw + i + 1],
                                                                in1=a_[:], op0=ALU.mult, op1=ALU.add),
                        R=[x_r, C.cst_r], W=[a_r])
                k.I("act", lambda e: e.activation(out=xbcT[:, ch, :], in_=a_[:], func=AF.Silu, bias=col(C, "convb", ch)),
                    R=[a_r, C.cst_r], W=[xbcT_r])
        for qs in range(4):
            for kc in range(8):
                k.I("pe", lambda e: e.matmul(PM[0][:, qs * 32:(qs + 1) * 32], hn[:, kc, qs * 128:(qs + 1) * 128], wdt[:, kc, :],
                                             start=(kc == 0), stop=(kc == 7)), R=[hn_r, wdt_r], W=[PM[1]],
                    inc=(kc == 7 and qs == 3))
        k.I("dve", lambda e: e.tensor_tensor(out=dt[:], in0=PM[0][:, 0:128].rearrange("p (q h) -> p q h", q=4),
                                              in1=rowc[:, 0:32].rearrange("p (o h) -> p o h", o=1).to_broadcast([128, 4, 32]),
                                              op=ALU.add), R=[PM[1], rowc_r], W=[dt_r])
        k.I("act", lambda e: e.activation(out=dt[:], in_=dt[:], func=AF.Exp), R=[dt_r], W=[dt_r])
        k.I("act", lambda e: e.activation(out=dt[:], in_=dt[:], func=AF.Ln, bias=1.0), R=[dt_r], W=[dt_r])
        k.I("dve", lambda e: e.tensor_tensor(out=da[:], in0=dt[:],
                                              in1=abc[:].rearrange("p (o h) -> p o h", o=1).to_broadcast([128, 4, 32]), op=ALU.mult),
            R=[dt_r, abc_r], W=[da_r])
        def ch_pre(qs):
            cs = slice(qs * 128, (qs + 1) * 128)
            first = (t == 0 and qs == 0)
            for half in range(2):
                for j in range(8):
                    k.I("pe", lambda e: e.transpose(PTt[:, j * 128:(j + 1) * 128], xbcT[:, half * 8 + j, cs], cmb(C, "ident")),
                        R=[xbcT_r, C.cmb_r], W=[PT_r], inc=(j == 7))
                k.I("act", lambda e: e.copy(out=xtok[:, half * 1024:(half + 1) * 1024], in_=PTt[:]), R=[PT_r], W=[xtok_r])
            for j in range(4):
                k.I("pe", lambda e: e.transpose(PTt[:, j * 128:(j + 1) * 128], xbcT[:, 16 + j, cs], cmb(C, "ident")),
                    R=[xbcT_r, C.cmb_r], W=[PT_r], inc=(j == 3))
            k.I("act", lambda e: e.copy(out=btok[:], in_=PTt[:, 0:512]), R=[PT_r], W=[btok_r])
            k.I("pe", lambda e: e.matmul(PM[0][:, 0:32], cmf(C, "upper"), da[:, qs, :], start=True, stop=True),
                R=[da_r, C.cmf_r], W=[PM[1]])
            k.I("pe", lambda e: e.matmul(PM[0][:, 32:64], cmf(C, "ones"), da[:, qs, :], start=True, stop=True),
                R=[da_r, C.cmf_r], W=[PM[1]])
            k.I("act", lambda e: e.copy(out=acum[:], in_=PM[0][:, 0:64]), R=[PM[1]], W=[acum_r])
            k.I("act", lambda e: e.activation(out=ea[:], in_=acum[:, 0:32], func=AF.Exp), R=[acum_r], W=[ea_r])
            k.I("act", lambda e: e.mul(out=nacum[:], in_=acum[:, 0:32], mul=-1.0), R=[acum_r], W=[nacum_r])
            k.I("act", lambda e: e.activation(out=cd[:], in_=acum[:, 32:64], func=AF.Exp), R=[acum_r], W=[cd_r])
            k.I("dve", lambda e: e.tensor_tensor(out=te[:], in0=acum[:, 32:64], in1=acum[:, 0:32], op=ALU.subtract),
                R=[acum_r], W=[te_r])
            k.I("act", lambda e: e.activation(out=te[:], in_=te[:], func=AF.Exp), R=[te_r], W=[te_r])
            for g in range(4):
                pp, pp_r = (PA, PB)[g % 2]
                k.I("pe", lambda e: e.matmul(pp[:, 0:128], xbcT[:, 16 + g, cs], xbcT[:, 20 + g, cs], start=True, stop=True),
                    R=[xbcT_r], W=[pp_r])
                k.I("dve", lambda e: e.tensor_tensor(out=cbm[:, g, :], in0=pp[:, 0:128], in1=cmf(C, "upper"), op=ALU.mult),
                    R=[pp_r, C.cmf_r], W=[cbm_r])
            k.I("dve", lambda e: e.tensor_tensor(out=xdt[:].rearrange("p (h d) -> p h d", h=32),
                                                  in0=xtok[:].rearrange("p (h d) -> p h d", h=32),
                                                  in1=dt[:, qs, :].rearrange("p (h o) -> p h o", o=1).to_broadcast([128, 32, 64]),
                                                  op=ALU.mult), R=[xtok_r, dt_r], W=[xdt_r])
            k.I("dve", lambda e: e.tensor_tensor(out=xw[:].rearrange("p (h d) -> p h d", h=32),
                                                  in0=xdt[:].rearrange("p (h d) -> p h d", h=32),
                                                  in1=te[:].rearrange("p (h o) -> p h o", o=1).to_broadcast([128, 32, 64]),
                                                  op=ALU.mult), R=[xdt_r, te_r], W=[xw_r])
        def ch_heads(qs):
            cs = slice(qs * 128, (qs + 1) * 128)
            PYs = [PY, PA]
            PZs = [PZ, PB]

            def s1(q):
                ps_, ps_r = (PS0, PS1)[q % 2]
                for i in range(4):
                    hh = 4 * q + i
                    k.I("pe", lambda e: e.matmul(ps_[:, i * 128:(i + 1) * 128], da[:, qs, hh:hh + 1].to_broadcast([128, 128]),
                                                 cmf(C, "upper"), start=True, stop=True), R=[da_r, C.cmf_r], W=[ps_r], inc=(i == 3))

            def s2(q):
                sg_, sg_r = seg[q % 2]
                ps_, ps_r = (PS0, PS1)[q % 2]
                k.I("dve", lambda e: e.tensor_tensor(out=sg_[:].rearrange("p (h t) -> p h t", h=4),
                                                      in0=ps_[:].rearrange("p (h t) -> p h t", h=4),
                                                      in1=acum[:, 4 * q:4 * q + 4].rearrange("p (h o) -> p h o", o=1).to_broadcast([128, 4, 128]),
                                                      op=ALU.subtract), R=[ps_r, acum_r], W=[sg_r])
                k.I("act", lambda e: e.activation(out=sg_[:], in_=sg_[:], func=AF.Relu, scale=-1.0), R=[sg_r], W=[sg_r])
                k.I("act", lambda e: e.activation(out=sg_[:], in_=sg_[:], func=AF.Exp, scale=-1.0), R=[sg_r], W=[sg_r])

            def s3(q):
                g = q // 2
                gs = slice(g * 512, (g + 1) * 512)
                sg_, sg_r = seg[q % 2]
                m_, m_r = mt_[q % 2]
                py, py_r = PYs[g % 2]
                pz, pz_r = PZs[g % 2]
                k.I("dve", lambda e: e.tensor_tensor(out=m_[:].rearrange("p (h t) -> p h t", h=4),
                                                      in0=sg_[:].rearrange("p (h t) -> p h t", h=4),
                                                      in1=cbm[:, g:g + 1, :].to_broadcast([128, 4, 128]), op=ALU.mult),
                    R=[sg_r, cbm_r], W=[m_r])
                for i in range(4):
                    hh = 4 * q + i
                    hl = hh % 8
                    k.I("pe", lambda e: e.matmul(py[:, hl * 64:(hl + 1) * 64], m_[:, i * 128:(i + 1) * 128], xdt[:, hh * 64:(hh + 1) * 64],
                                                 start=True, stop=True), R=[m_r, xdt_r], W=[py_r], inc=(i == 3))
                if q % 2 == 0:
                    return
                k.I("pe", lambda e: e.matmul(pz[:], xbcT[:, 20 + g, cs], stb[:, gs], start=True, stop=True),
                    R=[xbcT_r, stb_r], W=[pz_r])
                t1_, t1_r = tmp[g % 2]
                t2_, t2_r = tmp2[g % 2]
                k.I("dve", lambda e: e.tensor_tensor(out=t1_[:].rearrange("p (h d) -> p h d", h=8),
                                                      in0=pz[:].rearrange("p (h d) -> p h d", h=8),
                                                      in1=ea[:, g * 8:(g + 1) * 8].rearrange("p (h o) -> p h o", o=1).to_broadcast([128, 8, 64]),
                                                      op=ALU.mult), R=[pz_r, ea_r], W=[t1_r])
                k.I("dve", lambda e: e.tensor_tensor(out=t1_[:], in0=py[:], in1=t1_[:], op=ALU.add), R=[py_r, t1_r], W=[t1_r])
                k.I("dve", lambda e: e.tensor_tensor(out=t2_[:].rearrange("p (h d) -> p h d", h=8),
                                                      in0=xtok[:, gs].rearrange("p (h d) -> p h d", h=8),
                                                      in1=rowc[:, 64 + g * 8:64 + (g + 1) * 8].rearrange("p (h o) -> p h o", o=1).to_broadcast([128, 8, 64]),
                                                      op=ALU.mult), R=[xtok_r, rowc_r], W=[t2_r])
                k.I("dve", lambda e: e.tensor_tensor(out=t1_[:], in0=t1_[:], in1=t2_[:], op=ALU.add), R=[t1_r, t2_r], W=[t1_r])
                k.I("dve", lambda e: e.tensor_tensor(out=t2_[:], in0=t1_[:], in1=zs[:, qs, gs], op=ALU.mult), R=[t1_r, zs_r], W=[t2_r])
                k.I("act", lambda e: e.activation(out=junk[:], in_=t2_[:], func=AF.Square, accum_out=ssq[:, g:g + 1]),
                    R=[t2_r], W=[junk_r, ssq_r])
                k.I("act", lambda e: e.activation(out=ssq[:, g:g + 1], in_=ssq[:, g:g + 1], func=AF.Ln, bias=EPS, scale=1.0 / 512),
                    R=[ssq_r], W=[ssq_r])
                k.I("act", lambda e: e.activation(out=ssq[:, g:g + 1], in_=ssq[:, g:g + 1], func=AF.Exp, scale=-0.5),
                    R=[ssq_r], W=[ssq_r])
                k.I("act", lambda e: e.activation(out=yn[:, gs], in_=t2_[:], func=AF.Copy, scale=ssq[:, g:g + 1]),
                    R=[t2_r, ssq_r], W=[yn_r])
                k.I("pe", lambda e: e.matmul(pz[:], btok[:, g * 128:(g + 1) * 128], xw[:, gs], start=True, stop=True),
                    R=[btok_r, xw_r], W=[pz_r])
                k.I("pool", lambda e: e.tensor_tensor(out=state[:, gs].rearrange("p (h d) -> p h d", h=8),
                                                       in0=state[:, gs].rearrange("p (h d) -> p h d", h=8),
                                                       in1=cd[:, g * 8:(g + 1) * 8].rearrange("p (h o) -> p h o", o=1).to_broadcast([128, 8, 64]),
                                                       op=ALU.mult), R=[cd_r], W=[state_r])
                k.I("dve", lambda e: e.tensor_tensor(out=state[:, gs], in0=pz[:], in1=state[:, gs], op=ALU.add),
                    R=[pz_r], W=[state_r])
                k.I("act", lambda e: e.copy(out=stb[:, gs], in_=state[:, gs]), R=[state_r], W=[stb_r])

            for j in range(8 + 2):
                if j < 8:
                    s1(j)
                if 0 <= j - 1 < 8:
                    s2(j - 1)
                if 0 <= j - 2 < 8:
                    s3(j - 2)
        def ch_post(qs):
            cs = slice(qs * 128, (qs + 1) * 128)
            for half in range(2):
                for j in range(8):
                    k.I("pe", lambda e: e.transpose(PTt[:, j * 128:(j + 1) * 128], yn[:, (half * 8 + j) * 128:(half * 8 + j + 1) * 128],
                                                    cmb(C, "ident")), R=[yn_r, C.cmb_r], W=[PT_r], inc=(j == 7))
                k.I("act", lambda e: e.copy(out=ynT[:, half * 8:(half + 1) * 8, cs], in_=PTt[:].rearrange("p (c q) -> p c q", c=8)),
                    R=[PT_r], W=[ynT_r])
        ch_pre(0)
        for qs in range(4):
            ch_heads(qs)
            if qs + 1 < 4:
                ch_pre(qs + 1)
            ch_post(qs)
        for dc in range(8):
            pp, pp_r = (PA, PB)[dc % 2]
            for kc in range(16):
                k.I("pe", lambda e: e.matmul(pp[:], wout[:, kc, dc * 128:(dc + 1) * 128], ynT[:, kc, :],
                                             start=(kc == 0), stop=(kc == 15)), R=[wout_r, ynT_r], W=[pp_r], inc=(kc == 15))
            k.I("dve", lambda e: e.tensor_tensor(out=h[:, dc, :], in0=pp[:], in1=h[:, dc, :], op=ALU.add),
                R=[pp_r, h_r], W=[h_r])
        for c in range(8):
            k.dma("sp", h_out[c, :, t0:t0 + T], h[:, c, :], R=[h_r])


_CACHE = {}


def build_program():
    nc = bass.Bass("TRN2", target_bir_lowering=False)
    def din(name, shape, dt=F32):
        return nc.dram_tensor(name, list(shape), dt, kind="ExternalInput").ap()
    xT = din("xT", [8, 128, S]); memT = din("memT", [8, 128, 256]); pos = din("pos", [S], I32)
    cst_d = din("cst", [128, NCST]); cm_d = din("cm", [128, NCM]); rowc_d = din("rowc", [128, 96])
    hy_w_in = din("hy_w_in", [1024, 2048]); hy_w_out = din("hy_w_out", [1024, 1024]); pool_w = din("pool_w", [4, 128, 128])
    xa_wq = [din(f"xa_wq{l}", [1024, 1024]) for l in range(2)]
    xa_wkv = [din(f"xa_wkv{l}", [1024, 2048]) for l in range(2)]
    xa_wo = [din(f"xa_wo{l}", [1024, 1024]) for l in range(2)]
    ffn_wg = din("ffn_wg", [1024, 2816]); ffn_wu = din("ffn_wu", [1024, 2816]); ffn_wd = din("ffn_wd", [2816, 1024])
    ssd_w_in = din("ssd_w_in", [1024, 5152]); ssd_w_out = din("ssd_w_out", [2048, 1024])
    moe_wr = din("moe_wr", [1024, 8]); moe_wg = din("moe_wg", [8, 1024, 3584]); moe_wu = din("moe_wu", [8, 1024, 3584])
    moe_wd = din("moe_wd", [8, 3584, 1024])
    hs_ = [nc.dram_tensor(f"hscr{i}", [8, 128, S], F32, kind="Internal").ap() for i in range(5)]
    outT = nc.dram_tensor("outT", [8, 128, S], F32, kind="ExternalOutput").ap()
    with ExitStack() as es:
        k = KB(nc, es)
        C = setup_common(k, nc, cst_d, cm_d)
        def group(fn):
            with ExitStack() as ges:
                k.es = ges
                fn()
                k.barrier()
        BFW = {}

        def casts():
            for l in range(2):
                BFW[f"wq{l}"] = cast_w(k, nc, xa_wq[l], 1024, 1024, f"bf_wq{l}")
                BFW[f"wkv{l}"] = cast_w(k, nc, xa_wkv[l], 1024, 2048, f"bf_wkv{l}")
                BFW[f"wo{l}"] = cast_w(k, nc, xa_wo[l], 1024, 1024, f"bf_wo{l}")
                if l == 0:
                    BFW["wg"] = cast_w(k, nc, ffn_wg, 1024, 2816, "bf_wg")
                    BFW["wu"] = cast_w(k, nc, ffn_wu, 1024, 2816, "bf_wu")
                    BFW["wd"] = cast_w(k, nc, ffn_wd, 2816, 1024, "bf_wd")
                    BFW["win"] = cast_w(k, nc, ssd_w_in, 1024, 5152, "bf_win")
                    BFW["wout"] = cast_w(k, nc, ssd_w_out, 2048, 1024, "bf_wout")

        def xbf(l):
            return {"wq": BFW[f"wq{l}"], "wkv": BFW[f"wkv{l}"], "wo": BFW[f"wo{l}"]}

        group(lambda: g1_moba(k, nc, C, es, xT, hs_[0], pos, hy_w_in, hy_w_out, pool_w, after_loads=casts))
        group(lambda: g_xattn(k, nc, C, hs_[0], hs_[1], memT, xa_wq[0], xa_wkv[0], xa_wo[0], 0, bf=xbf(0)))
        group(lambda: g_ffn(k, nc, C, hs_[1], hs_[2], ffn_wg, ffn_wu, ffn_wd, bf=BFW))
        group(lambda: g_ssd(k, nc, C, hs_[2], hs_[3], rowc_d, ssd_w_in, ssd_w_out, bf=BFW))
        group(lambda: g_xattn(k, nc, C, hs_[3], hs_[4], memT, xa_wq[1], xa_wkv[1], xa_wo[1], 1, bf=xbf(1)))
        group(lambda: g_moe(k, nc, C, hs_[4], outT, moe_wr, moe_wg, moe_wu, moe_wd))
    return nc


def kernel(**inp):
    inp = {k_: np.asarray(v) for k_, v in inp.items()}
    if "nc" not in _CACHE:
        _CACHE["nc"] = build_program()
    nc = _CACHE["nc"]
    c, m = host_consts(inp)
    rowc = host_rowc(inp)
    f32 = lambda a: np.ascontiguousarray(a, dtype=np.float32)
    shared = {
        "pos": np.ascontiguousarray(inp["positions"], dtype=np.int32), "cst": c, "cm": m, "rowc": rowc,
        "hy_w_in": f32(inp["hy_w_in"][0]), "hy_w_out": f32(inp["hy_w_out"][0]), "pool_w": f32(inp["pool_w"][0]),
        "ffn_wg": f32(inp["ffn_w_gate"][0]), "ffn_wu": f32(inp["ffn_w_up"][0]), "ffn_wd": f32(inp["ffn_w_down"][0]),
        "ssd_w_in": f32(inp["ssd_w_in"][0]), "ssd_w_out": f32(inp["ssd_w_out"][0]),
        "moe_wr": f32(inp["moe_router"][0]), "moe_wg": f32(inp["moe_w_gate"][0]), "moe_wu": f32(inp["moe_w_up"][0]),
        "moe_wd": f32(inp["moe_w_down"][0]),
    }
    for l in range(2):
        shared[f"xa_wq{l}"] = f32(inp["xa_wq"][l]); shared[f"xa_wkv{l}"] = f32(inp["xa_wkv"][l]); shared[f"xa_wo{l}"] = f32(inp["xa_wo"][l])
    in_maps = []
    for b in range(8):
        mp = dict(shared)
        mp["xT"] = np.ascontiguousarray(inp["x"][b].T, dtype=np.float32).reshape(8, 128, S)
        mp["memT"] = np.ascontiguousarray(inp["mem"][b].T, dtype=np.float32).reshape(8, 128, 256)
        in_maps.append(mp)
    res = run_bass_kernel_spmd(nc, in_maps, core_ids=list(range(8)))
    out = np.empty((8, S, D), np.float32)
    for b in range(8):
        out[b] = np.asarray(res.results[b]["outT"]).reshape(D, S).T
    return out
```

```python
from concourse.bass_utils import run_bass_kernel_spmd
import numpy as np
import concourse.bass as bass
import concourse.mybir as mybir
from contextlib import ExitStack

F32 = mybir.dt.float32
BF16 = mybir.dt.bfloat16
I32 = mybir.dt.int32
AF = mybir.ActivationFunctionType
ALU = mybir.AluOpType
AX = mybir.AxisListType


class Res:
    __slots__ = ("w", "r", "name")

    def __init__(self, name=""):
        self.w = None
        self.r = {}
        self.name = name


class KB:
    RING = {"sp": 16, "pool": 12, "act": 4}

    def __init__(self, nc, es):
        self.nc = nc
        self.es = es
        self.eng = {"pe": nc.tensor, "dve": nc.vector, "act": nc.scalar,
                    "pool": nc.gpsimd, "sp": nc.sync}
        self.sems = {}
        for e in ("pe", "dve", "act", "pool"):
            self.sems[e] = es.enter_context(nc.semaphore("c_" + e))
        self.cnt = {e: 0 for e in ("pe", "dve", "act", "pool")}
        self.ring = {}
        self.ring_i = {}
        for q, n in self.RING.items():
            self.ring[q] = []
            for i in range(n):
                key = f"d_{q}{i}"
                self.sems[key] = es.enter_context(nc.semaphore(key))
                self.ring[q].append(key)
            self.ring_i[q] = 0
        self.seen = {e: {} for e in self.eng}
        self.n_inst = 0
        self.n_wait = 0
        self.out_tokens = []

    def sb(self, name, shape, dtype):
        self.uid = getattr(self, "uid", 0) + 1
        t = self.es.enter_context(self.nc.sbuf_tensor(f"s{self.uid}_" + name, list(shape), dtype))
        return t, Res(name)

    def ps(self, name, shape, dtype):
        t = self.es.enter_context(self.nc.psum_tensor("p_" + name, list(shape), dtype))
        return t, Res(name)

    def _waits(self, e, R, W):
        need = {}
        for r in R:
            if r.w is not None:
                k, v = r.w
                if need.get(k, 0) < v:
                    need[k] = v
        for w in W:
            if w.w is not None:
                k, v = w.w
                if need.get(k, 0) < v:
                    need[k] = v
            for k, v in w.r.items():
                if need.get(k, 0) < v:
                    need[k] = v
        seen = self.seen[e]
        eng = self.eng[e]
        for k, v in need.items():
            if e == "pe" and k == "pe":
                continue
            if seen.get(k, 0) >= v:
                continue
            eng.wait_ge(self.sems[k], v)
            seen[k] = v
            self.n_wait += 1

    def _commit(self, tok, R, W):
        k, v = tok
        for r in R:
            if r.r.get(k, 0) < v:
                r.r[k] = v
        for w in W:
            w.w = tok
            w.r = {}

    def I(self, e, fn, R=(), W=(), inc=True):
        self._waits(e, R, W)
        ins = fn(self.eng[e])
        self.n_inst += 1
        if inc:
            self.cnt[e] += 1
            ins.then_inc(self.sems[e], 1)
            tok = (e, self.cnt[e])
        else:
            tok = (e, self.cnt[e] + 1)
        self._commit(tok, R, W)
        return tok

    def dma(self, q, out, in_, R=(), W=(), **kw):
        e = q
        i = self.ring_i[q]
        n = len(self.ring[q])
        key = self.ring[q][i % n]
        prev = 16 * (i // n)
        eng = self.eng[e]
        if prev > 0 and self.seen[e].get(key, 0) < prev:
            eng.wait_ge(self.sems[key], prev)
            self.seen[e][key] = prev
        self._waits(e, R, W)
        ins = eng.dma_start(out=out, in_=in_, **kw)
        ins.then_inc(self.sems[key], 16)
        self.ring_i[q] = i + 1
        self.n_inst += 1
        tok = (key, prev + 16)
        self._commit(tok, R, W)
        return tok

    def finish(self, toks):
        eng = self.eng["sp"]
        for k, v in toks:
            eng.wait_ge(self.sems[k], v)


def kb_barrier(self):
    for e, eng in self.eng.items():
        for x in ("pe", "dve", "act", "pool"):
            v = self.cnt[x]
            if v > 0 and self.seen[e].get(x, 0) < v and not (e == x):
                eng.wait_ge(self.sems[x], v)
                self.seen[e][x] = v
        for q in self.ring:
            n = len(self.ring[q])
            tot = self.ring_i[q]
            for j in range(min(n, tot)):
                idx = tot - 1 - j
                key = self.ring[q][idx % n]
                v = 16 * (idx // n + 1)
                if self.seen[e].get(key, 0) < v:
                    eng.wait_ge(self.sems[key], v)
                    self.seen[e][key] = v


KB.barrier = kb_barrier


import numpy as np

S = 4096
D = 1024
T = 512
NT = S // T
EPS = 1e-6
NEG = -30000.0

CST = {}
_c = 0
def _col(name, n):
    global _c
    CST[name] = _c
    _c += n
for _n in ("mix0", "mix1", "xa0", "xa1", "mem0", "mem1", "ffn0", "ffn1"):
    _col(_n, 8)
_col("hyq", 1); _col("hyk", 1); _col("pscale", 4)
_col("xaq0", 2); _col("xaq1", 2); _col("xak0", 2); _col("xak1", 2)
_col("invf", 1)
_col("convw", 96); _col("convb", 24); _col("ssdn", 16)
_col("rcnt", 64)
NCST = _c

CM = {"ident": 0, "ones": 128, "bones": 256, "rot": 384, "tri01": 512, "upper": 640}
NCM = 768


def host_consts(inp):
    c = np.zeros((128, NCST), np.float32)
    def put(name, v):
        v = np.asarray(v, np.float32).reshape(-1, 128).T
        c[:, CST[name]:CST[name] + v.shape[1]] = v
    for l in range(2):
        put(f"mix{l}", inp["mix_norm"][l]); put(f"xa{l}", inp["xa_norm"][l])
        put(f"mem{l}", inp["mem_norm"][l]); put(f"ffn{l}", inp["ffn_norm"][l])
        put(f"xaq{l}", inp["xa_q_norm"][l]); put(f"xak{l}", inp["xa_k_norm"][l])
    put("hyq", np.tile(inp["hy_q_norm"][0], 2)); put("hyk", np.tile(inp["hy_k_norm"][0], 2))
    put("pscale", inp["pool_scale"][0])
    invf = (10000.0 ** (-np.arange(32, dtype=np.float32) / 32)).astype(np.float32)
    put("invf", np.tile(invf, 4))
    cw = inp["ssd_conv_w"][0]
    cwl = np.zeros((128, 96), np.float32)
    for ch in range(24):
        for i in range(4):
            cwl[:, 4 * ch + i] = cw[i, ch * 128:(ch + 1) * 128]
    c[:, CST["convw"]:CST["convw"] + 96] = cwl
    put("convb", inp["ssd_conv_b"][0]); put("ssdn", inp["ssd_norm"][0])
    rc = np.zeros((128, 64), np.float32)
    for g, w in enumerate((2, 4, 8, 16)):
        rc[:, g * 16:(g + 1) * 16] = 1.0 / np.minimum(np.arange(16) + 1, w)
    c[:, CST["rcnt"]:CST["rcnt"] + 64] = rc
    m = np.zeros((128, NCM), np.float32)
    m[:, 0:128] = np.eye(128)
    m[:, 128:256] = 1.0
    for p in range(128):
        for q in range(128):
            if p // 64 == q // 64:
                m[p, 256 + q] = 1.0
    for p in range(128):
        if p % 64 < 32:
            m[p + 32, 384 + p] = -1.0
        else:
            m[p - 32, 384 + p] = 1.0
    qi = np.arange(128)[:, None]; ki = np.arange(128)[None, :]
    m[:, 512:640] = (ki <= qi).astype(np.float32)
    m[:, 640:768] = (qi <= ki).astype(np.float32)
    return c, m


def host_rowc(inp):
    r = np.zeros((128, 96), np.float32)
    r[:, 0:32] = inp["ssd_dt_bias"][0][None, :]
    r[:, 32:64] = inp["ssd_a_log"][0][None, :]
    r[:, 64:96] = inp["ssd_d"][0][None, :]
    return r


class Ctx:
    pass


def setup_common(k, nc, cst_d, cm_d):
    C = Ctx()
    C.cst, C.cst_r = k.sb("cst", [128, NCST], F32)
    C.cmf, C.cmf_r = k.sb("cmf", [128, NCM], F32)
    C.cmb, C.cmb_r = k.sb("cmb", [128, NCM], BF16)
    C.trib, C.trib_r = k.sb("trib", [128, 128], F32)
    k.dma("sp", C.cst[:], cst_d[:, :], W=[C.cst_r])
    k.dma("sp", C.cmf[:], cm_d[:, :], W=[C.cmf_r])
    k.I("dve", lambda e: e.tensor_copy(out=C.cmb[:], in_=C.cmf[:]), R=[C.cmf_r], W=[C.cmb_r])
    k.I("dve", lambda e: e.tensor_scalar(out=C.trib[:], in0=C.cmf[:, 512:640], scalar1=-1.0, scalar2=-NEG,
                                          op0=ALU.add, op1=ALU.mult), R=[C.cmf_r], W=[C.trib_r])
    C.P = []
    for i in range(7):
        C.P.append(k.ps(f"P{i}", [128, 512], F32))
    C.PT = k.ps("PTb", [128, 1024], BF16)
    return C


def col(C, name, i=0):
    c = CST[name] + i
    return C.cst[:, c:c + 1]


def cmb(C, name, rows=128):
    o = CM[name]
    return C.cmb[0:rows, o:o + 128]


def load_w_bf16(k, dst, dst_r, src, nk, ncols, q="pool"):
    for c in range(nk):
        for c0 in range(0, ncols, 1024):
            c1 = min(ncols, c0 + 1024)
            k.dma(q, dst[:, c, c0:c1], src[c * 128:(c + 1) * 128, c0:c1], W=[dst_r])


def load_w_staged(k, dst, dst_r, src, nk, ncols, stg, cnt=[0]):
    for c0 in range(0, ncols, 512):
        c1 = min(ncols, c0 + 512)
        for c in range(nk):
            st, st_r = stg[cnt[0] % len(stg)]
            k.dma("sp", st[:, 0:c1 - c0], src[c * 128:(c + 1) * 128, c0:c1], W=[st_r])
            if cnt[0] % 2 == 0:
                k.I("dve", lambda e: e.tensor_copy(out=dst[:, c, c0:c1], in_=st[:, 0:c1 - c0]), R=[st_r], W=[dst_r])
            else:
                k.I("act", lambda e: e.copy(out=dst[:, c, c0:c1], in_=st[:, 0:c1 - c0]), R=[st_r], W=[dst_r])
            cnt[0] += 1


def cast_w(k, nc, src, rows, cols, name):
    dst = nc.dram_tensor(name, [rows, cols], BF16, kind="Internal").ap()
    r = Res(name)
    for r0 in range(0, rows, 128):
        for c0 in range(0, cols, 1024):
            c1 = min(cols, c0 + 1024)
            k.dma("pool", dst[r0:r0 + 128, c0:c1], src[r0:r0 + 128, c0:c1], W=[r])
    return dst, r


def load_bf(k, dst, dst_r, bf, c0, c1, nk=None):
    src, src_r = bf
    v = src.rearrange("(c p) f -> p c f", p=128)
    if nk is not None:
        v = v[:, 0:nk, :]
    k.dma("sp", dst[:, :, 0:c1 - c0], v[:, :, c0:c1], R=[src_r], W=[dst_r])


def rmsnorm_T(k, C, h, h_r, gname, hn, hn_r, sq, sq_r, rstd, rstd_r, PM, n=T):
    pm, pm_r = PM
    ns = len(sq_r)
    for c in range(8):
        k.I("act", lambda e: e.activation(out=sq[:, c % ns, 0:n], in_=h[:, c, 0:n], func=AF.Square),
            R=[h_r], W=[sq_r[c % ns]])
        k.I("pe", lambda e: e.matmul(pm[:, 0:n], cmb(C, "ones"), sq[:, c % ns, 0:n], start=(c == 0), stop=(c == 7)),
            R=[sq_r[c % ns], C.cmb_r], W=[pm_r])
    k.I("act", lambda e: e.activation(out=rstd[:, 0:n], in_=pm[:, 0:n], func=AF.Ln, bias=EPS, scale=1.0 / D),
        R=[pm_r], W=[rstd_r])
    k.I("act", lambda e: e.activation(out=rstd[:, 0:n], in_=rstd[:, 0:n], func=AF.Exp, scale=-0.5), R=[rstd_r], W=[rstd_r])
    for c in range(8):
        k.I("dve", lambda e: e.scalar_tensor_tensor(out=hn[:, c, 0:n], in0=h[:, c, 0:n], scalar=col(C, gname, c),
                                                    in1=rstd[:, 0:n], op0=ALU.mult, op1=ALU.mult),
            R=[h_r, rstd_r, C.cst_r], W=[hn_r])


def g1_moba(k, nc, C, es, xT, h1, positions, w_in_d, w_out_d, pool_w_d, after_loads=None):
    (PA, PB, PS0, PS1, PO0, PO1, PM) = C.P
    PTt, PT_r = C.PT
    win, win_r = k.sb("win", [128, 8, 2048], BF16)
    wout, wout_r = k.sb("wout", [128, 8, 1024], BF16)
    poolw, poolw_r = k.sb("poolw", [128, 4, 128], BF16)
    KT, KT_r = k.sb("KT", [128, 4, S], BF16)
    V, V_r = k.sb("V", [128, 32, 512], BF16)
    kmT, kmT_r = k.sb("kmT", [128, 4, 32], BF16)
    h, h_r = k.sb("h", [128, 8, T], F32)
    hn, hn_r = k.sb("hn", [128, 8, T], BF16)
    sq, _ = k.sb("sq", [128, 2, T], BF16)
    sq_r = [Res() for _ in range(2)]
    rstd, rstd_r = k.sb("rstd", [128, T], F32)
    qT, qT_r = k.sb("qT", [128, 4, T], BF16)
    catT, catT_r = k.sb("catT", [128, 8, T], BF16)
    qTz, qTz_r = k.sb("qTz", [128, 8, T], BF16)
    cos, cos_r = k.sb("cos", [128, T], F32)
    sin, sin_r = k.sb("sin", [128, T], F32)
    posi, posi_r = k.sb("posi", [128, T], I32)
    wk = [k.sb(f"wk{i}", [128, 528], F32) for i in range(6)]
    qnb, qnb_r = k.sb("qnb", [128, T], BF16)
    sqb, sqb_r = k.sb("sqb", [128, T], BF16)
    uext = [k.sb(f"uext{g}", [128, 528], F32) for g in range(4)]
    pooled, pooled_r = k.sb("pooled", [128, T], BF16)
    gsb, gsb_r = k.sb("gsb", [128, 8, 16], F32)
    biasq4, _ = k.sb("biasq4", [128, 4, 8, 16], F32)
    biasq4_r = [Res() for _ in range(4)]
    PMq_r = [Res() for _ in range(4)]
    PTv = [(PTt, PT_r), (PA[0][:, :].bitcast(BF16), PA[1])]
    m8, m8_r = k.sb("m8", [128, 8, 8], F32)
    Pb = [k.sb(f"Pb{i}", [128, T], BF16) for i in range(2)]
    PTs = [k.sb(f"PTs{i}", [128, T], BF16) for i in range(2)]
    rs = [k.sb(f"rs{i}", [128, 32], F32) for i in range(4)]
    rinv, rinv_r = k.sb("rinv", [128, 8], F32)
    otok, otok_r = k.sb("otok", [128, 512], BF16)
    dsm, dsm_r = k.sb("dsm", [128, 128], F32)

    for g in range(4):
        k.I("dve", lambda e: e.memset(uext[g][0][:, 0:16], 0.0), W=[uext[g][1]])
    k.I("dve", lambda e: e.memset(kmT[:], 0.0), W=[kmT_r])
    k.I("dve", lambda e: e.memset(qTz[:], 0.0), W=[qTz_r])
    stg = wk[0:4]
    load_w_staged(k, win, win_r, w_in_d, 8, 2048, stg)
    load_w_staged(k, wout, wout_r, w_out_d, 8, 1024, stg)
    for g in range(4):
        k.dma("pool", poolw[:, g, :], pool_w_d[g, :, :], W=[poolw_r])
    if after_loads is not None:
        after_loads()

    twopi = float(2 * np.pi)
    pcount = 0
    for t in range(NT):
        t0 = t * T
        for c in range(8):
            k.dma("sp", h[:, c, :], xT[c, :, t0:t0 + T], W=[h_r])
        k.dma("sp", posi[:], positions[t0:t0 + T].partition_broadcast(128), W=[posi_r])
        (a0, a0r), (a1, a1r), (a2, a2r) = wk[0], wk[1], wk[2]
        k.I("dve", lambda e: e.tensor_copy(out=a0[:, 0:T], in_=posi[:]), R=[posi_r], W=[a0r])
        k.I("dve", lambda e: e.tensor_scalar(out=a0[:, 0:T], in0=a0[:, 0:T], scalar1=col(C, "invf"), scalar2=1.0 / twopi,
                                              op0=ALU.mult, op1=ALU.mult), R=[a0r, C.cst_r], W=[a0r])
        for (dst, dst_r, off) in ((sin, sin_r, 0.5), (cos, cos_r, 0.75)):
            k.I("dve", lambda e: e.tensor_scalar(out=a1[:, 0:T], in0=a0[:, 0:T], scalar1=off, scalar2=None, op0=ALU.add),
                R=[a0r], W=[a1r])
            k.I("dve", lambda e: e.tensor_copy(out=posi[:], in_=a1[:, 0:T]), R=[a1r], W=[posi_r])
            k.I("dve", lambda e: e.tensor_copy(out=a2[:, 0:T], in_=posi[:]), R=[posi_r], W=[a2r])
            k.I("dve", lambda e: e.tensor_tensor(out=a1[:, 0:T], in0=a1[:, 0:T], in1=a2[:, 0:T], op=ALU.subtract),
                R=[a1r, a2r], W=[a1r])
            k.I("dve", lambda e: e.tensor_scalar(out=a2[:, 0:T], in0=a1[:, 0:T], scalar1=0.0, scalar2=None, op0=ALU.is_lt),
                R=[a1r], W=[a2r])
            k.I("dve", lambda e: e.tensor_tensor(out=a1[:, 0:T], in0=a1[:, 0:T], in1=a2[:, 0:T], op=ALU.add),
                R=[a1r, a2r], W=[a1r])
            k.I("act", lambda e: e.activation(out=dst[:], in_=a1[:, 0:T], func=AF.Sin, bias=-float(np.pi), scale=twopi),
                R=[a1r], W=[dst_r])
        rmsnorm_T(k, C, h, h_r, "mix0", hn, hn_r, sq, sq_r, rstd, rstd_r, PM)
        for c in range(8):
            pp, pp_r = (PA, PB)[c % 2]
            for kc in range(8):
                k.I("pe", lambda e: e.matmul(pp[:], win[:, kc, c * 128:(c + 1) * 128], hn[:, kc, :],
                                             start=(kc == 0), stop=(kc == 7)),
                    R=[win_r, hn_r], W=[pp_r], inc=(kc == 7))
            isq = c < 4
            gname = "hyq" if isq else "hyk"
            (qn, qn_r), (t1, t1_r), (t2, t2_r), (r2, r2_r) = wk[0], wk[1], wk[2], wk[3]
            k.I("act", lambda e: e.activation(out=sqb[:], in_=pp[:], func=AF.Square), R=[pp_r], W=[sqb_r])
            k.I("act", lambda e: e.activation(out=qn[:, 0:T], in_=pp[:], func=AF.Copy, scale=col(C, gname)),
                R=[pp_r, C.cst_r], W=[qn_r])
            k.I("pe", lambda e: e.matmul(PM[0][:], cmb(C, "bones"), sqb[:], start=True, stop=True),
                R=[sqb_r, C.cmb_r], W=[PM[1]])
            k.I("act", lambda e: e.activation(out=r2[:, 0:T], in_=PM[0][:], func=AF.Ln, bias=EPS, scale=1.0 / 64),
                R=[PM[1]], W=[r2_r])
            k.I("act", lambda e: e.activation(out=r2[:, 0:T], in_=r2[:, 0:T], func=AF.Exp, scale=-0.5), R=[r2_r], W=[r2_r])
            k.I("dve", lambda e: e.tensor_copy(out=qnb[:], in_=qn[:, 0:T]), R=[qn_r], W=[qnb_r])
            k.I("pe", lambda e: e.matmul(PM[0][:], cmb(C, "rot"), qnb[:], start=True, stop=True),
                R=[qnb_r, C.cmb_r], W=[PM[1]])
            k.I("dve", lambda e: e.tensor_tensor(out=t2[:, 0:T], in0=PM[0][:], in1=sin[:], op=ALU.mult),
                R=[PM[1], sin_r], W=[t2_r])
            k.I("dve", lambda e: e.tensor_tensor(out=t1[:, 0:T], in0=qn[:, 0:T], in1=cos[:], op=ALU.mult),
                R=[qn_r, cos_r], W=[t1_r])
            k.I("dve", lambda e: e.tensor_tensor(out=t1[:, 0:T], in0=t1[:, 0:T], in1=t2[:, 0:T], op=ALU.add),
                R=[t1_r, t2_r], W=[t1_r])
            if isq:
                k.I("dve", lambda e: e.tensor_tensor(out=qT[:, c, :], in0=t1[:, 0:T], in1=r2[:, 0:T], op=ALU.mult),
                    R=[t1_r, r2_r], W=[qT_r])
                k.I("act", lambda e: e.copy(out=qTz[0:64, 2 * c, :], in_=qT[0:64, c, :]), R=[qT_r], W=[qTz_r])
                k.I("act", lambda e: e.copy(out=qTz[64:128, 2 * c + 1, :], in_=qT[64:128, c, :]), R=[qT_r], W=[qTz_r])
            else:
                kc4 = c - 4
                k.I("dve", lambda e: e.tensor_tensor(out=t2[:, 0:T], in0=t1[:, 0:T], in1=r2[:, 0:T], op=ALU.mult),
                    R=[t1_r, r2_r], W=[t2_r])
                k.I("act", lambda e: e.copy(out=KT[:, kc4, t0:t0 + T], in_=t2[:, 0:T]), R=[t2_r], W=[KT_r])
                k.I("dve", lambda e: e.tensor_reduce(out=r2[:, 0:2], in_=t2[:, 0:T].rearrange("p (b x) -> p b x", b=2),
                                                      axis=AX.X, op=ALU.add), R=[t2_r], W=[r2_r])
                k.I("act", lambda e: e.mul(out=kmT[0:64, kc4, 2 * t:2 * t + 2], in_=r2[0:64, 0:2], mul=1.0 / 256),
                    R=[r2_r], W=[kmT_r])
                k.I("act", lambda e: e.mul(out=kmT[64:128, kc4, 16 + 2 * t:16 + 2 * t + 2], in_=r2[64:128, 0:2], mul=1.0 / 256),
                    R=[r2_r], W=[kmT_r])
        for qs in range(4):
            pp, pp_r = (PA, PB)[qs % 2]
            for kc in range(8):
                k.I("pe", lambda e: e.matmul(pp[:], hn[:, kc, qs * 128:(qs + 1) * 128], win[:, kc, 1024:1536],
                                             start=(kc == 0), stop=(kc == 7)),
                    R=[win_r, hn_r], W=[pp_r], inc=(kc == 7))
            k.I("act", lambda e: e.copy(out=V[:, 4 * t + qs, :], in_=pp[:]), R=[pp_r], W=[V_r])
        for g in range(4):
            w = 2 << g
            pp, pp_r = (PA, PB)[g % 2]
            ue, ue_r = uext[g]
            for kc in range(8):
                k.I("pe", lambda e: e.matmul(pp[:], win[:, kc, (12 + g) * 128:(13 + g) * 128], hn[:, kc, :],
                                             start=(kc == 0), stop=(kc == 7)),
                    R=[win_r, hn_r], W=[pp_r], inc=(kc == 7))
            k.I("act", lambda e: e.copy(out=ue[:, 16:528], in_=pp[:]), R=[pp_r], W=[ue_r])
            cur, cur_r = ue, ue_r
            step = 1
            bufs = [wk[4], wk[5]]
            bi = 0
            lo = 0
            while step < w:
                lo += step
                nxt, nxt_r = bufs[bi]
                bi ^= 1
                k.I("dve", lambda e: e.tensor_tensor(out=nxt[:, lo:528], in0=cur[:, lo:528], in1=cur[:, lo - step:528 - step],
                                                       op=ALU.add), R=[cur_r], W=[nxt_r])
                cur, cur_r = nxt, nxt_r
                step *= 2
            (pf, pf_r) = wk[3]
            k.I("dve", lambda e: e.scalar_tensor_tensor(out=pf[:, 0:T], in0=cur[:, 16:528], scalar=1.0 / w, in1=ue[:, 16:528],
                                                        op0=ALU.mult, op1=ALU.subtract), R=[cur_r, ue_r], W=[pf_r])
            if t == 0:
                rc = C.cst[:, CST["rcnt"] + 16 * g: CST["rcnt"] + 16 * g + 16]
                k.I("dve", lambda e: e.tensor_tensor(out=pf[:, 0:16], in0=cur[:, 16:32], in1=rc, op=ALU.mult),
                    R=[cur_r, C.cst_r], W=[pf_r])
                k.I("dve", lambda e: e.tensor_tensor(out=pf[:, 0:16], in0=pf[:, 0:16], in1=ue[:, 16:32], op=ALU.subtract),
                    R=[ue_r], W=[pf_r])
            k.I("act", lambda e: e.copy(out=pooled[:], in_=pf[:, 0:T]), R=[pf_r], W=[pooled_r])
            k.I("act", lambda e: e.copy(out=ue[:, 0:16], in_=ue[:, 512:528]), R=[cur_r], W=[ue_r])
            k.I("pe", lambda e: e.matmul(PM[0][:], poolw[:, g, :], pooled[:], start=True, stop=True),
                R=[poolw_r, pooled_r], W=[PM[1]])
            k.I("act", lambda e: e.activation(out=catT[:, 4 + g, :], in_=PM[0][:], func=AF.Copy, scale=col(C, "pscale", g)),
                R=[PM[1], C.cst_r], W=[catT_r])
        for q_ in range(4):
            PMq_r[q_].w = PM[1].w
            PMq_r[q_].r = dict(PM[1].r)
        for qs in range(4):
            gq = 4 * t + qs
            own = gq // 2
            q0 = qs * 128
            bq = biasq4[:, qs]
            if own > 0:
                for c4 in range(4):
                    k.I("pe", lambda e: e.matmul(PM[0][:, qs * 128 + c4 * 32:qs * 128 + (c4 + 1) * 32], qT[:, c4, q0:q0 + 128],
                                                 kmT[:, c4, :], start=True, stop=True),
                        R=[qT_r, kmT_r], W=[PM[1]], inc=(c4 == 3))
                k.I("dve", lambda e: e.memset(gsb[:], -1e30), W=[gsb_r])
                k.I("dve", lambda e: e.tensor_copy(out=gsb[:, :, 0:own],
                                                    in_=PM[0][:, qs * 128:(qs + 1) * 128].rearrange("p (h j) -> p h j", h=8)[:, :, 0:own]),
                    R=[PM[1]], W=[gsb_r])
                for hh in range(8):
                    k.I("dve", lambda e: e.max(out=m8[:, hh, :], in_=gsb[:, hh, :]), R=[gsb_r], W=[m8_r])
                k.I("dve", lambda e: e.tensor_tensor(out=bq[:, :, 0:own], in0=gsb[:, :, 0:own],
                                                      in1=m8[:, :, 2:3].to_broadcast([128, 8, own]), op=ALU.is_lt),
                    R=[gsb_r, m8_r], W=[biasq4_r[qs]])
                k.I("dve", lambda e: e.tensor_scalar(out=bq[:, :, 0:own], in0=bq[:, :, 0:own], scalar1=NEG, scalar2=None,
                                                      op0=ALU.mult), R=[biasq4_r[qs]], W=[biasq4_r[qs]])
            k.I("dve", lambda e: e.memset(bq[:, :, own:own + 1], 0.0), W=[biasq4_r[qs]])
        items = []
        for qs in range(4):
            gq = 4 * t + qs
            ngrp = gq // 4 + 1
            for hh in range(8):
                for kg in range(ngrp):
                    items.append((qs, hh, kg, kg == 0, kg == ngrp - 1, hh == 7 and kg == ngrp - 1))
        nrs_of = {}

        def stA(i):
            qs, hh, kg, fh, lh, lq = items[i]
            gq = 4 * t + qs
            kt_lo = kg * 4
            kt_hi = min(kt_lo + 4, gq + 1)
            nk = kt_hi - kt_lo
            PSx, PSx_r = (PS0, PS1)[i % 2]
            k.I("pe", lambda e: e.matmul(PSx[:, 0:nk * 128], qTz[:, hh, qs * 128:(qs + 1) * 128],
                                         KT[:, hh // 2, kt_lo * 128:kt_hi * 128], start=True, stop=True),
                R=[qTz_r, KT_r], W=[PSx_r])

        def stB(i):
            qs, hh, kg, fh, lh, lq = items[i]
            gq = 4 * t + qs
            kt_lo = kg * 4
            kt_hi = min(kt_lo + 4, gq + 1)
            PSx, PSx_r = (PS0, PS1)[i % 2]
            Pbx, Pbx_r = Pb[i % 2]
            rsx, rsx_r = rs[hh % 4]
            nrs = 0 if fh else nrs_of[(qs, hh)]
            kt = kt_lo
            while kt < kt_hi:
                j = kt // 2
                c0 = (kt - kt_lo) * 128
                if kt == gq:
                    k.I("act", lambda e: e.activation(out=Pbx[:, c0:c0 + 128], in_=PSx[:, c0:c0 + 128], func=AF.Exp, scale=0.125),
                        R=[PSx_r], W=[Pbx_r])
                    k.I("dve", lambda e: e.scalar_tensor_tensor(out=Pbx[:, c0:c0 + 128], in0=Pbx[:, c0:c0 + 128], scalar=1.0,
                                                                in1=cmb(C, "tri01"), op0=ALU.mult, op1=ALU.mult,
                                                                accum_out=rsx[:, nrs:nrs + 1]),
                        R=[C.cmb_r], W=[Pbx_r, rsx_r])
                    nrs += 1
                    kt += 1
                else:
                    n = 2 if (kt % 2 == 0 and kt + 1 < kt_hi and kt + 1 != gq) else 1
                    k.I("act", lambda e: e.activation(out=Pbx[:, c0:c0 + n * 128], in_=PSx[:, c0:c0 + n * 128],
                                                      func=AF.Exp, scale=0.125, bias=biasq4[:, qs, hh, j:j + 1],
                                                      accum_out=rsx[:, nrs:nrs + 1]),
                        R=[PSx_r, biasq4_r[qs]], W=[Pbx_r, rsx_r])
                    nrs += 1
                    kt += n
            nrs_of[(qs, hh)] = nrs

        def stC(i):
            qs, hh, kg, fh, lh, lq = items[i]
            gq = 4 * t + qs
            kt_lo = kg * 4
            nk = min(kt_lo + 4, gq + 1) - kt_lo
            Pbx, Pbx_r = Pb[i % 2]
            ptv, ptv_r = PTv[i % 2]
            for ii in range(nk):
                k.I("pe", lambda e: e.transpose(ptv[:, ii * 128:(ii + 1) * 128],
                                                Pbx[:, ii * 128:(ii + 1) * 128], cmb(C, "ident")),
                    R=[Pbx_r, C.cmb_r], W=[ptv_r], inc=(ii == nk - 1))
            PTx, PTx_r = PTs[i % 2]
            k.I("dve", lambda e: e.tensor_copy(out=PTx[:, 0:nk * 128], in_=ptv[:, 0:nk * 128]),
                R=[ptv_r], W=[PTx_r])

        def stE(i):
            qs, hh, kg, fh, lh, lq = items[i]
            gq = 4 * t + qs
            kt_lo = kg * 4
            nk = min(kt_lo + 4, gq + 1) - kt_lo
            PTx, PTx_r = PTs[i % 2]
            PO, PO_r = (PO0, PO1)[qs % 2]
            for ii in range(nk):
                ktt = kt_lo + ii
                k.I("pe", lambda e: e.matmul(PO[:, hh * 64:(hh + 1) * 64], PTx[:, ii * 128:(ii + 1) * 128],
                                             V[:, ktt, hh * 64:(hh + 1) * 64], start=(ktt == 0), stop=(ktt == gq)),
                    R=[PTx_r, V_r], W=[PO_r], inc=(ii == nk - 1))
            if lh:
                rsx, rsx_r = rs[hh % 4]
                nrs = nrs_of[(qs, hh)]
                k.I("dve", lambda e: e.tensor_reduce(out=rinv[:, hh:hh + 1], in_=rsx[:, 0:nrs], axis=AX.X, op=ALU.add),
                    R=[rsx_r], W=[rinv_r])
            if lq:
                q0 = qs * 128
                k.I("dve", lambda e: e.reciprocal(out=rinv[:], in_=rinv[:]), R=[rinv_r], W=[rinv_r])
                k.I("dve", lambda e: e.tensor_tensor(out=otok[:].rearrange("p (h d) -> p h d", h=8),
                                                      in0=PO[:].rearrange("p (h d) -> p h d", h=8),
                                                      in1=rinv[:].rearrange("p (h o) -> p h o", o=1).to_broadcast([128, 8, 64]),
                                                      op=ALU.mult), R=[PO_r, rinv_r], W=[otok_r])
                for c in range(4):
                    k.I("pe", lambda e: e.transpose(PTt[:, c * 128:(c + 1) * 128], otok[:, c * 128:(c + 1) * 128], cmb(C, "ident")),
                        R=[otok_r, C.cmb_r], W=[PT_r], inc=(c == 3))
                k.I("act", lambda e: e.copy(out=catT[:, 0:4, q0:q0 + 128], in_=PTt[:, 0:512].rearrange("p (c q) -> p c q", c=4)),
                    R=[PT_r], W=[catT_r])

        nit = len(items)
        for i in range(nit + 2):
            if i < nit:
                stA(i)
                stB(i)
            if 0 <= i - 1 < nit:
                stC(i - 1)
            if 0 <= i - 2 < nit:
                stE(i - 2)
        for dc in range(8):
            pp, pp_r = (PA, PB)[dc % 2]
            for kc in range(8):
                k.I("pe", lambda e: e.matmul(pp[:], wout[:, kc, dc * 128:(dc + 1) * 128], catT[:, kc, :],
                                             start=(kc == 0), stop=(kc == 7)),
                    R=[wout_r, catT_r], W=[pp_r], inc=(kc == 7))
            k.I("dve", lambda e: e.tensor_tensor(out=h[:, dc, :], in0=pp[:], in1=h[:, dc, :], op=ALU.add),
                R=[pp_r, h_r], W=[h_r])
        for c in range(8):
            k.dma("sp", h1[c, :, t0:t0 + T], h[:, c, :], R=[h_r])


def g_xattn(k, nc, C, h_in, h_out, memT_d, wq_d, wkv_d, wo_d, layer, bf=None):
    (PA, PB, PS0, PS1, PO0, PO1, PM) = C.P
    L = str(layer)
    wq, wq_r = k.sb("xwq", [128, 8, 1024], BF16)
    wo, wo_r = k.sb("xwo", [128, 8, 1024], BF16)
    wkv, wkv_r = k.sb("xwkv", [128, 8, 2048], BF16)
    memf, memf_r = k.sb("memf", [128, 8, 256], F32)
    memn, memn_r = k.sb("memn", [128, 8, 256], BF16)
    KxT, KxT_r = k.sb("KxT", [128, 8, 256], BF16)
    Vx, Vx_r = k.sb("Vx", [128, 2, 1024], BF16)
    h, h_r = k.sb("xh", [128, 8, T], F32)
    hn, hn_r = k.sb("xhn", [128, 8, T], BF16)
    sq, _ = k.sb("xsq", [128, 2, T], BF16)
    sq_r = [Res(), Res()]
    rstd, rstd_r = k.sb("xrstd", [128, T], F32)
    qx, qx_r = k.sb("qx", [128, 8, T], BF16)
    oT, oT_r = k.sb("oT", [128, 8, T], BF16)
    PTb_ = [k.sb(f"xPT{i}", [128, T], BF16) for i in range(2)]
    rden, rden_r = k.sb("rden", [128, T], F32)
    r2, r2_r = k.sb("xr2", [128, T], F32)

    if bf is None:
        load_w_bf16(k, wkv, wkv_r, wkv_d, 8, 2048)
        load_w_bf16(k, wq, wq_r, wq_d, 8, 1024)
        load_w_bf16(k, wo, wo_r, wo_d, 8, 1024)
    else:
        load_bf(k, wkv, wkv_r, bf["wkv"], 0, 2048)
        load_bf(k, wq, wq_r, bf["wq"], 0, 1024)
        load_bf(k, wo, wo_r, bf["wo"], 0, 1024)
    for c in range(8):
        k.dma("sp", memf[:, c, :], memT_d[c, :, :], W=[memf_r])
    rmsnorm_T(k, C, memf, memf_r, "mem" + L, memn, memn_r, sq, sq_r, rstd, rstd_r, PM, n=256)
    for hh in range(4):
        for cc in range(2):
            pp, pp_r = (PA, PB)[cc]
            fc = 2 * hh + cc
            for kc in range(8):
                k.I("pe", lambda e: e.matmul(pp[:, 0:256], wkv[:, kc, fc * 128:(fc + 1) * 128], memn[:, kc, :],
                                             start=(kc == 0), stop=(kc == 7)), R=[wkv_r, memn_r], W=[pp_r], inc=(kc == 7))
            k.I("act", lambda e: e.activation(out=sq[:, cc, 0:256], in_=pp[:, 0:256], func=AF.Square), R=[pp_r], W=[sq_r[cc]])
            k.I("pe", lambda e: e.matmul(PM[0][:, 0:256], cmb(C, "ones"), sq[:, cc, 0:256], start=(cc == 0), stop=(cc == 1)),
                R=[sq_r[cc], C.cmb_r], W=[PM[1]])
        k.I("act", lambda e: e.activation(out=r2[:, 0:256], in_=PM[0][:, 0:256], func=AF.Ln, bias=EPS, scale=1.0 / 256),
            R=[PM[1]], W=[r2_r])
        k.I("act", lambda e: e.activation(out=r2[:, 0:256], in_=r2[:, 0:256], func=AF.Exp, scale=-0.5), R=[r2_r], W=[r2_r])
        for cc in range(2):
            pp, pp_r = (PA, PB)[cc]
            k.I("dve", lambda e: e.scalar_tensor_tensor(out=KxT[:, 2 * hh + cc, :], in0=pp[:, 0:256], scalar=col(C, "xak" + L, cc),
                                                        in1=r2[:, 0:256], op0=ALU.mult, op1=ALU.mult),
                R=[pp_r, r2_r, C.cst_r], W=[KxT_r])
    for mt in range(2):
        for half in range(2):
            pp, pp_r = (PA, PB)[half]
            for kc in range(8):
                k.I("pe", lambda e: e.matmul(pp[:], memn[:, kc, mt * 128:(mt + 1) * 128],
                                             wkv[:, kc, 1024 + half * 512:1024 + (half + 1) * 512],
                                             start=(kc == 0), stop=(kc == 7)), R=[wkv_r, memn_r], W=[pp_r], inc=(kc == 7))
            k.I("act", lambda e: e.copy(out=Vx[:, mt, half * 512:(half + 1) * 512], in_=pp[:]), R=[pp_r], W=[Vx_r])

    hB = [(h, h_r), k.sb("xh2", [128, 8, T], F32)]
    hnB = [(hn, hn_r), k.sb("xhn2", [128, 8, T], BF16)]
    oTB = [(oT, oT_r), k.sb("oT2", [128, 8, T], BF16)]
    sqh = [[k.sb(f"xsqh{i}{c}", [128, T], BF16) for c in range(2)] for i in range(2)]
    r2B = [(r2, r2_r), k.sb("xr2b", [128, T], F32)]
    rdB = [(rden, rden_r), k.sb("rden2", [128, T], F32)]
    PTB = [PTb_, [k.sb(f"xPTb{i}", [128, T], BF16) for i in range(2)]]
    PD = (C.PT[0][:, :].bitcast(F32), C.PT[1])
    N = NT * 4

    def S0(t):
        h_, h_r_ = hB[t % 2]
        hn_, hn_r_ = hnB[t % 2]
        for c in range(8):
            k.dma("sp", h_[:, c, :], h_in[c, :, t * T:(t + 1) * T], W=[h_r_])
        rmsnorm_T(k, C, h_, h_r_, "xa" + L, hn_, hn_r_, sq, sq_r, rstd, rstd_r, PM)

    def S1(i):
        t, hh = divmod(i, 4)
        hn_, hn_r_ = hnB[t % 2]
        for cc in range(2):
            pp, pp_r = (PA, PB)[cc]
            fc = 2 * hh + cc
            s_, s_r = sqh[i % 2][cc]
            for kc in range(8):
                k.I("pe", lambda e: e.matmul(pp[:], wq[:, kc, fc * 128:(fc + 1) * 128], hn_[:, kc, :],
                                             start=(kc == 0), stop=(kc == 7)), R=[wq_r, hn_r_], W=[pp_r], inc=(kc == 7))
            k.I("act", lambda e: e.activation(out=s_[:], in_=pp[:], func=AF.Square), R=[pp_r], W=[s_r])
            k.I("pe", lambda e: e.matmul(PM[0][:], cmb(C, "ones"), s_[:], start=(cc == 0), stop=(cc == 1)),
                R=[s_r, C.cmb_r], W=[PM[1]])

    def S2(i):
        t, hh = divmod(i, 4)
        r_, r_r = r2B[i % 2]
        k.I("act", lambda e: e.activation(out=r_[:], in_=PM[0][:], func=AF.Ln, bias=EPS, scale=1.0 / 256),
            R=[PM[1]], W=[r_r])
        k.I("act", lambda e: e.activation(out=r_[:], in_=r_[:], func=AF.Exp, scale=-0.5), R=[r_r], W=[r_r])
        for cc in range(2):
            pp, pp_r = (PA, PB)[cc]
            k.I("dve", lambda e: e.scalar_tensor_tensor(out=qx[:, 2 * hh + cc, :], in0=pp[:], scalar=col(C, "xaq" + L, cc),
                                                        in1=r_[:], op0=ALU.mult, op1=ALU.mult),
                R=[pp_r, r_r, C.cst_r], W=[qx_r])

    def S3(i):
        t, hh = divmod(i, 4)
        for mt in range(2):
            ps_, ps_r = (PS0, PS1)[mt]
            pb, pb_r = PTB[i % 2][mt]
            for cc in range(2):
                k.I("pe", lambda e: e.matmul(ps_[:], KxT[:, 2 * hh + cc, mt * 128:(mt + 1) * 128], qx[:, 2 * hh + cc, :],
                                             start=(cc == 0), stop=(cc == 1)), R=[KxT_r, qx_r], W=[ps_r], inc=(cc == 1))
            k.I("act", lambda e: e.activation(out=pb[:], in_=ps_[:], func=AF.Exp, scale=1.0 / 16), R=[ps_r], W=[pb_r])

    def S4(i):
        t, hh = divmod(i, 4)
        oT_, oT_r_ = oTB[t % 2]
        rd, rd_r = rdB[i % 2]
        for mt in range(2):
            pb, pb_r = PTB[i % 2][mt]
            k.I("pe", lambda e: e.matmul(PD[0][:, 0:512], cmb(C, "ones"), pb[:], start=(mt == 0), stop=(mt == 1)),
                R=[pb_r, C.cmb_r], W=[PD[1]], inc=(mt == 1))
        k.I("act", lambda e: e.activation(out=rd[:], in_=PD[0][:, 0:512], func=AF.Ln), R=[PD[1]], W=[rd_r])
        k.I("act", lambda e: e.activation(out=rd[:], in_=rd[:], func=AF.Exp, scale=-1.0), R=[rd_r], W=[rd_r])
        for cc in range(2):
            po, po_r = (PO0, PO1)[cc]
            for mt in range(2):
                pb, pb_r = PTB[i % 2][mt]
                k.I("pe", lambda e: e.matmul(po[:], Vx[:, mt, (2 * hh + cc) * 128:(2 * hh + cc + 1) * 128], pb[:],
                                             start=(mt == 0), stop=(mt == 1)), R=[Vx_r, pb_r], W=[po_r], inc=(mt == 1))
            k.I("dve", lambda e: e.tensor_tensor(out=oT_[:, 2 * hh + cc, :], in0=po[:], in1=rd[:], op=ALU.mult),
                R=[po_r, rd_r], W=[oT_r_])
        if hh == 3:
            h_, h_r_ = hB[t % 2]
            for dc in range(8):
                pp, pp_r = (PO0, PO1)[dc % 2]
                for kc in range(8):
                    k.I("pe", lambda e: e.matmul(pp[:], wo[:, kc, dc * 128:(dc + 1) * 128], oT_[:, kc, :],
                                                 start=(kc == 0), stop=(kc == 7)), R=[wo_r, oT_r_], W=[pp_r], inc=(kc == 7))
                k.I("dve", lambda e: e.tensor_tensor(out=h_[:, dc, :], in0=pp[:], in1=h_[:, dc, :], op=ALU.add),
                    R=[pp_r, h_r_], W=[h_r_])
            for c in range(8):
                k.dma("sp", h_out[c, :, t * T:(t + 1) * T], h_[:, c, :], R=[h_r_])

    for i in range(N + 2):
        if i < N:
            if i % 4 == 0:
                S0(i // 4)
            S1(i)
            S2(i)
        if 0 <= i - 1 < N:
            S3(i - 1)
        if 0 <= i - 2 < N:
            S4(i - 2)


def g_ffn(k, nc, C, h_in, h_out, wg_d, wu_d, wd_d, bf=None):
    (PA, PB, PS0, PS1, PO0, PO1, PM) = C.P
    NF = 22
    wg, wg_r = k.sb("fwg", [128, 8, 2816], BF16)
    wu, wu_r = k.sb("fwu", [128, 8, 2816], BF16)
    wd, wd_r = k.sb("fwd", [128, NF, 1024], BF16)
    h, h_r = k.sb("fh", [128, 8, T], F32)
    hn, hn_r = k.sb("fhn", [128, 8, T], BF16)
    sq, _ = k.sb("fsq", [128, 2, T], BF16)
    sq_r = [Res(), Res()]
    rstd, rstd_r = k.sb("frstd", [128, T], F32)
    act, act_r = k.sb("fact", [128, NF, T], BF16)
    sg = [k.sb(f"fsg{i}", [128, T], BF16) for i in range(2)]
    if bf is None:
        load_w_bf16(k, wg, wg_r, wg_d, 8, 2816)
        load_w_bf16(k, wu, wu_r, wu_d, 8, 2816)
        load_w_bf16(k, wd, wd_r, wd_d, NF, 1024)
    else:
        for c0 in range(0, 2816, 704):
            k.dma("sp", wg[:, :, c0:c0 + 704], bf["wg"][0].rearrange("(c p) f -> p c f", p=128)[:, :, c0:c0 + 704],
                  R=[bf["wg"][1]], W=[wg_r])
            k.dma("sp", wu[:, :, c0:c0 + 704], bf["wu"][0].rearrange("(c p) f -> p c f", p=128)[:, :, c0:c0 + 704],
                  R=[bf["wu"][1]], W=[wu_r])
        for f0 in range(0, NF, 11):
            k.dma("sp", wd[:, f0:f0 + 11, :], bf["wd"][0].rearrange("(c p) f -> p c f", p=128)[:, f0:f0 + 11, :],
                  R=[bf["wd"][1]], W=[wd_r])
    hB = [(h, h_r), (h, h_r)]
    hnB = [(hn, hn_r), k.sb("fhn2", [128, 8, T], BF16)]
    rst = [k.sb(f"frst{i}", [128, T], F32) for i in range(2)]

    def prologue(t):
        h_, h_r_ = hB[t % 2]
        hn_, hn_r_ = hnB[t % 2]
        for c in range(8):
            k.dma("sp", h_[:, c, :], h_in[c, :, t * T:(t + 1) * T], W=[h_r_])
        rmsnorm_T(k, C, h_, h_r_, "ffn0", hn_, hn_r_, sq, sq_r, rstd, rstd_r, PM)

    prologue(0)
    for t in range(NT):
        t0 = t * T
        h_, h_r_ = hB[t % 2]
        hn_, hn_r_ = hnB[t % 2]
        for fc in range(NF):
            (pg, pg_r), (pu, pu_r) = ((PA, PB), (PS0, PS1))[fc % 2]
            for kc in range(8):
                k.I("pe", lambda e: e.matmul(pg[:], wg[:, kc, fc * 128:(fc + 1) * 128], hn_[:, kc, :],
                                             start=(kc == 0), stop=(kc == 7)), R=[wg_r, hn_r_], W=[pg_r], inc=(kc == 7))
            for kc in range(8):
                k.I("pe", lambda e: e.matmul(pu[:], wu[:, kc, fc * 128:(fc + 1) * 128], hn_[:, kc, :],
                                             start=(kc == 0), stop=(kc == 7)), R=[wu_r, hn_r_], W=[pu_r], inc=(kc == 7))
            s_, s_r = sg[fc % 2]
            k.I("act", lambda e: e.activation(out=s_[:], in_=pg[:], func=AF.Silu), R=[pg_r], W=[s_r])
            k.I("dve", lambda e: e.tensor_tensor(out=act[:, fc, :], in0=pu[:], in1=s_[:], op=ALU.mult),
                R=[pu_r, s_r], W=[act_r])
            if fc == NF // 2 and t + 1 < NT:
                prologue(t + 1)
        for dc in range(8):
            pp, pp_r = (PO0, PO1)[dc % 2]
            for fc in range(NF):
                k.I("pe", lambda e: e.matmul(pp[:], wd[:, fc, dc * 128:(dc + 1) * 128], act[:, fc, :],
                                             start=(fc == 0), stop=(fc == NF - 1)), R=[wd_r, act_r], W=[pp_r], inc=(fc == NF - 1))
            r_, r_r = rst[dc % 2]
            k.dma("sp", r_[:], h_in[dc, :, t0:t0 + T], W=[r_r])
            k.I("dve", lambda e: e.tensor_tensor(out=r_[:], in0=pp[:], in1=r_[:], op=ALU.add),
                R=[pp_r, r_r], W=[r_r])
            k.dma("sp", h_out[dc, :, t0:t0 + T], r_[:], R=[r_r])


def g_moe(k, nc, C, h_in, h_out, wr_d, wg_d, wu_d, wd_d, n_exp=8):
    (PA, PB, PS0, PS1, PO0, PO1, PM) = C.P
    SUP = min(2048, NT * T)
    NSUP = (NT * T) // SUP
    NSB = SUP // 128
    NTT = SUP // T
    hs, hs_r = k.sb("mhs", [128, 8, SUP], F32)
    hn, hn_r = k.sb("mhn", [128, 8, SUP], BF16)
    gbc, gbc_r = k.sb("mgbc", [128, 8, SUP], BF16)
    wgs = [k.sb(f"mwg{i}", [128, 8, 512], BF16) for i in range(2)]
    wus = [k.sb(f"mwu{i}", [128, 8, 512], BF16) for i in range(2)]
    wds = [k.sb(f"mwd{i}", [128, 4, 1024], BF16) for i in range(2)]
    act = [k.sb(f"mact{i}", [128, 4, T], BF16) for i in range(2)]
    sg = [k.sb(f"msg{i}", [128, T], BF16) for i in range(2)]
    sg2 = [k.sb(f"msg2{i}", [128, T], BF16) for i in range(2)]
    sq, _ = k.sb("msq", [128, 2, T], BF16)
    sq_r = [Res(), Res()]
    rstd, rstd_r = k.sb("mrstd", [128, T], F32)
    wr, wr_r = k.sb("mwr", [128, 8, 8], BF16)
    Lg, Lg_r = k.sb("mL", [128, NSB, 8], F32)
    Eg, Eg_r = k.sb("mE", [128, NSB, 8], F32)
    Sg, Sg_r = k.sb("mS", [128, NSB, 8], F32)
    m8, m8_r = k.sb("mm8", [128, NSB, 8], F32)
    rd, rd_r = k.sb("mrd", [128, NSB], F32)
    Gb, Gb_r = k.sb("mGb", [128, NSB, 8], BF16)
    Gbig, Gbig_r = k.sb("mGbig", [128, 128], BF16)
    for kc in range(8):
        k.dma("pool", wr[:, kc, :], wr_d[kc * 128:(kc + 1) * 128, :], W=[wr_r])
    wcount = 0
    for sp_ in range(NSUP):
        s0 = sp_ * SUP
        for c in range(8):
            k.dma("sp", hs[:, c, :], h_in[c, :, s0:s0 + SUP], W=[hs_r])
        for tt in range(NTT):
            hv = hs[:, :, tt * T:(tt + 1) * T]
            hnv = hn[:, :, tt * T:(tt + 1) * T]
            rmsnorm_T(k, C, hv, hs_r, "ffn1", hnv, hn_r, sq, sq_r, rstd, rstd_r, PM)
        for sb_ in range(NSB):
            for kc in range(8):
                k.I("pe", lambda e: e.matmul(PM[0][:, sb_ * 8:(sb_ + 1) * 8], hn[:, kc, sb_ * 128:(sb_ + 1) * 128], wr[:, kc, :],
                                             start=(kc == 0), stop=(kc == 7)), R=[hn_r, wr_r], W=[PM[1]],
                    inc=(kc == 7 and sb_ == NSB - 1))
        k.I("dve", lambda e: e.tensor_copy(out=Lg[:], in_=PM[0][:, 0:NSB * 8].rearrange("p (s e) -> p s e", e=8)),
            R=[PM[1]], W=[Lg_r])
        for sb_ in range(NSB):
            k.I("dve", lambda e: e.max(out=m8[:, sb_, :], in_=Lg[:, sb_, :]), R=[Lg_r], W=[m8_r])
        k.I("dve", lambda e: e.tensor_tensor(out=Eg[:], in0=Lg[:], in1=m8[:, :, 0:1].to_broadcast([128, NSB, 8]), op=ALU.subtract),
            R=[Lg_r, m8_r], W=[Eg_r])
        k.I("act", lambda e: e.activation(out=Eg[:], in_=Eg[:], func=AF.Exp), R=[Eg_r], W=[Eg_r])
        k.I("dve", lambda e: e.tensor_tensor(out=Sg[:], in0=Lg[:], in1=m8[:, :, 1:2].to_broadcast([128, NSB, 8]), op=ALU.is_ge),
            R=[Lg_r, m8_r], W=[Sg_r])
        k.I("dve", lambda e: e.tensor_tensor(out=Eg[:], in0=Eg[:], in1=Sg[:], op=ALU.mult), R=[Eg_r, Sg_r], W=[Eg_r])
        k.I("dve", lambda e: e.tensor_tensor(out=rd[:], in0=m8[:, :, 1], in1=m8[:, :, 0], op=ALU.subtract), R=[m8_r], W=[rd_r])
        k.I("act", lambda e: e.activation(out=rd[:], in_=rd[:], func=AF.Exp), R=[rd_r], W=[rd_r])
        k.I("dve", lambda e: e.tensor_scalar(out=rd[:], in0=rd[:], scalar1=1.0, scalar2=None, op0=ALU.add), R=[rd_r], W=[rd_r])
        k.I("dve", lambda e: e.reciprocal(out=rd[:], in_=rd[:]), R=[rd_r], W=[rd_r])
        k.I("dve", lambda e: e.tensor_tensor(out=Gb[:], in0=Eg[:], in1=rd[:].rearrange("p (s o) -> p s o", o=1).to_broadcast([128, NSB, 8]),
                                              op=ALU.mult), R=[Eg_r, rd_r], W=[Gb_r])
        for ex in range(n_exp):
            for sb_ in range(NSB):
                pp, pp_r = (PA, PB)[(sb_ // 4) % 2]
                k.I("pe", lambda e: e.matmul(pp[:, (sb_ % 4) * 128:(sb_ % 4 + 1) * 128],
                                             Gb[:, sb_, ex:ex + 1].to_broadcast([128, 128]), cmb(C, "ident"), start=True, stop=True),
                    R=[Gb_r, C.cmb_r], W=[pp_r], inc=(sb_ % 4 == 3))
                if sb_ % 4 == 3:
                    k.I("act", lambda e: e.copy(out=gbc[:, ex, (sb_ - 3) * 128:(sb_ + 1) * 128], in_=pp[:]), R=[pp_r], W=[gbc_r])
        pend = None

        def gu(ex, wset, tt, icnt):
            (wg_, wg_r), (wu_, wu_r), (wd_, wd_r) = wset
            a_, a_r = act[icnt % 2]
            tsl = slice(tt * T, (tt + 1) * T)
            for fc in range(4):
                (pg, pg_r), (pu, pu_r) = ((PA, PB), (PS0, PS1))[fc % 2]
                for kc in range(8):
                    k.I("pe", lambda e: e.matmul(pg[:], wg_[:, kc, fc * 128:(fc + 1) * 128], hn[:, kc, tsl],
                                                 start=(kc == 0), stop=(kc == 7)), R=[wg_r, hn_r], W=[pg_r], inc=(kc == 7))
                for kc in range(8):
                    k.I("pe", lambda e: e.matmul(pu[:], wu_[:, kc, fc * 128:(fc + 1) * 128], hn[:, kc, tsl],
                                                 start=(kc == 0), stop=(kc == 7)), R=[wu_r, hn_r], W=[pu_r], inc=(kc == 7))
                s_, s_r = sg[fc % 2]
                s2, s2_r = sg2[fc % 2]
                k.I("act", lambda e: e.activation(out=s_[:], in_=pg[:], func=AF.Silu), R=[pg_r], W=[s_r])
                k.I("dve", lambda e: e.tensor_tensor(out=s2[:], in0=s_[:], in1=gbc[:, ex, tsl], op=ALU.mult),
                    R=[s_r, gbc_r], W=[s2_r])
                k.I("dve", lambda e: e.tensor_tensor(out=a_[:, fc, :], in0=pu[:], in1=s2[:], op=ALU.mult),
                    R=[pu_r, s2_r], W=[a_r])

        def down(wset, tt, icnt):
            (wd_, wd_r) = wset[2]
            a_, a_r = act[icnt % 2]
            tsl = slice(tt * T, (tt + 1) * T)
            for dc in range(8):
                pp, pp_r = (PO0, PO1)[dc % 2]
                for fc in range(4):
                    k.I("pe", lambda e: e.matmul(pp[:], wd_[:, fc, dc * 128:(dc + 1) * 128], a_[:, fc, :],
                                                 start=(fc == 0), stop=(fc == 3)), R=[wd_r, a_r], W=[pp_r], inc=(fc == 3))
                k.I("dve", lambda e: e.tensor_tensor(out=hs[:, dc, tsl], in0=pp[:], in1=hs[:, dc, tsl], op=ALU.add),
                    R=[pp_r, hs_r], W=[hs_r])

        icnt = 0
        for ex in range(n_exp):
            for fg in range(7):
                b = wcount % 2
                wcount += 1
                wset = (wgs[b], wus[b], wds[b])
                (wg_, wg_r), (wu_, wu_r), (wd_, wd_r) = wset
                f0 = fg * 512
                for kc in range(8):
                    k.dma("pool", wg_[:, kc, :], wg_d[ex, kc * 128:(kc + 1) * 128, f0:f0 + 512], W=[wg_r])
                    k.dma("pool", wu_[:, kc, :], wu_d[ex, kc * 128:(kc + 1) * 128, f0:f0 + 512], W=[wu_r])
                for fc in range(4):
                    k.dma("pool", wd_[:, fc, :], wd_d[ex, f0 + fc * 128:f0 + (fc + 1) * 128, :], W=[wd_r])
                for tt in range(NTT):
                    gu(ex, wset, tt, icnt)
                    if pend is not None:
                        down(*pend)
                    pend = (wset, tt, icnt)
                    icnt += 1
        down(*pend)
        pend = None
        for c in range(8):
            k.dma("sp", h_out[c, :, s0:s0 + SUP], hs[:, c, :], R=[hs_r])


def cmf(C, name):
    o = CM[name]
    return C.cmf[:, o:o + 128]


def g_ssd(k, nc, C, h_in, h_out, rowc_d, w_in_d, w_out_d, bf=None):
    (PA, PB, PS0, PS1, PY, PZ, PM) = C.P
    PTt, PT_r = C.PT
    wout, wout_r = k.sb("swout", [128, 16, 1024], BF16)
    wdt, wdt_r = k.sb("swdt", [128, 8, 32], BF16)
    NWB = 2 if bf is None else 3
    wblk = [k.sb(f"swb{i}", [128, 8, 512], BF16) for i in range(NWB)]
    rowc, rowc_r = k.sb("rowc", [128, 96], F32)
    abc, abc_r = k.sb("sabc", [128, 32], F32)
    h, h_r = k.sb("sh", [128, 8, T], F32)
    hn, hn_r = k.sb("shn", [128, 8, T], BF16)
    sq, _ = k.sb("ssq", [128, 2, T], BF16)
    sq_r = [Res(), Res()]
    rstd, rstd_r = k.sb("srstd", [128, T], F32)
    xr = [k.sb(f"sxr{i}", [128, 515], F32) for i in range(2)]
    acc = [k.sb(f"sacc{i}", [128, T], F32) for i in range(2)]
    halo, halo_r = k.sb("shalo", [128, 24, 3], F32)
    xbcT, xbcT_r = k.sb("sxbcT", [128, 24, T], BF16)
    zs, zs_r = k.sb("szs", [128, 4, 2048], BF16)
    dt, dt_r = k.sb("sdt", [128, 4, 32], F32)
    da, da_r = k.sb("sda", [128, 4, 32], F32)
    xtok, xtok_r = k.sb("sxtok", [128, 2048], BF16)
    btok, btok_r = k.sb("sbtok", [128, 512], BF16)
    acum, acum_r = k.sb("sacum", [128, 64], F32)
    ea, ea_r = k.sb("sea", [128, 32], F32)
    nacum, nacum_r = k.sb("snacum", [128, 32], F32)
    te, te_r = k.sb("ste", [128, 32], F32)
    cd, cd_r = k.sb("scd", [128, 32], F32)
    seg = [k.sb(f"sseg{i}", [128, 512], F32) for i in range(2)]
    mt_ = [k.sb(f"smt{i}", [128, 512], BF16) for i in range(2)]
    cbm, cbm_r = k.sb("scbm", [128, 4, 128], F32)
    xdt, xdt_r = k.sb("sxdt", [128, 2048], BF16)
    xw, xw_r = k.sb("sxw", [128, 2048], BF16)
    state, state_r = k.sb("sstate", [128, 2048], F32)
    stb, stb_r = k.sb("sstb", [128, 2048], BF16)
    tmp = [k.sb(f"stmp{i}", [128, 512], F32) for i in range(2)]
    tmp2 = [k.sb(f"stmq{i}", [128, 512], F32) for i in range(2)]
    yz, yz_r = k.sb("syz", [128, 512], F32)
    junk, junk_r = k.sb("sjunk", [128, 512], BF16)
    ssq, ssq_r = k.sb("sssq", [128, 4], F32)
    yn, yn_r = k.sb("syn", [128, 2048], BF16)
    ynT, ynT_r = k.sb("synT", [128, 16, T], BF16)

    if bf is None:
        load_w_bf16(k, wout, wout_r, w_out_d, 16, 1024)
    else:
        load_bf(k, wout, wout_r, bf["wout"], 0, 1024)
    for kc in range(8):
        k.dma("pool", wdt[:, kc, :], w_in_d[kc * 128:(kc + 1) * 128, 5120:5152], W=[wdt_r])
    k.dma("sp", rowc[:], rowc_d[:, :], W=[rowc_r])
    k.I("act", lambda e: e.activation(out=abc[:], in_=rowc[:, 32:64], func=AF.Exp), R=[rowc_r], W=[abc_r])
    k.I("dve", lambda e: e.tensor_scalar(out=abc[:], in0=abc[:], scalar1=-1.0, scalar2=None, op0=ALU.mult), R=[abc_r], W=[abc_r])
    for kc in range(16):
        k.I("dve", lambda e: e.tensor_scalar(out=wout[:, kc, :], in0=wout[:, kc, :], scalar1=col(C, "ssdn", kc), scalar2=None,
                                              op0=ALU.mult), R=[wout_r, C.cst_r], W=[wout_r])
    k.I("pool", lambda e: e.memset(halo[:], 0.0), W=[halo_r])
    k.I("pool", lambda e: e.memset(state[:], 0.0), W=[state_r])
    k.I("pool", lambda e: e.memset(stb[:], 0.0), W=[stb_r])
    wc = 0
    hcount = 0
    for t in range(NT):
        t0 = t * T
        for c in range(8):
            k.dma("sp", h[:, c, :], h_in[c, :, t0:t0 + T], W=[h_r])
        rmsnorm_T(k, C, h, h_r, "mix1", hn, hn_r, sq, sq_r, rstd, rstd_r, PM)
        for zb in range(4):
            wb, wb_r = wblk[wc % NWB]
            wc += 1
            if bf is None:
                for kc in range(8):
                    k.dma("pool", wb[:, kc, :], w_in_d[kc * 128:(kc + 1) * 128, zb * 512:(zb + 1) * 512], W=[wb_r])
            else:
                load_bf(k, wb, wb_r, bf["win"], zb * 512, (zb + 1) * 512)
            for qs in range(4):
                pp, pp_r = (PA, PB)[qs % 2]
                for kc in range(8):
                    k.I("pe", lambda e: e.matmul(pp[:], hn[:, kc, qs * 128:(qs + 1) * 128], wb[:, kc, :],
                                                 start=(kc == 0), stop=(kc == 7)), R=[hn_r, wb_r], W=[pp_r], inc=(kc == 7))
                k.I("act", lambda e: e.activation(out=zs[:, qs, zb * 512:(zb + 1) * 512], in_=pp[:], func=AF.Silu),
                    R=[pp_r], W=[zs_r])
        for xb in range(6):
            wb, wb_r = wblk[wc % NWB]
            wc += 1
            if bf is None:
                for kc in range(8):
                    k.dma("pool", wb[:, kc, :], w_in_d[kc * 128:(kc + 1) * 128, 2048 + xb * 512:2048 + (xb + 1) * 512], W=[wb_r])
            else:
                load_bf(k, wb, wb_r, bf["win"], 2048 + xb * 512, 2048 + (xb + 1) * 512)
            for c4 in range(4):
                ch = xb * 4 + c4
                pp, pp_r = (PA, PB)[c4 % 2]
                x_, x_r = xr[c4 % 2]
                a_, a_r = acc[c4 % 2]
                for kc in range(8):
                    k.I("pe", lambda e: e.matmul(pp[:], wb[:, kc, c4 * 128:(c4 + 1) * 128], hn[:, kc, :],
                                                 start=(kc == 0), stop=(kc == 7)), R=[hn_r, wb_r], W=[pp_r], inc=(kc == 7))
                k.I("act", lambda e: e.copy(out=x_[:, 3:515], in_=pp[:]), R=[pp_r], W=[x_r])
                k.I("act", lambda e: e.copy(out=x_[:, 0:3], in_=halo[:, ch, :]), R=[halo_r], W=[x_r])
                k.I("act", lambda e: e.copy(out=halo[:, ch, :], in_=x_[:, 512:515]), R=[x_r], W=[halo_r])
                cw = CST["convw"] + 4 * ch
                k.I("dve", lambda e: e.tensor_scalar(out=a_[:], in0=x_[:, 0:512], scalar1=C.cst[:, cw:cw + 1], scalar2=None, op0=ALU.mult),
                    R=[x_r, C.cst_r], W=[a_r])
                for i in range(1, 4):
                    k.I("dve", lambda e: e.scalar_tensor_tensor(out=a_[:], in0=x_[:, i:i + 512], scalar=C.cst[:, cw + i:cw + i + 1],
                                                                in1=a_[:], op0=ALU.mult, op1=ALU.add),
                        R=[x_r, C.cst_r], W=[a_r])
                k.I("act", lambda e: e.activation(out=xbcT[:, ch, :], in_=a_[:], func=AF.Silu, bias=col(C, "convb", ch)),
                    R=[a_r, C.cst_r], W=[xbcT_r])
        for qs in range(4):
            for kc in range(8):
                k.I("pe", lambda e: e.matmul(PM[0][:, qs * 32:(qs + 1) * 32], hn[:, kc, qs * 128:(qs + 1) * 128], wdt[:, kc, :],
                                             start=(kc == 0), stop=(kc == 7)), R=[hn_r, wdt_r], W=[PM[1]],
                    inc=(kc == 7 and qs == 3))
        k.I("dve", lambda e: e.tensor_tensor(out=dt[:], in0=PM[0][:, 0:128].rearrange("p (q h) -> p q h", q=4),
                                              in1=rowc[:, 0:32].rearrange("p (o h) -> p o h", o=1).to_broadcast([128, 4, 32]),
                                              op=ALU.add), R=[PM[1], rowc_r], W=[dt_r])
        k.I("act", lambda e: e.activation(out=dt[:], in_=dt[:], func=AF.Exp), R=[dt_r], W=[dt_r])
        k.I("act", lambda e: e.activation(out=dt[:], in_=dt[:], func=AF.Ln, bias=1.0), R=[dt_r], W=[dt_r])
        k.I("dve", lambda e: e.tensor_tensor(out=da[:], in0=dt[:],
                                              in1=abc[:].rearrange("p (o h) -> p o h", o=1).to_broadcast([128, 4, 32]), op=ALU.mult),
            R=[dt_r, abc_r], W=[da_r])
        def ch_pre(qs):
            cs = slice(qs * 128, (qs + 1) * 128)
            first = (t == 0 and qs == 0)
            for half in range(2):
                for j in range(8):
                    k.I("pe", lambda e: e.transpose(PTt[:, j * 128:(j + 1) * 128], xbcT[:, half * 8 + j, cs], cmb(C, "ident")),
                        R=[xbcT_r, C.cmb_r], W=[PT_r], inc=(j == 7))
                k.I("act", lambda e: e.copy(out=xtok[:, half * 1024:(half + 1) * 1024], in_=PTt[:]), R=[PT_r], W=[xtok_r])
            for j in range(4):
                k.I("pe", lambda e: e.transpose(PTt[:, j * 128:(j + 1) * 128], xbcT[:, 16 + j, cs], cmb(C, "ident")),
                    R=[xbcT_r, C.cmb_r], W=[PT_r], inc=(j == 3))
            k.I("act", lambda e: e.copy(out=btok[:], in_=PTt[:, 0:512]), R=[PT_r], W=[btok_r])
            k.I("pe", lambda e: e.matmul(PM[0][:, 0:32], cmf(C, "upper"), da[:, qs, :], start=True, stop=True),
                R=[da_r, C.cmf_r], W=[PM[1]])
            k.I("pe", lambda e: e.matmul(PM[0][:, 32:64], cmf(C, "ones"), da[:, qs, :], start=True, stop=True),
                R=[da_r, C.cmf_r], W=[PM[1]])
            k.I("act", lambda e: e.copy(out=acum[:], in_=PM[0][:, 0:64]), R=[PM[1]], W=[acum_r])
            k.I("act", lambda e: e.activation(out=ea[:], in_=acum[:, 0:32], func=AF.Exp), R=[acum_r], W=[ea_r])
            k.I("act", lambda e: e.mul(out=nacum[:], in_=acum[:, 0:32], mul=-1.0), R=[acum_r], W=[nacum_r])
            k.I("act", lambda e: e.activation(out=cd[:], in_=acum[:, 32:64], func=AF.Exp), R=[acum_r], W=[cd_r])
            k.I("dve", lambda e: e.tensor_tensor(out=te[:], in0=acum[:, 32:64], in1=acum[:, 0:32], op=ALU.subtract),
                R=[acum_r], W=[te_r])
            k.I("act", lambda e: e.activation(out=te[:], in_=te[:], func=AF.Exp), R=[te_r], W=[te_r])
            for g in range(4):
                pp, pp_r = (PA, PB)[g % 2]
                k.I("pe", lambda e: e.matmul(pp[:, 0:128], xbcT[:, 16 + g, cs], xbcT[:, 20 + g, cs], start=True, stop=True),
                    R=[xbcT_r], W=[pp_r])
                k.I("dve", lambda e: e.tensor_tensor(out=cbm[:, g, :], in0=pp[:, 0:128], in1=cmf(C, "upper"), op=ALU.mult),
                    R=[pp_r, C.cmf_r], W=[cbm_r])
            k.I("dve", lambda e: e.tensor_tensor(out=xdt[:].rearrange("p (h d) -> p h d", h=32),
                                                  in0=xtok[:].rearrange("p (h d) -> p h d", h=32),
                                                  in1=dt[:, qs, :].rearrange("p (h o) -> p h o", o=1).to_broadcast([128, 32, 64]),
                                                  op=ALU.mult), R=[xtok_r, dt_r], W=[xdt_r])
            k.I("dve", lambda e: e.tensor_tensor(out=xw[:].rearrange("p (h d) -> p h d", h=32),
                                                  in0=xdt[:].rearrange("p (h d) -> p h d", h=32),
                                                  in1=te[:].rearrange("p (h o) -> p h o", o=1).to_broadcast([128, 32, 64]),
                                                  op=ALU.mult), R=[xdt_r, te_r], W=[xw_r])
        def ch_heads(qs):
            cs = slice(qs * 128, (qs + 1) * 128)
            PYs = [PY, PA]
            PZs = [PZ, PB]

            def s1(q):
                ps_, ps_r = (PS0, PS1)[q % 2]
                for i in range(4):
                    hh = 4 * q + i
                    k.I("pe", lambda e: e.matmul(ps_[:, i * 128:(i + 1) * 128], da[:, qs, hh:hh + 1].to_broadcast([128, 128]),
                                                 cmf(C, "upper"), start=True, stop=True), R=[da_r, C.cmf_r], W=[ps_r], inc=(i == 3))

            def s2(q):
                sg_, sg_r = seg[q % 2]
                ps_, ps_r = (PS0, PS1)[q % 2]
                k.I("dve", lambda e: e.tensor_tensor(out=sg_[:].rearrange("p (h t) -> p h t", h=4),
                                                      in0=ps_[:].rearrange("p (h t) -> p h t", h=4),
                                                      in1=acum[:, 4 * q:4 * q + 4].rearrange("p (h o) -> p h o", o=1).to_broadcast([128, 4, 128]),
                                                      op=ALU.subtract), R=[ps_r, acum_r], W=[sg_r])
                k.I("act", lambda e: e.activation(out=sg_[:], in_=sg_[:], func=AF.Relu, scale=-1.0), R=[sg_r], W=[sg_r])
                k.I("act", lambda e: e.activation(out=sg_[:], in_=sg_[:], func=AF.Exp, scale=-1.0), R=[sg_r], W=[sg_r])

            def s3(q):
                g = q // 2
                gs = slice(g * 512, (g + 1) * 512)
                sg_, sg_r = seg[q % 2]
                m_, m_r = mt_[q % 2]
                py, py_r = PYs[g % 2]
                pz, pz_r = PZs[g % 2]
                k.I("dve", lambda e: e.tensor_tensor(out=m_[:].rearrange("p (h t) -> p h t", h=4),
                                                      in0=sg_[:].rearrange("p (h t) -> p h t", h=4),
                                                      in1=cbm[:, g:g + 1, :].to_broadcast([128, 4, 128]), op=ALU.mult),
                    R=[sg_r, cbm_r], W=[m_r])
                for i in range(4):
                    hh = 4 * q + i
                    hl = hh % 8
                    k.I("pe", lambda e: e.matmul(py[:, hl * 64:(hl + 1) * 64], m_[:, i * 128:(i + 1) * 128], xdt[:, hh * 64:(hh + 1) * 64],
                                                 start=True, stop=True), R=[m_r, xdt_r], W=[py_r], inc=(i == 3))
                if q % 2 == 0:
                    return
                k.I("pe", lambda e: e.matmul(pz[:], xbcT[:, 20 + g, cs], stb[:, gs], start=True, stop=True),
                    R=[xbcT_r, stb_r], W=[pz_r])
                t1_, t1_r = tmp[g % 2]
                t2_, t2_r = tmp2[g % 2]
                k.I("dve", lambda e: e.tensor_tensor(out=t1_[:].rearrange("p (h d) -> p h d", h=8),
                                                      in0=pz[:].rearrange("p (h d) -> p h d", h=8),
                                                      in1=ea[:, g * 8:(g + 1) * 8].rearrange("p (h o) -> p h o", o=1).to_broadcast([128, 8, 64]),
                                                      op=ALU.mult), R=[pz_r, ea_r], W=[t1_r])
                k.I("dve", lambda e: e.tensor_tensor(out=t1_[:], in0=py[:], in1=t1_[:], op=ALU.add), R=[py_r, t1_r], W=[t1_r])
                k.I("dve", lambda e: e.tensor_tensor(out=t2_[:].rearrange("p (h d) -> p h d", h=8),
                                                      in0=xtok[:, gs].rearrange("p (h d) -> p h d", h=8),
                                                      in1=rowc[:, 64 + g * 8:64 + (g + 1) * 8].rearrange("p (h o) -> p h o", o=1).to_broadcast([128, 8, 64]),
                                                      op=ALU.mult), R=[xtok_r, rowc_r], W=[t2_r])
                k.I("dve", lambda e: e.tensor_tensor(out=t1_[:], in0=t1_[:], in1=t2_[:], op=ALU.add), R=[t1_r, t2_r], W=[t1_r])
                k.I("dve", lambda e: e.tensor_tensor(out=t2_[:], in0=t1_[:], in1=zs[:, qs, gs], op=ALU.mult), R=[t1_r, zs_r], W=[t2_r])
                k.I("act", lambda e: e.activation(out=junk[:], in_=t2_[:], func=AF.Square, accum_out=ssq[:, g:g + 1]),
                    R=[t2_r], W=[junk_r, ssq_r])
                k.I("act", lambda e: e.activation(out=ssq[:, g:g + 1], in_=ssq[:, g:g + 1], func=AF.Ln, bias=EPS, scale=1.0 / 512),
                    R=[ssq_r], W=[ssq_r])
                k.I("act", lambda e: e.activation(out=ssq[:, g:g + 1], in_=ssq[:, g:g + 1], func=AF.Exp, scale=-0.5),
                    R=[ssq_r], W=[ssq_r])
                k.I("act", lambda e: e.activation(out=yn[:, gs], in_=t2_[:], func=AF.Copy, scale=ssq[:, g:g + 1]),
                    R=[t2_r, ssq_r], W=[yn_r])
                k.I("pe", lambda e: e.matmul(pz[:], btok[:, g * 128:(g + 1) * 128], xw[:, gs], start=True, stop=True),
                    R=[btok_r, xw_r], W=[pz_r])
                k.I("pool", lambda e: e.tensor_tensor(out=state[:, gs].rearrange("p (h d) -> p h d", h=8),
                                                       in0=state[:, gs].rearrange("p (h d) -> p h d", h=8),
                                                       in1=cd[:, g * 8:(g + 1) * 8].rearrange("p (h o) -> p h o", o=1).to_broadcast([128, 8, 64]),
                                                       op=ALU.mult), R=[cd_r], W=[state_r])
                k.I("dve", lambda e: e.tensor_tensor(out=state[:, gs], in0=pz[:], in1=state[:, gs], op=ALU.add),
                    R=[pz_r], W=[state_r])
                k.I("act", lambda e: e.copy(out=stb[:, gs], in_=state[:, gs]), R=[state_r], W=[stb_r])

            for j in range(8 + 2):
                if j < 8:
                    s1(j)
                if 0 <= j - 1 < 8:
                    s2(j - 1)
                if 0 <= j - 2 < 8:
                    s3(j - 2)
        def ch_post(qs):
            cs = slice(qs * 128, (qs + 1) * 128)
            for half in range(2):
                for j in range(8):
                    k.I("pe", lambda e: e.transpose(PTt[:, j * 128:(j + 1) * 128], yn[:, (half * 8 + j) * 128:(half * 8 + j + 1) * 128],
                                                    cmb(C, "ident")), R=[yn_r, C.cmb_r], W=[PT_r], inc=(j == 7))
                k.I("act", lambda e: e.copy(out=ynT[:, half * 8:(half + 1) * 8, cs], in_=PTt[:].rearrange("p (c q) -> p c q", c=8)),
                    R=[PT_r], W=[ynT_r])
        ch_pre(0)
        for qs in range(4):
            ch_heads(qs)
            if qs + 1 < 4:
                ch_pre(qs + 1)
            ch_post(qs)
        for dc in range(8):
            pp, pp_r = (PA, PB)[dc % 2]
            for kc in range(16):
                k.I("pe", lambda e: e.matmul(pp[:], wout[:, kc, dc * 128:(dc + 1) * 128], ynT[:, kc, :],
                                             start=(kc == 0), stop=(kc == 15)), R=[wout_r, ynT_r], W=[pp_r], inc=(kc == 15))
            k.I("dve", lambda e: e.tensor_tensor(out=h[:, dc, :], in0=pp[:], in1=h[:, dc, :], op=ALU.add),
                R=[pp_r, h_r], W=[h_r])
        for c in range(8):
            k.dma("sp", h_out[c, :, t0:t0 + T], h[:, c, :], R=[h_r])


_CACHE = {}


def build_program():
    nc = bass.Bass("TRN2", target_bir_lowering=False)
    def din(name, shape, dt=F32):
        return nc.dram_tensor(name, list(shape), dt, kind="ExternalInput").ap()
    xT = din("xT", [8, 128, S]); memT = din("memT", [8, 128, 256]); pos = din("pos", [S], I32)
    cst_d = din("cst", [128, NCST]); cm_d = din("cm", [128, NCM]); rowc_d = din("rowc", [128, 96])
    hy_w_in = din("hy_w_in", [1024, 2048]); hy_w_out = din("hy_w_out", [1024, 1024]); pool_w = din("pool_w", [4, 128, 128])
    xa_wq = [din(f"xa_wq{l}", [1024, 1024]) for l in range(2)]
    xa_wkv = [din(f"xa_wkv{l}", [1024, 2048]) for l in range(2)]
    xa_wo = [din(f"xa_wo{l}", [1024, 1024]) for l in range(2)]
    ffn_wg = din("ffn_wg", [1024, 2816]); ffn_wu = din("ffn_wu", [1024, 2816]); ffn_wd = din("ffn_wd", [2816, 1024])
    ssd_w_in = din("ssd_w_in", [1024, 5152]); ssd_w_out = din("ssd_w_out", [2048, 1024])
    moe_wr = din("moe_wr", [1024, 8]); moe_wg = din("moe_wg", [8, 1024, 3584]); moe_wu = din("moe_wu", [8, 1024, 3584])
    moe_wd = din("moe_wd", [8, 3584, 1024])
    hs_ = [nc.dram_tensor(f"hscr{i}", [8, 128, S], F32, kind="Internal").ap() for i in range(5)]
    outT = nc.dram_tensor("outT", [8, 128, S], F32, kind="ExternalOutput").ap()
    with ExitStack() as es:
        k = KB(nc, es)
        C = setup_common(k, nc, cst_d, cm_d)
        def group(fn):
            with ExitStack() as ges:
                k.es = ges
                fn()
                k.barrier()
        BFW = {}

        def casts():
            for l in range(2):
                BFW[f"wq{l}"] = cast_w(k, nc, xa_wq[l], 1024, 1024, f"bf_wq{l}")
                BFW[f"wkv{l}"] = cast_w(k, nc, xa_wkv[l], 1024, 2048, f"bf_wkv{l}")
                BFW[f"wo{l}"] = cast_w(k, nc, xa_wo[l], 1024, 1024, f"bf_wo{l}")
                if l == 0:
                    BFW["wg"] = cast_w(k, nc, ffn_wg, 1024, 2816, "bf_wg")
                    BFW["wu"] = cast_w(k, nc, ffn_wu, 1024, 2816, "bf_wu")
                    BFW["wd"] = cast_w(k, nc, ffn_wd, 2816, 1024, "bf_wd")
                    BFW["win"] = cast_w(k, nc, ssd_w_in, 1024, 5152, "bf_win")
                    BFW["wout"] = cast_w(k, nc, ssd_w_out, 2048, 1024, "bf_wout")

        def xbf(l):
            return {"wq": BFW[f"wq{l}"], "wkv": BFW[f"wkv{l}"], "wo": BFW[f"wo{l}"]}

        group(lambda: g1_moba(k, nc, C, es, xT, hs_[0], pos, hy_w_in, hy_w_out, pool_w, after_loads=casts))
        group(lambda: g_xattn(k, nc, C, hs_[0], hs_[1], memT, xa_wq[0], xa_wkv[0], xa_wo[0], 0, bf=xbf(0)))
        group(lambda: g_ffn(k, nc, C, hs_[1], hs_[2], ffn_wg, ffn_wu, ffn_wd, bf=BFW))
        group(lambda: g_ssd(k, nc, C, hs_[2], hs_[3], rowc_d, ssd_w_in, ssd_w_out, bf=BFW))
        group(lambda: g_xattn(k, nc, C, hs_[3], hs_[4], memT, xa_wq[1], xa_wkv[1], xa_wo[1], 1, bf=xbf(1)))
        group(lambda: g_moe(k, nc, C, hs_[4], outT, moe_wr, moe_wg, moe_wu, moe_wd))
    return nc


def kernel(**inp):
    inp = {k_: np.asarray(v) for k_, v in inp.items()}
    if "nc" not in _CACHE:
        _CACHE["nc"] = build_program()
    nc = _CACHE["nc"]
    c, m = host_consts(inp)
    rowc = host_rowc(inp)
    f32 = lambda a: np.ascontiguousarray(a, dtype=np.float32)
    shared = {
        "pos": np.ascontiguousarray(inp["positions"], dtype=np.int32), "cst": c, "cm": m, "rowc": rowc,
        "hy_w_in": f32(inp["hy_w_in"][0]), "hy_w_out": f32(inp["hy_w_out"][0]), "pool_w": f32(inp["pool_w"][0]),
        "ffn_wg": f32(inp["ffn_w_gate"][0]), "ffn_wu": f32(inp["ffn_w_up"][0]), "ffn_wd": f32(inp["ffn_w_down"][0]),
        "ssd_w_in": f32(inp["ssd_w_in"][0]), "ssd_w_out": f32(inp["ssd_w_out"][0]),
        "moe_wr": f32(inp["moe_router"][0]), "moe_wg": f32(inp["moe_w_gate"][0]), "moe_wu": f32(inp["moe_w_up"][0]),
        "moe_wd": f32(inp["moe_w_down"][0]),
    }
    for l in range(2):
        shared[f"xa_wq{l}"] = f32(inp["xa_wq"][l]); shared[f"xa_wkv{l}"] = f32(inp["xa_wkv"][l]); shared[f"xa_wo{l}"] = f32(inp["xa_wo"][l])
    in_maps = []
    for b in range(8):
        mp = dict(shared)
        mp["xT"] = np.ascontiguousarray(inp["x"][b].T, dtype=np.float32).reshape(8, 128, S)
        mp["memT"] = np.ascontiguousarray(inp["mem"][b].T, dtype=np.float32).reshape(8, 128, 256)
        in_maps.append(mp)
    res = run_bass_kernel_spmd(nc, in_maps, core_ids=list(range(8)))
    out = np.empty((8, S, D), np.float32)
    for b in range(8):
        out[b] = np.asarray(res.results[b]["outT"]).reshape(D, S).T
    return out
```

```python
from concourse.bass_utils import run_bass_kernel_spmd
import numpy as np
import concourse.bass as bass
import concourse.mybir as mybir
from contextlib import ExitStack

F32 = mybir.dt.float32
BF16 = mybir.dt.bfloat16
I32 = mybir.dt.int32
AF = mybir.ActivationFunctionType
ALU = mybir.AluOpType
AX = mybir.AxisListType


class Res:
    __slots__ = ("w", "r", "name")

    def __init__(self, name=""):
        self.w = None
        self.r = {}
        self.name = name


class KB:
    RING = {"sp": 16, "pool": 12, "act": 4}

    def __init__(self, nc, es):
        self.nc = nc
        self.es = es
        self.eng = {"pe": nc.tensor, "dve": nc.vector, "act": nc.scalar,
                    "pool": nc.gpsimd, "sp": nc.sync}
        self.sems = {}
        for e in ("pe", "dve", "act", "pool"):
            self.sems[e] = es.enter_context(nc.semaphore("c_" + e))
        self.cnt = {e: 0 for e in ("pe", "dve", "act", "pool")}
        self.ring = {}
        self.ring_i = {}
        for q, n in self.RING.items():
            self.ring[q] = []
            for i in range(n):
                key = f"d_{q}{i}"
                self.sems[key] = es.enter_context(nc.semaphore(key))
                self.ring[q].append(key)
            self.ring_i[q] = 0
        self.seen = {e: {} for e in self.eng}
        self.n_inst = 0
        self.n_wait = 0
        self.out_tokens = []

    def sb(self, name, shape, dtype):
        self.uid = getattr(self, "uid", 0) + 1
        t = self.es.enter_context(self.nc.sbuf_tensor(f"s{self.uid}_" + name, list(shape), dtype))
        return t, Res(name)

    def ps(self, name, shape, dtype):
        t = self.es.enter_context(self.nc.psum_tensor("p_" + name, list(shape), dtype))
        return t, Res(name)

    def _waits(self, e, R, W):
        need = {}
        for r in R:
            if r.w is not None:
                k, v = r.w
                if need.get(k, 0) < v:
                    need[k] = v
        for w in W:
            if w.w is not None:
                k, v = w.w
                if need.get(k, 0) < v:
                    need[k] = v
            for k, v in w.r.items():
                if need.get(k, 0) < v:
                    need[k] = v
        seen = self.seen[e]
        eng = self.eng[e]
        for k, v in need.items():
            if e == "pe" and k == "pe":
                continue
            if seen.get(k, 0) >= v:
                continue
            eng.wait_ge(self.sems[k], v)
            seen[k] = v
            self.n_wait += 1

    def _commit(self, tok, R, W):
        k, v = tok
        for r in R:
            if r.r.get(k, 0) < v:
                r.r[k] = v
        for w in W:
            w.w = tok
            w.r = {}

    def I(self, e, fn, R=(), W=(), inc=True):
        self._waits(e, R, W)
        ins = fn(self.eng[e])
        self.n_inst += 1
        if inc:
            self.cnt[e] += 1
            ins.then_inc(self.sems[e], 1)
            tok = (e, self.cnt[e])
        else:
            tok = (e, self.cnt[e] + 1)
        self._commit(tok, R, W)
        return tok

    def dma(self, q, out, in_, R=(), W=(), **kw):
        e = q
        i = self.ring_i[q]
        n = len(self.ring[q])
        key = self.ring[q][i % n]
        prev = 16 * (i // n)
        eng = self.eng[e]
        if prev > 0 and self.seen[e].get(key, 0) < prev:
            eng.wait_ge(self.sems[key], prev)
            self.seen[e][key] = prev
        self._waits(e, R, W)
        ins = eng.dma_start(out=out, in_=in_, **kw)
        ins.then_inc(self.sems[key], 16)
        self.ring_i[q] = i + 1
        self.n_inst += 1
        tok = (key, prev + 16)
        self._commit(tok, R, W)
        return tok

    def finish(self, toks):
        eng = self.eng["sp"]
        for k, v in toks:
            eng.wait_ge(self.sems[k], v)


def kb_barrier(self):
    for e, eng in self.eng.items():
        for x in ("pe", "dve", "act", "pool"):
            v = self.cnt[x]
            if v > 0 and self.seen[e].get(x, 0) < v and not (e == x):
                eng.wait_ge(self.sems[x], v)
                self.seen[e][x] = v
        for q in self.ring:
            n = len(self.ring[q])
            tot = self.ring_i[q]
            for j in range(min(n, tot)):
                idx = tot - 1 - j
                key = self.ring[q][idx % n]
                v = 16 * (idx // n + 1)
                if self.seen[e].get(key, 0) < v:
                    eng.wait_ge(self.sems[key], v)
                    self.seen[e][key] = v


KB.barrier = kb_barrier


import numpy as np

S = 4096
D = 1024
T = 512
NT = S // T
EPS = 1e-6
NEG = -30000.0

CST = {}
_c = 0
def _col(name, n):
    global _c
    CST[name] = _c
    _c += n
for _n in ("mix0", "mix1", "xa0", "xa1", "mem0", "mem1", "ffn0", "ffn1"):
    _col(_n, 8)
_col("hyq", 1); _col("hyk", 1); _col("pscale", 4)
_col("xaq0", 2); _col("xaq1", 2); _col("xak0", 2); _col("xak1", 2)
_col("invf", 1)
_col("convw", 96); _col("convb", 24); _col("ssdn", 16)
_col("rcnt", 64)
NCST = _c

CM = {"ident": 0, "ones": 128, "bones": 256, "rot": 384, "tri01": 512, "upper": 640}
NCM = 768


def host_consts(inp):
    c = np.zeros((128, NCST), np.float32)
    def put(name, v):
        v = np.asarray(v, np.float32).reshape(-1, 128).T
        c[:, CST[name]:CST[name] + v.shape[1]] = v
    for l in range(2):
        put(f"mix{l}", inp["mix_norm"][l]); put(f"xa{l}", inp["xa_norm"][l])
        put(f"mem{l}", inp["mem_norm"][l]); put(f"ffn{l}", inp["ffn_norm"][l])
        put(f"xaq{l}", inp["xa_q_norm"][l]); put(f"xak{l}", inp["xa_k_norm"][l])
    put("hyq", np.tile(inp["hy_q_norm"][0], 2)); put("hyk", np.tile(inp["hy_k_norm"][0], 2))
    put("pscale", inp["pool_scale"][0])
    invf = (10000.0 ** (-np.arange(32, dtype=np.float32) / 32)).astype(np.float32)
    put("invf", np.tile(invf, 4))
    cw = inp["ssd_conv_w"][0]
    cwl = np.zeros((128, 96), np.float32)
    for ch in range(24):
        for i in range(4):
            cwl[:, 4 * ch + i] = cw[i, ch * 128:(ch + 1) * 128]
    c[:, CST["convw"]:CST["convw"] + 96] = cwl
    put("convb", inp["ssd_conv_b"][0]); put("ssdn", inp["ssd_norm"][0])
    rc = np.zeros((128, 64), np.float32)
    for g, w in enumerate((2, 4, 8, 16)):
        rc[:, g * 16:(g + 1) * 16] = 1.0 / np.minimum(np.arange(16) + 1, w)
    c[:, CST["rcnt"]:CST["rcnt"] + 64] = rc
    m = np.zeros((128, NCM), np.float32)
    m[:, 0:128] = np.eye(128)
    m[:, 128:256] = 1.0
    for p in range(128):
        for q in range(128):
            if p // 64 == q // 64:
                m[p, 256 + q] = 1.0
    for p in range(128):
        if p % 64 < 32:
            m[p + 32, 384 + p] = -1.0
        else:
            m[p - 32, 384 + p] = 1.0
    qi = np.arange(128)[:, None]; ki = np.arange(128)[None, :]
    m[:, 512:640] = (ki <= qi).astype(np.float32)
    m[:, 640:768] = (qi <= ki).astype(np.float32)
    return c, m


def host_rowc(inp):
    r = np.zeros((128, 96), np.float32)
    r[:, 0:32] = inp["ssd_dt_bias"][0][None, :]
    r[:, 32:64] = inp["ssd_a_log"][0][None, :]
    r[:, 64:96] = inp["ssd_d"][0][None, :]
    return r


class Ctx:
    pass


def setup_common(k, nc, cst_d, cm_d):
    C = Ctx()
    C.cst, C.cst_r = k.sb("cst", [128, NCST], F32)
    C.cmf, C.cmf_r = k.sb("cmf", [128, NCM], F32)
    C.cmb, C.cmb_r = k.sb("cmb", [128, NCM], BF16)
    C.trib, C.trib_r = k.sb("trib", [128, 128], F32)
    k.dma("sp", C.cst[:], cst_d[:, :], W=[C.cst_r])
    k.dma("sp", C.cmf[:], cm_d[:, :], W=[C.cmf_r])
    k.I("dve", lambda e: e.tensor_copy(out=C.cmb[:], in_=C.cmf[:]), R=[C.cmf_r], W=[C.cmb_r])
    k.I("dve", lambda e: e.tensor_scalar(out=C.trib[:], in0=C.cmf[:, 512:640], scalar1=-1.0, scalar2=-NEG,
                                          op0=ALU.add, op1=ALU.mult), R=[C.cmf_r], W=[C.trib_r])
    C.P = []
    for i in range(7):
        C.P.append(k.ps(f"P{i}", [128, 512], F32))
    C.PT = k.ps("PTb", [128, 1024], BF16)
    return C


def col(C, name, i=0):
    c = CST[name] + i
    return C.cst[:, c:c + 1]


def cmb(C, name, rows=128):
    o = CM[name]
    return C.cmb[0:rows, o:o + 128]


def load_w_bf16(k, dst, dst_r, src, nk, ncols, q="pool"):
    for c in range(nk):
        for c0 in range(0, ncols, 1024):
            c1 = min(ncols, c0 + 1024)
            k.dma(q, dst[:, c, c0:c1], src[c * 128:(c + 1) * 128, c0:c1], W=[dst_r])


def load_w_staged(k, dst, dst_r, src, nk, ncols, stg, cnt=[0]):
    for c0 in range(0, ncols, 512):
        c1 = min(ncols, c0 + 512)
        for c in range(nk):
            st, st_r = stg[cnt[0] % len(stg)]
            k.dma("sp", st[:, 0:c1 - c0], src[c * 128:(c + 1) * 128, c0:c1], W=[st_r])
            if cnt[0] % 2 == 0:
                k.I("dve", lambda e: e.tensor_copy(out=dst[:, c, c0:c1], in_=st[:, 0:c1 - c0]), R=[st_r], W=[dst_r])
            else:
                k.I("act", lambda e: e.copy(out=dst[:, c, c0:c1], in_=st[:, 0:c1 - c0]), R=[st_r], W=[dst_r])
            cnt[0] += 1


def cast_w(k, nc, src, rows, cols, name):
    dst = nc.dram_tensor(name, [rows, cols], BF16, kind="Internal").ap()
    r = Res(name)
    for r0 in range(0, rows, 128):
        for c0 in range(0, cols, 1024):
            c1 = min(cols, c0 + 1024)
            k.dma("pool", dst[r0:r0 + 128, c0:c1], src[r0:r0 + 128, c0:c1], W=[r])
    return dst, r


def load_bf(k, dst, dst_r, bf, c0, c1, nk=None):
    src, src_r = bf
    v = src.rearrange("(c p) f -> p c f", p=128)
    if nk is not None:
        v = v[:, 0:nk, :]
    k.dma("sp", dst[:, :, 0:c1 - c0], v[:, :, c0:c1], R=[src_r], W=[dst_r])


def rmsnorm_T(k, C, h, h_r, gname, hn, hn_r, sq, sq_r, rstd, rstd_r, PM, n=T):
    pm, pm_r = PM
    ns = len(sq_r)
    for c in range(8):
        k.I("act", lambda e: e.activation(out=sq[:, c % ns, 0:n], in_=h[:, c, 0:n], func=AF.Square),
            R=[h_r], W=[sq_r[c % ns]])
        k.I("pe", lambda e: e.matmul(pm[:, 0:n], cmb(C, "ones"), sq[:, c % ns, 0:n], start=(c == 0), stop=(c == 7)),
            R=[sq_r[c % ns], C.cmb_r], W=[pm_r])
    k.I("act", lambda e: e.activation(out=rstd[:, 0:n], in_=pm[:, 0:n], func=AF.Ln, bias=EPS, scale=1.0 / D),
        R=[pm_r], W=[rstd_r])
    k.I("act", lambda e: e.activation(out=rstd[:, 0:n], in_=rstd[:, 0:n], func=AF.Exp, scale=-0.5), R=[rstd_r], W=[rstd_r])
    for c in range(8):
        k.I("dve", lambda e: e.scalar_tensor_tensor(out=hn[:, c, 0:n], in0=h[:, c, 0:n], scalar=col(C, gname, c),
                                                    in1=rstd[:, 0:n], op0=ALU.mult, op1=ALU.mult),
            R=[h_r, rstd_r, C.cst_r], W=[hn_r])


def g1_moba(k, nc, C, es, xT, h1, positions, w_in_d, w_out_d, pool_w_d, after_loads=None):
    (PA, PB, PS0, PS1, PO0, PO1, PM) = C.P
    PTt, PT_r = C.PT
    win, win_r = k.sb("win", [128, 8, 2048], BF16)
    wout, wout_r = k.sb("wout", [128, 8, 1024], BF16)
    poolw, poolw_r = k.sb("poolw", [128, 4, 128], BF16)
    KT, KT_r = k.sb("KT", [128, 4, S], BF16)
    V, V_r = k.sb("V", [128, 32, 512], BF16)
    kmT, kmT_r = k.sb("kmT", [128, 4, 32], BF16)
    h, h_r = k.sb("h", [128, 8, T], F32)
    hn, hn_r = k.sb("hn", [128, 8, T], BF16)
    sq, _ = k.sb("sq", [128, 2, T], BF16)
    sq_r = [Res() for _ in range(2)]
    rstd, rstd_r = k.sb("rstd", [128, T], F32)
    qT, qT_r = k.sb("qT", [128, 4, T], BF16)
    catT, catT_r = k.sb("catT", [128, 8, T], BF16)
    qTz, qTz_r = k.sb("qTz", [128, 8, T], BF16)
    cos, cos_r = k.sb("cos", [128, T], F32)
    sin, sin_r = k.sb("sin", [128, T], F32)
    posi, posi_r = k.sb("posi", [128, T], I32)
    wk = [k.sb(f"wk{i}", [128, 528], F32) for i in range(6)]
    qnb, qnb_r = k.sb("qnb", [128, T], BF16)
    sqb, sqb_r = k.sb("sqb", [128, T], BF16)
    uext = [k.sb(f"uext{g}", [128, 528], F32) for g in range(4)]
    pooled, pooled_r = k.sb("pooled", [128, T], BF16)
    gsb, gsb_r = k.sb("gsb", [128, 8, 16], F32)
    biasq4, _ = k.sb("biasq4", [128, 4, 8, 16], F32)
    biasq4_r = [Res() for _ in range(4)]
    PMq_r = [Res() for _ in range(4)]
    PTv = [(PTt, PT_r), (PA[0][:, :].bitcast(BF16), PA[1])]
    m8, m8_r = k.sb("m8", [128, 8, 8], F32)
    Pb = [k.sb(f"Pb{i}", [128, T], BF16) for i in range(2)]
    PTs = [k.sb(f"PTs{i}", [128, T], BF16) for i in range(2)]
    rs = [k.sb(f"rs{i}", [128, 32], F32) for i in range(4)]
    rinv, rinv_r = k.sb("rinv", [128, 8], F32)
    otok, otok_r = k.sb("otok", [128, 512], BF16)
    dsm, dsm_r = k.sb("dsm", [128, 128], F32)

    for g in range(4):
        k.I("dve", lambda e: e.memset(uext[g][0][:, 0:16], 0.0), W=[uext[g][1]])
    k.I("dve", lambda e: e.memset(kmT[:], 0.0), W=[kmT_r])
    k.I("dve", lambda e: e.memset(qTz[:], 0.0), W=[qTz_r])
    stg = wk[0:4]
    load_w_staged(k, win, win_r, w_in_d, 8, 2048, stg)
    load_w_staged(k, wout, wout_r, w_out_d, 8, 1024, stg)
    for g in range(4):
        k.dma("pool", poolw[:, g, :], pool_w_d[g, :, :], W=[poolw_r])
    if after_loads is not None:
        after_loads()

    twopi = float(2 * np.pi)
    pcount = 0
    for t in range(NT):
        t0 = t * T
        for c in range(8):
            k.dma("sp", h[:, c, :], xT[c, :, t0:t0 + T], W=[h_r])
        k.dma("sp", posi[:], positions[t0:t0 + T].partition_broadcast(128), W=[posi_r])
        (a0, a0r), (a1, a1r), (a2, a2r) = wk[0], wk[1], wk[2]
        k.I("dve", lambda e: e.tensor_copy(out=a0[:, 0:T], in_=posi[:]), R=[posi_r], W=[a0r])
        k.I("dve", lambda e: e.tensor_scalar(out=a0[:, 0:T], in0=a0[:, 0:T], scalar1=col(C, "invf"), scalar2=1.0 / twopi,
                                              op0=ALU.mult, op1=ALU.mult), R=[a0r, C.cst_r], W=[a0r])
        for (dst, dst_r, off) in ((sin, sin_r, 0.5), (cos, cos_r, 0.75)):
            k.I("dve", lambda e: e.tensor_scalar(out=a1[:, 0:T], in0=a0[:, 0:T], scalar1=off, scalar2=None, op0=ALU.add),
                R=[a0r], W=[a1r])
            k.I("dve", lambda e: e.tensor_copy(out=posi[:], in_=a1[:, 0:T]), R=[a1r], W=[posi_r])
            k.I("dve", lambda e: e.tensor_copy(out=a2[:, 0:T], in_=posi[:]), R=[posi_r], W=[a2r])
            k.I("dve", lambda e: e.tensor_tensor(out=a1[:, 0:T], in0=a1[:, 0:T], in1=a2[:, 0:T], op=ALU.subtract),
                R=[a1r, a2r], W=[a1r])
            k.I("dve", lambda e: e.tensor_scalar(out=a2[:, 0:T], in0=a1[:, 0:T], scalar1=0.0, scalar2=None, op0=ALU.is_lt),
                R=[a1r], W=[a2r])
            k.I("dve", lambda e: e.tensor_tensor(out=a1[:, 0:T], in0=a1[:, 0:T], in1=a2[:, 0:T], op=ALU.add),
                R=[a1r, a2r], W=[a1r])
            k.I("act", lambda e: e.activation(out=dst[:], in_=a1[:, 0:T], func=AF.Sin, bias=-float(np.pi), scale=twopi),
                R=[a1r], W=[dst_r])
        rmsnorm_T(k, C, h, h_r, "mix0", hn, hn_r, sq, sq_r, rstd, rstd_r, PM)
        for c in range(8):
            pp, pp_r = (PA, PB)[c % 2]
            for kc in range(8):
                k.I("pe", lambda e: e.matmul(pp[:], win[:, kc, c * 128:(c + 1) * 128], hn[:, kc, :],
                                             start=(kc == 0), stop=(kc == 7)),
                    R=[win_r, hn_r], W=[pp_r], inc=(kc == 7))
            isq = c < 4
            gname = "hyq" if isq else "hyk"
            (qn, qn_r), (t1, t1_r), (t2, t2_r), (r2, r2_r) = wk[0], wk[1], wk[2], wk[3]
            k.I("act", lambda e: e.activation(out=sqb[:], in_=pp[:], func=AF.Square), R=[pp_r], W=[sqb_r])
            k.I("act", lambda e: e.activation(out=qn[:, 0:T], in_=pp[:], func=AF.Copy, scale=col(C, gname)),
                R=[pp_r, C.cst_r], W=[qn_r])
            k.I("pe", lambda e: e.matmul(PM[0][:], cmb(C, "bones"), sqb[:], start=True, stop=True),
                R=[sqb_r, C.cmb_r], W=[PM[1]])
            k.I("act", lambda e: e.activation(out=r2[:, 0:T], in_=PM[0][:], func=AF.Ln, bias=EPS, scale=1.0 / 64),
                R=[PM[1]], W=[r2_r])
            k.I("act", lambda e: e.activation(out=r2[:, 0:T], in_=r2[:, 0:T], func=AF.Exp, scale=-0.5), R=[r2_r], W=[r2_r])
            k.I("dve", lambda e: e.tensor_copy(out=qnb[:], in_=qn[:, 0:T]), R=[qn_r], W=[qnb_r])
            k.I("pe", lambda e: e.matmul(PM[0][:], cmb(C, "rot"), qnb[:], start=True, stop=True),
                R=[qnb_r, C.cmb_r], W=[PM[1]])
            k.I("dve", lambda e: e.tensor_tensor(out=t2[:, 0:T], in0=PM[0][:], in1=sin[:], op=ALU.mult),
                R=[PM[1], sin_r], W=[t2_r])
            k.I("dve", lambda e: e.tensor_tensor(out=t1[:, 0:T], in0=qn[:, 0:T], in1=cos[:], op=ALU.mult),
                R=[qn_r, cos_r], W=[t1_r])
            k.I("dve", lambda e: e.tensor_tensor(out=t1[:, 0:T], in0=t1[:, 0:T], in1=t2[:, 0:T], op=ALU.add),
                R=[t1_r, t2_r], W=[t1_r])
            if isq:
                k.I("dve", lambda e: e.tensor_tensor(out=qT[:, c, :], in0=t1[:, 0:T], in1=r2[:, 0:T], op=ALU.mult),
                    R=[t1_r, r2_r], W=[qT_r])
                k.I("act", lambda e: e.copy(out=qTz[0:64, 2 * c, :], in_=qT[0:64, c, :]), R=[qT_r], W=[qTz_r])
                k.I("act", lambda e: e.copy(out=qTz[64:128, 2 * c + 1, :], in_=qT[64:128, c, :]), R=[qT_r], W=[qTz_r])
            else:
                kc4 = c - 4
                k.I("dve", lambda e: e.tensor_tensor(out=t2[:, 0:T], in0=t1[:, 0:T], in1=r2[:, 0:T], op=ALU.mult),
                    R=[t1_r, r2_r], W=[t2_r])
                k.I("act", lambda e: e.copy(out=KT[:, kc4, t0:t0 + T], in_=t2[:, 0:T]), R=[t2_r], W=[KT_r])
                k.I("dve", lambda e: e.tensor_reduce(out=r2[:, 0:2], in_=t2[:, 0:T].rearrange("p (b x) -> p b x", b=2),
                                                      axis=AX.X, op=ALU.add), R=[t2_r], W=[r2_r])
                k.I("act", lambda e: e.mul(out=kmT[0:64, kc4, 2 * t:2 * t + 2], in_=r2[0:64, 0:2], mul=1.0 / 256),
                    R=[r2_r], W=[kmT_r])
                k.I("act", lambda e: e.mul(out=kmT[64:128, kc4, 16 + 2 * t:16 + 2 * t + 2], in_=r2[64:128, 0:2], mul=1.0 / 256),
                    R=[r2_r], W=[kmT_r])
        for qs in range(4):
            pp, pp_r = (PA, PB)[qs % 2]
            for kc in range(8):
                k.I("pe", lambda e: e.matmul(pp[:], hn[:, kc, qs * 128:(qs + 1) * 128], win[:, kc, 1024:1536],
                                             start=(kc == 0), stop=(kc == 7)),
                    R=[win_r, hn_r], W=[pp_r], inc=(kc == 7))
            k.I("act", lambda e: e.copy(out=V[:, 4 * t + qs, :], in_=pp[:]), R=[pp_r], W=[V_r])
        for g in range(4):
            w = 2 << g
            pp, pp_r = (PA, PB)[g % 2]
            ue, ue_r = uext[g]
            for kc in range(8):
                k.I("pe", lambda e: e.matmul(pp[:], win[:, kc, (12 + g) * 128:(13 + g) * 128], hn[:, kc, :],
                                             start=(kc == 0), stop=(kc == 7)),
                    R=[win_r, hn_r], W=[pp_r], inc=(kc == 7))
            k.I("act", lambda e: e.copy(out=ue[:, 16:528], in_=pp[:]), R=[pp_r], W=[ue_r])
            cur, cur_r = ue, ue_r
            step = 1
            bufs = [wk[4], wk[5]]
            bi = 0
            lo = 0
            while step < w:
                lo += step
                nxt, nxt_r = bufs[bi]
                bi ^= 1
                k.I("dve", lambda e: e.tensor_tensor(out=nxt[:, lo:528], in0=cur[:, lo:528], in1=cur[:, lo - step:528 - step],
                                                       op=ALU.add), R=[cur_r], W=[nxt_r])
                cur, cur_r = nxt, nxt_r
                step *= 2
            (pf, pf_r) = wk[3]
            k.I("dve", lambda e: e.scalar_tensor_tensor(out=pf[:, 0:T], in0=cur[:, 16:528], scalar=1.0 / w, in1=ue[:, 16:528],
                                                        op0=ALU.mult, op1=ALU.subtract), R=[cur_r, ue_r], W=[pf_r])
            if t == 0:
                rc = C.cst[:, CST["rcnt"] + 16 * g: CST["rcnt"] + 16 * g + 16]
                k.I("dve", lambda e: e.tensor_tensor(out=pf[:, 0:16], in0=cur[:, 16:32], in1=rc, op=ALU.mult),
                    R=[cur_r, C.cst_r], W=[pf_r])
                k.I("dve", lambda e: e.tensor_tensor(out=pf[:, 0:16], in0=pf[:, 0:16], in1=ue[:, 16:32], op=ALU.subtract),
                    R=[ue_r], W=[pf_r])
            k.I("act", lambda e: e.copy(out=pooled[:], in_=pf[:, 0:T]), R=[pf_r], W=[pooled_r])
            k.I("act", lambda e: e.copy(out=ue[:, 0:16], in_=ue[:, 512:528]), R=[cur_r], W=[ue_r])
            k.I("pe", lambda e: e.matmul(PM[0][:], poolw[:, g, :], pooled[:], start=True, stop=True),
                R=[poolw_r, pooled_r], W=[PM[1]])
            k.I("act", lambda e: e.activation(out=catT[:, 4 + g, :], in_=PM[0][:], func=AF.Copy, scale=col(C, "pscale", g)),
                R=[PM[1], C.cst_r], W=[catT_r])
        for q_ in range(4):
            PMq_r[q_].w = PM[1].w
            PMq_r[q_].r = dict(PM[1].r)
        for qs in range(4):
            gq = 4 * t + qs
            own = gq // 2
            q0 = qs * 128
            bq = biasq4[:, qs]
            if own > 0:
                for c4 in range(4):
                    k.I("pe", lambda e: e.matmul(PM[0][:, qs * 128 + c4 * 32:qs * 128 + (c4 + 1) * 32], qT[:, c4, q0:q0 + 128],
                                                 kmT[:, c4, :], start=True, stop=True),
                        R=[qT_r, kmT_r], W=[PM[1]], inc=(c4 == 3))
                k.I("dve", lambda e: e.memset(gsb[:], -1e30), W=[gsb_r])
                k.I("dve", lambda e: e.tensor_copy(out=gsb[:, :, 0:own],
                                                    in_=PM[0][:, qs * 128:(qs + 1) * 128].rearrange("p (h j) -> p h j", h=8)[:, :, 0:own]),
                    R=[PM[1]], W=[gsb_r])
                for hh in range(8):
                    k.I("dve", lambda e: e.max(out=m8[:, hh, :], in_=gsb[:, hh, :]), R=[gsb_r], W=[m8_r])
                k.I("dve", lambda e: e.tensor_tensor(out=bq[:, :, 0:own], in0=gsb[:, :, 0:own],
                                                      in1=m8[:, :, 2:3].to_broadcast([128, 8, own]), op=ALU.is_lt),
                    R=[gsb_r, m8_r], W=[biasq4_r[qs]])
                k.I("dve", lambda e: e.tensor_scalar(out=bq[:, :, 0:own], in0=bq[:, :, 0:own], scalar1=NEG, scalar2=None,
                                                      op0=ALU.mult), R=[biasq4_r[qs]], W=[biasq4_r[qs]])
            k.I("dve", lambda e: e.memset(bq[:, :, own:own + 1], 0.0), W=[biasq4_r[qs]])
        items = []
        for qs in range(4):
            gq = 4 * t + qs
            ngrp = gq // 4 + 1
            for hh in range(8):
                for kg in range(ngrp):
                    items.append((qs, hh, kg, kg == 0, kg == ngrp - 1, hh == 7 and kg == ngrp - 1))
        nrs_of = {}

        def stA(i):
            qs, hh, kg, fh, lh, lq = items[i]
            gq = 4 * t + qs
            kt_lo = kg * 4
            kt_hi = min(kt_lo + 4, gq + 1)
            nk = kt_hi - kt_lo
            PSx, PSx_r = (PS0, PS1)[i % 2]
            k.I("pe", lambda e: e.matmul(PSx[:, 0:nk * 128], qTz[:, hh, qs * 128:(qs + 1) * 128],
                                         KT[:, hh // 2, kt_lo * 128:kt_hi * 128], start=True, stop=True),
                R=[qTz_r, KT_r], W=[PSx_r])

        def stB(i):
            qs, hh, kg, fh, lh, lq = items[i]
            gq = 4 * t + qs
            kt_lo = kg * 4
            kt_hi = min(kt_lo + 4, gq + 1)
            PSx, PSx_r = (PS0, PS1)[i % 2]
            Pbx, Pbx_r = Pb[i % 2]
            rsx, rsx_r = rs[hh % 4]
            nrs = 0 if fh else nrs_of[(qs, hh)]
            kt = kt_lo
            while kt < kt_hi:
                j = kt // 2
                c0 = (kt - kt_lo) * 128
                if kt == gq:
                    k.I("act", lambda e: e.activation(out=Pbx[:, c0:c0 + 128], in_=PSx[:, c0:c0 + 128], func=AF.Exp, scale=0.125),
                        R=[PSx_r], W=[Pbx_r])
                    k.I("dve", lambda e: e.scalar_tensor_tensor(out=Pbx[:, c0:c0 + 128], in0=Pbx[:, c0:c0 + 128], scalar=1.0,
                                                                in1=cmb(C, "tri01"), op0=ALU.mult, op1=ALU.mult,
                                                                accum_out=rsx[:, nrs:nrs + 1]),
                        R=[C.cmb_r], W=[Pbx_r, rsx_r])
                    nrs += 1
                    kt += 1
                else:
                    n = 2 if (kt % 2 == 0 and kt + 1 < kt_hi and kt + 1 != gq) else 1
                    k.I("act", lambda e: e.activation(out=Pbx[:, c0:c0 + n * 128], in_=PSx[:, c0:c0 + n * 128],
                                                      func=AF.Exp, scale=0.125, bias=biasq4[:, qs, hh, j:j + 1],
                                                      accum_out=rsx[:, nrs:nrs + 1]),
                        R=[PSx_r, biasq4_r[qs]], W=[Pbx_r, rsx_r])
                    nrs += 1
                    kt += n
            nrs_of[(qs, hh)] = nrs

        def stC(i):
            qs, hh, kg, fh, lh, lq = items[i]
            gq = 4 * t + qs
            kt_lo = kg * 4
            nk = min(kt_lo + 4, gq + 1) - kt_lo
            Pbx, Pbx_r = Pb[i % 2]
            ptv, ptv_r = PTv[i % 2]
            for ii in range(nk):
                k.I("pe", lambda e: e.transpose(ptv[:, ii * 128:(ii + 1) * 128],
                                                Pbx[:, ii * 128:(ii + 1) * 128], cmb(C, "ident")),
                    R=[Pbx_r, C.cmb_r], W=[ptv_r], inc=(ii == nk - 1))
            PTx, PTx_r = PTs[i % 2]
            k.I("dve", lambda e: e.tensor_copy(out=PTx[:, 0:nk * 128], in_=ptv[:, 0:nk * 128]),
                R=[ptv_r], W=[PTx_r])

        def stE(i):
            qs, hh, kg, fh, lh, lq = items[i]
            gq = 4 * t + qs
            kt_lo = kg * 4
            nk = min(kt_lo + 4, gq + 1) - kt_lo
            PTx, PTx_r = PTs[i % 2]
            PO, PO_r = (PO0, PO1)[qs % 2]
            for ii in range(nk):
                ktt = kt_lo + ii
                k.I("pe", lambda e: e.matmul(PO[:, hh * 64:(hh + 1) * 64], PTx[:, ii * 128:(ii + 1) * 128],
                                             V[:, ktt, hh * 64:(hh + 1) * 64], start=(ktt == 0), stop=(ktt == gq)),
                    R=[PTx_r, V_r], W=[PO_r], inc=(ii == nk - 1))
            if lh:
                rsx, rsx_r = rs[hh % 4]
                nrs = nrs_of[(qs, hh)]
                k.I("dve", lambda e: e.tensor_reduce(out=rinv[:, hh:hh + 1], in_=rsx[:, 0:nrs], axis=AX.X, op=ALU.add),
                    R=[rsx_r], W=[rinv_r])
            if lq:
                q0 = qs * 128
                k.I("dve", lambda e: e.reciprocal(out=rinv[:], in_=rinv[:]), R=[rinv_r], W=[rinv_r])
                k.I("dve", lambda e: e.tensor_tensor(out=otok[:].rearrange("p (h d) -> p h d", h=8),
                                                      in0=PO[:].rearrange("p (h d) -> p h d", h=8),
                                                      in1=rinv[:].rearrange("p (h o) -> p h o", o=1).to_broadcast([128, 8, 64]),
                                                      op=ALU.mult), R=[PO_r, rinv_r], W=[otok_r])
                for c in range(4):
                    k.I("pe", lambda e: e.transpose(PTt[:, c * 128:(c + 1) * 128], otok[:, c * 128:(c + 1) * 128], cmb(C, "ident")),
                        R=[otok_r, C.cmb_r], W=[PT_r], inc=(c == 3))
                k.I("act", lambda e: e.copy(out=catT[:, 0:4, q0:q0 + 128], in_=PTt[:, 0:512].rearrange("p (c q) -> p c q", c=4)),
                    R=[PT_r], W=[catT_r])

        nit = len(items)
        for i in range(nit + 2):
            if i < nit:
                stA(i)
                stB(i)
            if 0 <= i - 1 < nit:
                stC(i - 1)
            if 0 <= i - 2 < nit:
                stE(i - 2)
        for dc in range(8):
            pp, pp_r = (PA, PB)[dc % 2]
            for kc in range(8):
                k.I("pe", lambda e: e.matmul(pp[:], wout[:, kc, dc * 128:(dc + 1) * 128], catT[:, kc, :],
                                             start=(kc == 0), stop=(kc == 7)),
                    R=[wout_r, catT_r], W=[pp_r], inc=(kc == 7))
            k.I("dve", lambda e: e.tensor_tensor(out=h[:, dc, :], in0=pp[:], in1=h[:, dc, :], op=ALU.add),
                R=[pp_r, h_r], W=[h_r])
        for c in range(8):
            k.dma("sp", h1[c, :, t0:t0 + T], h[:, c, :], R=[h_r])


def g_xattn(k, nc, C, h_in, h_out, memT_d, wq_d, wkv_d, wo_d, layer, bf=None):
    (PA, PB, PS0, PS1, PO0, PO1, PM) = C.P
    L = str(layer)
    wq, wq_r = k.sb("xwq", [128, 8, 1024], BF16)
    wo, wo_r = k.sb("xwo", [128, 8, 1024], BF16)
    wkv, wkv_r = k.sb("xwkv", [128, 8, 2048], BF16)
    memf, memf_r = k.sb("memf", [128, 8, 256], F32)
    memn, memn_r = k.sb("memn", [128, 8, 256], BF16)
    KxT, KxT_r = k.sb("KxT", [128, 8, 256], BF16)
    Vx, Vx_r = k.sb("Vx", [128, 2, 1024], BF16)
    h, h_r = k.sb("xh", [128, 8, T], F32)
    hn, hn_r = k.sb("xhn", [128, 8, T], BF16)
    sq, _ = k.sb("xsq", [128, 2, T], BF16)
    sq_r = [Res(), Res()]
    rstd, rstd_r = k.sb("xrstd", [128, T], F32)
    qx, qx_r = k.sb("qx", [128, 8, T], BF16)
    oT, oT_r = k.sb("oT", [128, 8, T], BF16)
    PTb_ = [k.sb(f"xPT{i}", [128, T], BF16) for i in range(2)]
    rden, rden_r = k.sb("rden", [128, T], F32)
    r2, r2_r = k.sb("xr2", [128, T], F32)

    if bf is None:
        load_w_bf16(k, wkv, wkv_r, wkv_d, 8, 2048)
        load_w_bf16(k, wq, wq_r, wq_d, 8, 1024)
        load_w_bf16(k, wo, wo_r, wo_d, 8, 1024)
    else:
        load_bf(k, wkv, wkv_r, bf["wkv"], 0, 2048)
        load_bf(k, wq, wq_r, bf["wq"], 0, 1024)
        load_bf(k, wo, wo_r, bf["wo"], 0, 1024)
    for c in range(8):
        k.dma("sp", memf[:, c, :], memT_d[c, :, :], W=[memf_r])
    rmsnorm_T(k, C, memf, memf_r, "mem" + L, memn, memn_r, sq, sq_r, rstd, rstd_r, PM, n=256)
    for hh in range(4):
        for cc in range(2):
            pp, pp_r = (PA, PB)[cc]
            fc = 2 * hh + cc
            for kc in range(8):
                k.I("pe", lambda e: e.matmul(pp[:, 0:256], wkv[:, kc, fc * 128:(fc + 1) * 128], memn[:, kc, :],
                                             start=(kc == 0), stop=(kc == 7)), R=[wkv_r, memn_r], W=[pp_r], inc=(kc == 7))
            k.I("act", lambda e: e.activation(out=sq[:, cc, 0:256], in_=pp[:, 0:256], func=AF.Square), R=[pp_r], W=[sq_r[cc]])
            k.I("pe", lambda e: e.matmul(PM[0][:, 0:256], cmb(C, "ones"), sq[:, cc, 0:256], start=(cc == 0), stop=(cc == 1)),
                R=[sq_r[cc], C.cmb_r], W=[PM[1]])
        k.I("act", lambda e: e.activation(out=r2[:, 0:256], in_=PM[0][:, 0:256], func=AF.Ln, bias=EPS, scale=1.0 / 256),
            R=[PM[1]], W=[r2_r])
        k.I("act", lambda e: e.activation(out=r2[:, 0:256], in_=r2[:, 0:256], func=AF.Exp, scale=-0.5), R=[r2_r], W=[r2_r])
        for cc in range(2):
            pp, pp_r = (PA, PB)[cc]
            k.I("dve", lambda e: e.scalar_tensor_tensor(out=KxT[:, 2 * hh + cc, :], in0=pp[:, 0:256], scalar=col(C, "xak" + L, cc),
                                                        in1=r2[:, 0:256], op0=ALU.mult, op1=ALU.mult),
                R=[pp_r, r2_r, C.cst_r], W=[KxT_r])
    for mt in range(2):
        for half in range(2):
            pp, pp_r = (PA, PB)[half]
            for kc in range(8):
                k.I("pe", lambda e: e.matmul(pp[:], memn[:, kc, mt * 128:(mt + 1) * 128],
                                             wkv[:, kc, 1024 + half * 512:1024 + (half + 1) * 512],
                                             start=(kc == 0), stop=(kc == 7)), R=[wkv_r, memn_r], W=[pp_r], inc=(kc == 7))
            k.I("act", lambda e: e.copy(out=Vx[:, mt, half * 512:(half + 1) * 512], in_=pp[:]), R=[pp_r], W=[Vx_r])

    hB = [(h, h_r), k.sb("xh2", [128, 8, T], F32)]
    hnB = [(hn, hn_r), k.sb("xhn2", [128, 8, T], BF16)]
    oTB = [(oT, oT_r), k.sb("oT2", [128, 8, T], BF16)]
    sqh = [[k.sb(f"xsqh{i}{c}", [128, T], BF16) for c in range(2)] for i in range(2)]
    r2B = [(r2, r2_r), k.sb("xr2b", [128, T], F32)]
    rdB = [(rden, rden_r), k.sb("rden2", [128, T], F32)]
    PTB = [PTb_, [k.sb(f"xPTb{i}", [128, T], BF16) for i in range(2)]]
    PD = (C.PT[0][:, :].bitcast(F32), C.PT[1])
    N = NT * 4

    def S0load(t):
        h_, h_r_ = hB[t % 2]
        for c in range(8):
            k.dma("sp", h_[:, c, :], h_in[c, :, t * T:(t + 1) * T], W=[h_r_])

    def S0(t):
        h_, h_r_ = hB[t % 2]
        hn_, hn_r_ = hnB[t % 2]
        rmsnorm_T(k, C, h_, h_r_, "xa" + L, hn_, hn_r_, sq, sq_r, rstd, rstd_r, PM)

    def S1(i):
        t, hh = divmod(i, 4)
        hn_, hn_r_ = hnB[t % 2]
        for cc in range(2):
            pp, pp_r = (PA, PB)[cc]
            fc = 2 * hh + cc
            s_, s_r = sqh[i % 2][cc]
            for kc in range(8):
                k.I("pe", lambda e: e.matmul(pp[:], wq[:, kc, fc * 128:(fc + 1) * 128], hn_[:, kc, :],
                                             start=(kc == 0), stop=(kc == 7)), R=[wq_r, hn_r_], W=[pp_r], inc=(kc == 7))
            k.I("act", lambda e: e.activation(out=s_[:], in_=pp[:], func=AF.Square), R=[pp_r], W=[s_r])
            k.I("pe", lambda e: e.matmul(PM[0][:], cmb(C, "ones"), s_[:], start=(cc == 0), stop=(cc == 1)),
                R=[s_r, C.cmb_r], W=[PM[1]])

    def S2(i):
        t, hh = divmod(i, 4)
        r_, r_r = r2B[i % 2]
        k.I("act", lambda e: e.activation(out=r_[:], in_=PM[0][:], func=AF.Ln, bias=EPS, scale=1.0 / 256),
            R=[PM[1]], W=[r_r])
        k.I("act", lambda e: e.activation(out=r_[:], in_=r_[:], func=AF.Exp, scale=-0.5), R=[r_r], W=[r_r])
        for cc in range(2):
            pp, pp_r = (PA, PB)[cc]
            k.I("dve", lambda e: e.scalar_tensor_tensor(out=qx[:, 2 * hh + cc, :], in0=pp[:], scalar=col(C, "xaq" + L, cc),
                                                        in1=r_[:], op0=ALU.mult, op1=ALU.mult),
                R=[pp_r, r_r, C.cst_r], W=[qx_r])

    def S3(i):
        t, hh = divmod(i, 4)
        for mt in range(2):
            ps_, ps_r = (PS0, PS1)[mt]
            pb, pb_r = PTB[i % 2][mt]
            for cc in range(2):
                k.I("pe", lambda e: e.matmul(ps_[:], KxT[:, 2 * hh + cc, mt * 128:(mt + 1) * 128], qx[:, 2 * hh + cc, :],
                                             start=(cc == 0), stop=(cc == 1)), R=[KxT_r, qx_r], W=[ps_r], inc=(cc == 1))
            k.I("act", lambda e: e.activation(out=pb[:], in_=ps_[:], func=AF.Exp, scale=1.0 / 16), R=[ps_r], W=[pb_r])

    def S4(i):
        t, hh = divmod(i, 4)
        oT_, oT_r_ = oTB[t % 2]
        rd, rd_r = rdB[i % 2]
        for mt in range(2):
            pb, pb_r = PTB[i % 2][mt]
            k.I("pe", lambda e: e.matmul(PD[0][:, 0:512], cmb(C, "ones"), pb[:], start=(mt == 0), stop=(mt == 1)),
                R=[pb_r, C.cmb_r], W=[PD[1]], inc=(mt == 1))
        k.I("act", lambda e: e.activation(out=rd[:], in_=PD[0][:, 0:512], func=AF.Ln), R=[PD[1]], W=[rd_r])
        k.I("act", lambda e: e.activation(out=rd[:], in_=rd[:], func=AF.Exp, scale=-1.0), R=[rd_r], W=[rd_r])
        for cc in range(2):
            po, po_r = (PO0, PO1)[cc]
            for mt in range(2):
                pb, pb_r = PTB[i % 2][mt]
                k.I("pe", lambda e: e.matmul(po[:], Vx[:, mt, (2 * hh + cc) * 128:(2 * hh + cc + 1) * 128], pb[:],
                                             start=(mt == 0), stop=(mt == 1)), R=[Vx_r, pb_r], W=[po_r], inc=(mt == 1))
            k.I("dve", lambda e: e.tensor_tensor(out=oT_[:, 2 * hh + cc, :], in0=po[:], in1=rd[:], op=ALU.mult),
                R=[po_r, rd_r], W=[oT_r_])
        if hh == 3:
            h_, h_r_ = hB[t % 2]
            for dc in range(8):
                pp, pp_r = (PO0, PO1)[dc % 2]
                for kc in range(8):
                    k.I("pe", lambda e: e.matmul(pp[:], wo[:, kc, dc * 128:(dc + 1) * 128], oT_[:, kc, :],
                                                 start=(kc == 0), stop=(kc == 7)), R=[wo_r, oT_r_], W=[pp_r], inc=(kc == 7))
                k.I("dve", lambda e: e.tensor_tensor(out=h_[:, dc, :], in0=pp[:], in1=h_[:, dc, :], op=ALU.add),
                    R=[pp_r, h_r_], W=[h_r_])
            for c in range(8):
                k.dma("sp", h_out[c, :, t * T:(t + 1) * T], h_[:, c, :], R=[h_r_])

    S0load(0)
    for i in range(N + 2):
        if i < N:
            if i % 4 == 0:
                S0(i // 4)
            S1(i)
            S2(i)
        if 0 <= i - 1 < N:
            S3(i - 1)
        if 0 <= i - 2 < N:
            S4(i - 2)
        if i < N and i % 4 == 1 and i // 4 + 1 < NT:
            S0load(i // 4 + 1)


def g_ffn(k, nc, C, h_in, h_out, wg_d, wu_d, wd_d, bf=None):
    (PA, PB, PS0, PS1, PO0, PO1, PM) = C.P
    NF = 22
    wg, wg_r = k.sb("fwg", [128, 8, 2816], BF16)
    wu, wu_r = k.sb("fwu", [128, 8, 2816], BF16)
    wd, wd_r = k.sb("fwd", [128, NF, 1024], BF16)
    h, h_r = k.sb("fh", [128, 8, T], F32)
    hn, hn_r = k.sb("fhn", [128, 8, T], BF16)
    sq, _ = k.sb("fsq", [128, 2, T], BF16)
    sq_r = [Res(), Res()]
    rstd, rstd_r = k.sb("frstd", [128, T], F32)
    act, act_r = k.sb("fact", [128, NF, T], BF16)
    sg = [k.sb(f"fsg{i}", [128, T], BF16) for i in range(2)]
    if bf is None:
        load_w_bf16(k, wg, wg_r, wg_d, 8, 2816)
        load_w_bf16(k, wu, wu_r, wu_d, 8, 2816)
        load_w_bf16(k, wd, wd_r, wd_d, NF, 1024)
    else:
        for c0 in range(0, 2816, 704):
            k.dma("sp", wg[:, :, c0:c0 + 704], bf["wg"][0].rearrange("(c p) f -> p c f", p=128)[:, :, c0:c0 + 704],
                  R=[bf["wg"][1]], W=[wg_r])
            k.dma("sp", wu[:, :, c0:c0 + 704], bf["wu"][0].rearrange("(c p) f -> p c f", p=128)[:, :, c0:c0 + 704],
                  R=[bf["wu"][1]], W=[wu_r])
        for f0 in range(0, NF, 11):
            k.dma("sp", wd[:, f0:f0 + 11, :], bf["wd"][0].rearrange("(c p) f -> p c f", p=128)[:, f0:f0 + 11, :],
                  R=[bf["wd"][1]], W=[wd_r])
    hB = [(h, h_r), (h, h_r)]
    hnB = [(hn, hn_r), k.sb("fhn2", [128, 8, T], BF16)]
    rst = [k.sb(f"frst{i}", [128, T], F32) for i in range(2)]

    def prologue(t):
        h_, h_r_ = hB[t % 2]
        hn_, hn_r_ = hnB[t % 2]
        for c in range(8):
            k.dma("sp", h_[:, c, :], h_in[c, :, t * T:(t + 1) * T], W=[h_r_])
        rmsnorm_T(k, C, h_, h_r_, "ffn0", hn_, hn_r_, sq, sq_r, rstd, rstd_r, PM)

    prologue(0)
    for t in range(NT):
        t0 = t * T
        h_, h_r_ = hB[t % 2]
        hn_, hn_r_ = hnB[t % 2]
        for fc in range(NF):
            (pg, pg_r), (pu, pu_r) = ((PA, PB), (PS0, PS1))[fc % 2]
            for kc in range(8):
                k.I("pe", lambda e: e.matmul(pg[:], wg[:, kc, fc * 128:(fc + 1) * 128], hn_[:, kc, :],
                                             start=(kc == 0), stop=(kc == 7)), R=[wg_r, hn_r_], W=[pg_r], inc=(kc == 7))
            for kc in range(8):
                k.I("pe", lambda e: e.matmul(pu[:], wu[:, kc, fc * 128:(fc + 1) * 128], hn_[:, kc, :],
                                             start=(kc == 0), stop=(kc == 7)), R=[wu_r, hn_r_], W=[pu_r], inc=(kc == 7))
            s_, s_r = sg[fc % 2]
            k.I("act", lambda e: e.activation(out=s_[:], in_=pg[:], func=AF.Silu), R=[pg_r], W=[s_r])
            k.I("dve", lambda e: e.tensor_tensor(out=act[:, fc, :], in0=pu[:], in1=s_[:], op=ALU.mult),
                R=[pu_r, s_r], W=[act_r])
            if fc == NF // 2 and t + 1 < NT:
                prologue(t + 1)
        for dc in range(8):
            pp, pp_r = (PO0, PO1)[dc % 2]
            for fc in range(NF):
                k.I("pe", lambda e: e.matmul(pp[:], wd[:, fc, dc * 128:(dc + 1) * 128], act[:, fc, :],
                                             start=(fc == 0), stop=(fc == NF - 1)), R=[wd_r, act_r], W=[pp_r], inc=(fc == NF - 1))
            r_, r_r = rst[dc % 2]
            k.dma("sp", r_[:], h_in[dc, :, t0:t0 + T], W=[r_r])
            k.I("dve", lambda e: e.tensor_tensor(out=r_[:], in0=pp[:], in1=r_[:], op=ALU.add),
                R=[pp_r, r_r], W=[r_r])
            k.dma("sp", h_out[dc, :, t0:t0 + T], r_[:], R=[r_r])


def g_moe(k, nc, C, h_in, h_out, wr_d, wg_d, wu_d, wd_d, n_exp=8):
    (PA, PB, PS0, PS1, PO0, PO1, PM) = C.P
    SUP = min(2048, NT * T)
    NSUP = (NT * T) // SUP
    NSB = SUP // 128
    NTT = SUP // T
    hs, hs_r = k.sb("mhs", [128, 8, SUP], F32)
    hn, hn_r = k.sb("mhn", [128, 8, SUP], BF16)
    gbc, gbc_r = k.sb("mgbc", [128, 8, SUP], BF16)
    wgs = [k.sb(f"mwg{i}", [128, 8, 512], BF16) for i in range(2)]
    wus = [k.sb(f"mwu{i}", [128, 8, 512], BF16) for i in range(2)]
    wds = [k.sb(f"mwd{i}", [128, 4, 1024], BF16) for i in range(2)]
    act = [k.sb(f"mact{i}", [128, 4, T], BF16) for i in range(2)]
    sg = [k.sb(f"msg{i}", [128, T], BF16) for i in range(2)]
    sg2 = [k.sb(f"msg2{i}", [128, T], BF16) for i in range(2)]
    sq, _ = k.sb("msq", [128, 2, T], BF16)
    sq_r = [Res(), Res()]
    rstd, rstd_r = k.sb("mrstd", [128, T], F32)
    wr, wr_r = k.sb("mwr", [128, 8, 8], BF16)
    Lg, Lg_r = k.sb("mL", [128, NSB, 8], F32)
    Eg, Eg_r = k.sb("mE", [128, NSB, 8], F32)
    Sg, Sg_r = k.sb("mS", [128, NSB, 8], F32)
    m8, m8_r = k.sb("mm8", [128, NSB, 8], F32)
    rd, rd_r = k.sb("mrd", [128, NSB], F32)
    Gb, Gb_r = k.sb("mGb", [128, NSB, 8], BF16)
    Gbig, Gbig_r = k.sb("mGbig", [128, 128], BF16)
    for kc in range(8):
        k.dma("pool", wr[:, kc, :], wr_d[kc * 128:(kc + 1) * 128, :], W=[wr_r])
    wcount = 0
    for sp_ in range(NSUP):
        s0 = sp_ * SUP
        for c in range(8):
            k.dma("sp", hs[:, c, :], h_in[c, :, s0:s0 + SUP], W=[hs_r])
        for tt in range(NTT):
            hv = hs[:, :, tt * T:(tt + 1) * T]
            hnv = hn[:, :, tt * T:(tt + 1) * T]
            rmsnorm_T(k, C, hv, hs_r, "ffn1", hnv, hn_r, sq, sq_r, rstd, rstd_r, PM)
        for sb_ in range(NSB):
            for kc in range(8):
                k.I("pe", lambda e: e.matmul(PM[0][:, sb_ * 8:(sb_ + 1) * 8], hn[:, kc, sb_ * 128:(sb_ + 1) * 128], wr[:, kc, :],
                                             start=(kc == 0), stop=(kc == 7)), R=[hn_r, wr_r], W=[PM[1]],
                    inc=(kc == 7 and sb_ == NSB - 1))
        k.I("dve", lambda e: e.tensor_copy(out=Lg[:], in_=PM[0][:, 0:NSB * 8].rearrange("p (s e) -> p s e", e=8)),
            R=[PM[1]], W=[Lg_r])
        for sb_ in range(NSB):
            k.I("dve", lambda e: e.max(out=m8[:, sb_, :], in_=Lg[:, sb_, :]), R=[Lg_r], W=[m8_r])
        k.I("dve", lambda e: e.tensor_tensor(out=Eg[:], in0=Lg[:], in1=m8[:, :, 0:1].to_broadcast([128, NSB, 8]), op=ALU.subtract),
            R=[Lg_r, m8_r], W=[Eg_r])
        k.I("act", lambda e: e.activation(out=Eg[:], in_=Eg[:], func=AF.Exp), R=[Eg_r], W=[Eg_r])
        k.I("dve", lambda e: e.tensor_tensor(out=Sg[:], in0=Lg[:], in1=m8[:, :, 1:2].to_broadcast([128, NSB, 8]), op=ALU.is_ge),
            R=[Lg_r, m8_r], W=[Sg_r])
        k.I("dve", lambda e: e.tensor_tensor(out=Eg[:], in0=Eg[:], in1=Sg[:], op=ALU.mult), R=[Eg_r, Sg_r], W=[Eg_r])
        k.I("dve", lambda e: e.tensor_tensor(out=rd[:], in0=m8[:, :, 1], in1=m8[:, :, 0], op=ALU.subtract), R=[m8_r], W=[rd_r])
        k.I("act", lambda e: e.activation(out=rd[:], in_=rd[:], func=AF.Exp), R=[rd_r], W=[rd_r])
        k.I("dve", lambda e: e.tensor_scalar(out=rd[:], in0=rd[:], scalar1=1.0, scalar2=None, op0=ALU.add), R=[rd_r], W=[rd_r])
        k.I("dve", lambda e: e.reciprocal(out=rd[:], in_=rd[:]), R=[rd_r], W=[rd_r])
        k.I("dve", lambda e: e.tensor_tensor(out=Gb[:], in0=Eg[:], in1=rd[:].rearrange("p (s o) -> p s o", o=1).to_broadcast([128, NSB, 8]),
                                              op=ALU.mult), R=[Eg_r, rd_r], W=[Gb_r])
        for ex in range(n_exp):
            for sb_ in range(NSB):
                pp, pp_r = (PA, PB)[(sb_ // 4) % 2]
                k.I("pe", lambda e: e.matmul(pp[:, (sb_ % 4) * 128:(sb_ % 4 + 1) * 128],
                                             Gb[:, sb_, ex:ex + 1].to_broadcast([128, 128]), cmb(C, "ident"), start=True, stop=True),
                    R=[Gb_r, C.cmb_r], W=[pp_r], inc=(sb_ % 4 == 3))
                if sb_ % 4 == 3:
                    k.I("act", lambda e: e.copy(out=gbc[:, ex, (sb_ - 3) * 128:(sb_ + 1) * 128], in_=pp[:]), R=[pp_r], W=[gbc_r])
        pend = None

        def gu(ex, wset, tt, icnt):
            (wg_, wg_r), (wu_, wu_r), (wd_, wd_r) = wset
            a_, a_r = act[icnt % 2]
            tsl = slice(tt * T, (tt + 1) * T)
            for fc in range(4):
                (pg, pg_r), (pu, pu_r) = ((PA, PB), (PS0, PS1))[fc % 2]
                for kc in range(8):
                    k.I("pe", lambda e: e.matmul(pg[:], wg_[:, kc, fc * 128:(fc + 1) * 128], hn[:, kc, tsl],
                                                 start=(kc == 0), stop=(kc == 7)), R=[wg_r, hn_r], W=[pg_r], inc=(kc == 7))
                for kc in range(8):
                    k.I("pe", lambda e: e.matmul(pu[:], wu_[:, kc, fc * 128:(fc + 1) * 128], hn[:, kc, tsl],
                                                 start=(kc == 0), stop=(kc == 7)), R=[wu_r, hn_r], W=[pu_r], inc=(kc == 7))
                s_, s_r = sg[fc % 2]
                s2, s2_r = sg2[fc % 2]
                k.I("act", lambda e: e.activation(out=s_[:], in_=pg[:], func=AF.Silu), R=[pg_r], W=[s_r])
                k.I("dve", lambda e: e.tensor_tensor(out=s2[:], in0=s_[:], in1=gbc[:, ex, tsl], op=ALU.mult),
                    R=[s_r, gbc_r], W=[s2_r])
                k.I("dve", lambda e: e.tensor_tensor(out=a_[:, fc, :], in0=pu[:], in1=s2[:], op=ALU.mult),
                    R=[pu_r, s2_r], W=[a_r])

        def down(wset, tt, icnt):
            (wd_, wd_r) = wset[2]
            a_, a_r = act[icnt % 2]
            tsl = slice(tt * T, (tt + 1) * T)
            for dc in range(8):
                pp, pp_r = (PO0, PO1)[dc % 2]
                for fc in range(4):
                    k.I("pe", lambda e: e.matmul(pp[:], wd_[:, fc, dc * 128:(dc + 1) * 128], a_[:, fc, :],
                                                 start=(fc == 0), stop=(fc == 3)), R=[wd_r, a_r], W=[pp_r], inc=(fc == 3))
                k.I("dve", lambda e: e.tensor_tensor(out=hs[:, dc, tsl], in0=pp[:], in1=hs[:, dc, tsl], op=ALU.add),
                    R=[pp_r, hs_r], W=[hs_r])

        icnt = 0
        for ex in range(n_exp):
            for fg in range(7):
                b = wcount % 2
                wcount += 1
                wset = (wgs[b], wus[b], wds[b])
                (wg_, wg_r), (wu_, wu_r), (wd_, wd_r) = wset
                f0 = fg * 512
                for kc in range(8):
                    k.dma("pool", wg_[:, kc, :], wg_d[ex, kc * 128:(kc + 1) * 128, f0:f0 + 512], W=[wg_r])
                    k.dma("pool", wu_[:, kc, :], wu_d[ex, kc * 128:(kc + 1) * 128, f0:f0 + 512], W=[wu_r])
                for fc in range(4):
                    k.dma("pool", wd_[:, fc, :], wd_d[ex, f0 + fc * 128:f0 + (fc + 1) * 128, :], W=[wd_r])
                for tt in range(NTT):
                    gu(ex, wset, tt, icnt)
                    if pend is not None:
                        down(*pend)
                    pend = (wset, tt, icnt)
                    icnt += 1
        down(*pend)
        pend = None
        for c in range(8):
            k.dma("sp", h_out[c, :, s0:s0 + SUP], hs[:, c, :], R=[hs_r])


def cmf(C, name):
    o = CM[name]
    return C.cmf[:, o:o + 128]


def g_ssd(k, nc, C, h_in, h_out, rowc_d, w_in_d, w_out_d, bf=None):
    (PA, PB, PS0, PS1, PY, PZ, PM) = C.P
    PTt, PT_r = C.PT
    wout, wout_r = k.sb("swout", [128, 16, 1024], BF16)
    wdt, wdt_r = k.sb("swdt", [128, 8, 32], BF16)
    NWB = 2 if bf is None else 3
    wblk = [k.sb(f"swb{i}", [128, 8, 512], BF16) for i in range(NWB)]
    rowc, rowc_r = k.sb("rowc", [128, 96], F32)
    abc, abc_r = k.sb("sabc", [128, 32], F32)
    h, h_r = k.sb("sh", [128, 8, T], F32)
    hn, hn_r = k.sb("shn", [128, 8, T], BF16)
    sq, _ = k.sb("ssq", [128, 2, T], BF16)
    sq_r = [Res(), Res()]
    rstd, rstd_r = k.sb("srstd", [128, T], F32)
    xr = [k.sb(f"sxr{i}", [128, 515], F32) for i in range(2)]
    acc = [k.sb(f"sacc{i}", [128, T], F32) for i in range(2)]
    halo, halo_r = k.sb("shalo", [128, 24, 3], F32)
    xbcT, xbcT_r = k.sb("sxbcT", [128, 24, T], BF16)
    zs, zs_r = k.sb("szs", [128, 4, 2048], BF16)
    dt, dt_r = k.sb("sdt", [128, 4, 32], F32)
    da, da_r = k.sb("sda", [128, 4, 32], F32)
    xtok, xtok_r = k.sb("sxtok", [128, 2048], BF16)
    btok, btok_r = k.sb("sbtok", [128, 512], BF16)
    acum, acum_r = k.sb("sacum", [128, 64], F32)
    ea, ea_r = k.sb("sea", [128, 32], F32)
    nacum, nacum_r = k.sb("snacum", [128, 32], F32)
    te, te_r = k.sb("ste", [128, 32], F32)
    cd, cd_r = k.sb("scd", [128, 32], F32)
    seg = [k.sb(f"sseg{i}", [128, 512], F32) for i in range(2)]
    mt_ = [k.sb(f"smt{i}", [128, 512], BF16) for i in range(2)]
    cbm, cbm_r = k.sb("scbm", [128, 4, 128], F32)
    xdt, xdt_r = k.sb("sxdt", [128, 2048], BF16)
    xw, xw_r = k.sb("sxw", [128, 2048], BF16)
    state, state_r = k.sb("sstate", [128, 2048], F32)
    stb, stb_r = k.sb("sstb", [128, 2048], BF16)
    tmp = [k.sb(f"stmp{i}", [128, 512], F32) for i in range(2)]
    tmp2 = [k.sb(f"stmq{i}", [128, 512], F32) for i in range(2)]
    yz, yz_r = k.sb("syz", [128, 512], F32)
    junk, junk_r = k.sb("sjunk", [128, 512], BF16)
    ssq, ssq_r = k.sb("sssq", [128, 4], F32)
    yn, yn_r = k.sb("syn", [128, 2048], BF16)
    ynT, ynT_r = k.sb("synT", [128, 16, T], BF16)

    if bf is None:
        load_w_bf16(k, wout, wout_r, w_out_d, 16, 1024)
    else:
        load_bf(k, wout, wout_r, bf["wout"], 0, 1024)
    for kc in range(8):
        k.dma("pool", wdt[:, kc, :], w_in_d[kc * 128:(kc + 1) * 128, 5120:5152], W=[wdt_r])
    k.dma("sp", rowc[:], rowc_d[:, :], W=[rowc_r])
    k.I("act", lambda e: e.activation(out=abc[:], in_=rowc[:, 32:64], func=AF.Exp), R=[rowc_r], W=[abc_r])
    k.I("dve", lambda e: e.tensor_scalar(out=abc[:], in0=abc[:], scalar1=-1.0, scalar2=None, op0=ALU.mult), R=[abc_r], W=[abc_r])
    for kc in range(16):
        k.I("dve", lambda e: e.tensor_scalar(out=wout[:, kc, :], in0=wout[:, kc, :], scalar1=col(C, "ssdn", kc), scalar2=None,
                                              op0=ALU.mult), R=[wout_r, C.cst_r], W=[wout_r])
    k.I("pool", lambda e: e.memset(halo[:], 0.0), W=[halo_r])
    k.I("pool", lambda e: e.memset(state[:], 0.0), W=[state_r])
    k.I("pool", lambda e: e.memset(stb[:], 0.0), W=[stb_r])
    wc = 0
    hcount = 0
    for t in range(NT):
        t0 = t * T
        for c in range(8):
            k.dma("sp", h[:, c, :], h_in[c, :, t0:t0 + T], W=[h_r])
        rmsnorm_T(k, C, h, h_r, "mix1", hn, hn_r, sq, sq_r, rstd, rstd_r, PM)
        for zb in range(4):
            wb, wb_r = wblk[wc % NWB]
            wc += 1
            if bf is None:
                for kc in range(8):
                    k.dma("pool", wb[:, kc, :], w_in_d[kc * 128:(kc + 1) * 128, zb * 512:(zb + 1) * 512], W=[wb_r])
            else:
                load_bf(k, wb, wb_r, bf["win"], zb * 512, (zb + 1) * 512)
            for qs in range(4):
                pp, pp_r = (PA, PB)[qs % 2]
                for kc in range(8):
                    k.I("pe", lambda e: e.matmul(pp[:], hn[:, kc, qs * 128:(qs + 1) * 128], wb[:, kc, :],
                                                 start=(kc == 0), stop=(kc == 7)), R=[hn_r, wb_r], W=[pp_r], inc=(kc == 7))
                k.I("act", lambda e: e.activation(out=zs[:, qs, zb * 512:(zb + 1) * 512], in_=pp[:], func=AF.Silu),
                    R=[pp_r], W=[zs_r])
        for xb in range(6):
            wb, wb_r = wblk[wc % NWB]
            wc += 1
            if bf is None:
                for kc in range(8):
                    k.dma("pool", wb[:, kc, :], w_in_d[kc * 128:(kc + 1) * 128, 2048 + xb * 512:2048 + (xb + 1) * 512], W=[wb_r])
            else:
                load_bf(k, wb, wb_r, bf["win"], 2048 + xb * 512, 2048 + (xb + 1) * 512)
            for c4 in range(4):
                ch = xb * 4 + c4
                pp, pp_r = (PA, PB)[c4 % 2]
                x_, x_r = xr[c4 % 2]
                a_, a_r = acc[c4 % 2]
                for kc in range(8):
                    k.I("pe", lambda e: e.matmul(pp[:], wb[:, kc, c4 * 128:(c4 + 1) * 128], hn[:, kc, :],
                                                 start=(kc == 0), stop=(kc == 7)), R=[hn_r, wb_r], W=[pp_r], inc=(kc == 7))
                k.I("act", lambda e: e.copy(out=x_[:, 3:515], in_=pp[:]), R=[pp_r], W=[x_r])
                k.I("act", lambda e: e.copy(out=x_[:, 0:3], in_=halo[:, ch, :]), R=[halo_r], W=[x_r])
                k.I("act", lambda e: e.copy(out=halo[:, ch, :], in_=x_[:, 512:515]), R=[x_r], W=[halo_r])
                cw = CST["convw"] + 4 * ch
                k.I("dve", lambda e: e.tensor_scalar(out=a_[:], in0=x_[:, 0:512], scalar1=C.cst[:, cw:cw + 1], scalar2=None, op0=ALU.mult),
                    R=[x_r, C.cst_r], W=[a_r])
                for i in range(1, 4):
                    k.I("dve", lambda e: e.scalar_tensor_tensor(out=a_[:], in0=x_[:, i:i + 512], scalar=C.cst[:, cw + i:cw + i + 1],
                                                                in1=a_[:], op0=ALU.mult, op1=ALU.add),
                        R=[x_r, C.cst_r], W=[a_r])
                k.I("act", lambda e: e.activation(out=xbcT[:, ch, :], in_=a_[:], func=AF.Silu, bias=col(C, "convb", ch)),
                    R=[a_r, C.cst_r], W=[xbcT_r])
        for qs in range(4):
            for kc in range(8):
                k.I("pe", lambda e: e.matmul(PM[0][:, qs * 32:(qs + 1) * 32], hn[:, kc, qs * 128:(qs + 1) * 128], wdt[:, kc, :],
                                             start=(kc == 0), stop=(kc == 7)), R=[hn_r, wdt_r], W=[PM[1]],
                    inc=(kc == 7 and qs == 3))
        k.I("dve", lambda e: e.tensor_tensor(out=dt[:], in0=PM[0][:, 0:128].rearrange("p (q h) -> p q h", q=4),
                                              in1=rowc[:, 0:32].rearrange("p (o h) -> p o h", o=1).to_broadcast([128, 4, 32]),
                                              op=ALU.add), R=[PM[1], rowc_r], W=[dt_r])
        k.I("act", lambda e: e.activation(out=dt[:], in_=dt[:], func=AF.Exp), R=[dt_r], W=[dt_r])
        k.I("act", lambda e: e.activation(out=dt[:], in_=dt[:], func=AF.Ln, bias=1.0), R=[dt_r], W=[dt_r])
        k.I("dve", lambda e: e.tensor_tensor(out=da[:], in0=dt[:],
                                              in1=abc[:].rearrange("p (o h) -> p o h", o=1).to_broadcast([128, 4, 32]), op=ALU.mult),
            R=[dt_r, abc_r], W=[da_r])
        def ch_pre(qs):
            cs = slice(qs * 128, (qs + 1) * 128)
            first = (t == 0 and qs == 0)
            for half in range(2):
                for j in range(8):
                    k.I("pe", lambda e: e.transpose(PTt[:, j * 128:(j + 1) * 128], xbcT[:, half * 8 + j, cs], cmb(C, "ident")),
                        R=[xbcT_r, C.cmb_r], W=[PT_r], inc=(j == 7))
                k.I("act", lambda e: e.copy(out=xtok[:, half * 1024:(half + 1) * 1024], in_=PTt[:]), R=[PT_r], W=[xtok_r])
            for j in range(4):
                k.I("pe", lambda e: e.transpose(PTt[:, j * 128:(j + 1) * 128], xbcT[:, 16 + j, cs], cmb(C, "ident")),
                    R=[xbcT_r, C.cmb_r], W=[PT_r], inc=(j == 3))
            k.I("act", lambda e: e.copy(out=btok[:], in_=PTt[:, 0:512]), R=[PT_r], W=[btok_r])
            k.I("pe", lambda e: e.matmul(PM[0][:, 0:32], cmf(C, "upper"), da[:, qs, :], start=True, stop=True),
                R=[da_r, C.cmf_r], W=[PM[1]])
            k.I("pe", lambda e: e.matmul(PM[0][:, 32:64], cmf(C, "ones"), da[:, qs, :], start=True, stop=True),
                R=[da_r, C.cmf_r], W=[PM[1]])
            k.I("act", lambda e: e.copy(out=acum[:], in_=PM[0][:, 0:64]), R=[PM[1]], W=[acum_r])
            k.I("act", lambda e: e.activation(out=ea[:], in_=acum[:, 0:32], func=AF.Exp), R=[acum_r], W=[ea_r])
            k.I("act", lambda e: e.mul(out=nacum[:], in_=acum[:, 0:32], mul=-1.0), R=[acum_r], W=[nacum_r])
            k.I("act", lambda e: e.activation(out=cd[:], in_=acum[:, 32:64], func=AF.Exp), R=[acum_r], W=[cd_r])
            k.I("dve", lambda e: e.tensor_tensor(out=te[:], in0=acum[:, 32:64], in1=acum[:, 0:32], op=ALU.subtract),
                R=[acum_r], W=[te_r])
            k.I("act", lambda e: e.activation(out=te[:], in_=te[:], func=AF.Exp), R=[te_r], W=[te_r])
            for g in range(4):
                pp, pp_r = (PA, PB)[g % 2]
                k.I("pe", lambda e: e.matmul(pp[:, 0:128], xbcT[:, 16 + g, cs], xbcT[:, 20 + g, cs], start=True, stop=True),
                    R=[xbcT_r], W=[pp_r])
                k.I("dve", lambda e: e.tensor_tensor(out=cbm[:, g, :], in0=pp[:, 0:128], in1=cmf(C, "upper"), op=ALU.mult),
                    R=[pp_r, C.cmf_r], W=[cbm_r])
            k.I("dve", lambda e: e.tensor_tensor(out=xdt[:].rearrange("p (h d) -> p h d", h=32),
                                                  in0=xtok[:].rearrange("p (h d) -> p h d", h=32),
                                                  in1=dt[:, qs, :].rearrange("p (h o) -> p h o", o=1).to_broadcast([128, 32, 64]),
                                                  op=ALU.mult), R=[xtok_r, dt_r], W=[xdt_r])
            k.I("dve", lambda e: e.tensor_tensor(out=xw[:].rearrange("p (h d) -> p h d", h=32),
                                                  in0=xdt[:].rearrange("p (h d) -> p h d", h=32),
                                                  in1=te[:].rearrange("p (h o) -> p h o", o=1).to_broadcast([128, 32, 64]),
                                                  op=ALU.mult), R=[xdt_r, te_r], W=[xw_r])
        def ch_heads(qs):
            cs = slice(qs * 128, (qs + 1) * 128)
            PYs = [PY, PA]
            PZs = [PZ, PB]

            def s1(q):
                ps_, ps_r = (PS0, PS1)[q % 2]
                for i in range(4):
                    hh = 4 * q + i
                    k.I("pe", lambda e: e.matmul(ps_[:, i * 128:(i + 1) * 128], da[:, qs, hh:hh + 1].to_broadcast([128, 128]),
                                                 cmf(C, "upper"), start=True, stop=True), R=[da_r, C.cmf_r], W=[ps_r], inc=(i == 3))

            def s2(q):
                sg_, sg_r = seg[q % 2]
                ps_, ps_r = (PS0, PS1)[q % 2]
                k.I("dve", lambda e: e.tensor_tensor(out=sg_[:].rearrange("p (h t) -> p h t", h=4),
                                                      in0=ps_[:].rearrange("p (h t) -> p h t", h=4),
                                                      in1=acum[:, 4 * q:4 * q + 4].rearrange("p (h o) -> p h o", o=1).to_broadcast([128, 4, 128]),
                                                      op=ALU.subtract), R=[ps_r, acum_r], W=[sg_r])
                k.I("act", lambda e: e.activation(out=sg_[:], in_=sg_[:], func=AF.Relu, scale=-1.0), R=[sg_r], W=[sg_r])
                k.I("act", lambda e: e.activation(out=sg_[:], in_=sg_[:], func=AF.Exp, scale=-1.0), R=[sg_r], W=[sg_r])

            def s3(q):
                g = q // 2
                gs = slice(g * 512, (g + 1) * 512)
                sg_, sg_r = seg[q % 2]
                m_, m_r = mt_[q % 2]
                py, py_r = PYs[g % 2]
                pz, pz_r = PZs[g % 2]
                k.I("dve", lambda e: e.tensor_tensor(out=m_[:].rearrange("p (h t) -> p h t", h=4),
                                                      in0=sg_[:].rearrange("p (h t) -> p h t", h=4),
                                                      in1=cbm[:, g:g + 1, :].to_broadcast([128, 4, 128]), op=ALU.mult),
                    R=[sg_r, cbm_r], W=[m_r])
                for i in range(4):
                    hh = 4 * q + i
                    hl = hh % 8
                    k.I("pe", lambda e: e.matmul(py[:, hl * 64:(hl + 1) * 64], m_[:, i * 128:(i + 1) * 128], xdt[:, hh * 64:(hh + 1) * 64],
                                                 start=True, stop=True), R=[m_r, xdt_r], W=[py_r], inc=(i == 3))
                if q % 2 == 0:
                    return
                k.I("pe", lambda e: e.matmul(pz[:], xbcT[:, 20 + g, cs], stb[:, gs], start=True, stop=True),
                    R=[xbcT_r, stb_r], W=[pz_r])
                t1_, t1_r = tmp[g % 2]
                t2_, t2_r = tmp2[g % 2]
                k.I("dve", lambda e: e.tensor_tensor(out=t1_[:].rearrange("p (h d) -> p h d", h=8),
                                                      in0=pz[:].rearrange("p (h d) -> p h d", h=8),
                                                      in1=ea[:, g * 8:(g + 1) * 8].rearrange("p (h o) -> p h o", o=1).to_broadcast([128, 8, 64]),
                                                      op=ALU.mult), R=[pz_r, ea_r], W=[t1_r])
                k.I("dve", lambda e: e.tensor_tensor(out=t1_[:], in0=py[:], in1=t1_[:], op=ALU.add), R=[py_r, t1_r], W=[t1_r])
                k.I("dve", lambda e: e.tensor_tensor(out=t2_[:].rearrange("p (h d) -> p h d", h=8),
                                                      in0=xtok[:, gs].rearrange("p (h d) -> p h d", h=8),
                                                      in1=rowc[:, 64 + g * 8:64 + (g + 1) * 8].rearrange("p (h o) -> p h o", o=1).to_broadcast([128, 8, 64]),
                                                      op=ALU.mult), R=[xtok_r, rowc_r], W=[t2_r])
                k.I("dve", lambda e: e.tensor_tensor(out=t1_[:], in0=t1_[:], in1=t2_[:], op=ALU.add), R=[t1_r, t2_r], W=[t1_r])
                k.I("dve", lambda e: e.tensor_tensor(out=t2_[:], in0=t1_[:], in1=zs[:, qs, gs], op=ALU.mult), R=[t1_r, zs_r], W=[t2_r])
                k.I("act", lambda e: e.activation(out=junk[:], in_=t2_[:], func=AF.Square, accum_out=ssq[:, g:g + 1]),
                    R=[t2_r], W=[junk_r, ssq_r])
                k.I("act", lambda e: e.activation(out=ssq[:, g:g + 1], in_=ssq[:, g:g + 1], func=AF.Ln, bias=EPS, scale=1.0 / 512),
                    R=[ssq_r], W=[ssq_r])
                k.I("act", lambda e: e.activation(out=ssq[:, g:g + 1], in_=ssq[:, g:g + 1], func=AF.Exp, scale=-0.5),
                    R=[ssq_r], W=[ssq_r])
                k.I("act", lambda e: e.activation(out=yn[:, gs], in_=t2_[:], func=AF.Copy, scale=ssq[:, g:g + 1]),
                    R=[t2_r, ssq_r], W=[yn_r])
                k.I("pe", lambda e: e.matmul(pz[:], btok[:, g * 128:(g + 1) * 128], xw[:, gs], start=True, stop=True),
                    R=[btok_r, xw_r], W=[pz_r])
                k.I("pool", lambda e: e.tensor_tensor(out=state[:, gs].rearrange("p (h d) -> p h d", h=8),
                                                       in0=state[:, gs].rearrange("p (h d) -> p h d", h=8),
                                                       in1=cd[:, g * 8:(g + 1) * 8].rearrange("p (h o) -> p h o", o=1).to_broadcast([128, 8, 64]),
                                                       op=ALU.mult), R=[cd_r], W=[state_r])
                k.I("dve", lambda e: e.tensor_tensor(out=state[:, gs], in0=pz[:], in1=state[:, gs], op=ALU.add),
                    R=[pz_r], W=[state_r])
                k.I("act", lambda e: e.copy(out=stb[:, gs], in_=state[:, gs]), R=[state_r], W=[stb_r])

            for j in range(8 + 2):
                if j < 8:
                    s1(j)
                if 0 <= j - 1 < 8:
                    s2(j - 1)
                if 0 <= j - 2 < 8:
                    s3(j - 2)
        def ch_post(qs):
            cs = slice(qs * 128, (qs + 1) * 128)
            for half in range(2):
                for j in range(8):
                    k.I("pe", lambda e: e.transpose(PTt[:, j * 128:(j + 1) * 128], yn[:, (half * 8 + j) * 128:(half * 8 + j + 1) * 128],
                                                    cmb(C, "ident")), R=[yn_r, C.cmb_r], W=[PT_r], inc=(j == 7))
                k.I("act", lambda e: e.copy(out=ynT[:, half * 8:(half + 1) * 8, cs], in_=PTt[:].rearrange("p (c q) -> p c q", c=8)),
                    R=[PT_r], W=[ynT_r])
        ch_pre(0)
        for qs in range(4):
            ch_heads(qs)
            if qs + 1 < 4:
                ch_pre(qs + 1)
            ch_post(qs)
        for dc in range(8):
            pp, pp_r = (PA, PB)[dc % 2]
            for kc in range(16):
                k.I("pe", lambda e: e.matmul(pp[:], wout[:, kc, dc * 128:(dc + 1) * 128], ynT[:, kc, :],
                                             start=(kc == 0), stop=(kc == 15)), R=[wout_r, ynT_r], W=[pp_r], inc=(kc == 15))
            k.I("dve", lambda e: e.tensor_tensor(out=h[:, dc, :], in0=pp[:], in1=h[:, dc, :], op=ALU.add),
                R=[pp_r, h_r], W=[h_r])
        for c in range(8):
            k.dma("sp", h_out[c, :, t0:t0 + T], h[:, c, :], R=[h_r])


_CACHE = {}


def build_program():
    nc = bass.Bass("TRN2", target_bir_lowering=False)
    def din(name, shape, dt=F32):
        return nc.dram_tensor(name, list(shape), dt, kind="ExternalInput").ap()
    xT = din("xT", [8, 128, S]); memT = din("memT", [8, 128, 256]); pos = din("pos", [S], I32)
    cst_d = din("cst", [128, NCST]); cm_d = din("cm", [128, NCM]); rowc_d = din("rowc", [128, 96])
    hy_w_in = din("hy_w_in", [1024, 2048]); hy_w_out = din("hy_w_out", [1024, 1024]); pool_w = din("pool_w", [4, 128, 128])
    xa_wq = [din(f"xa_wq{l}", [1024, 1024]) for l in range(2)]
    xa_wkv = [din(f"xa_wkv{l}", [1024, 2048]) for l in range(2)]
    xa_wo = [din(f"xa_wo{l}", [1024, 1024]) for l in range(2)]
    ffn_wg = din("ffn_wg", [1024, 2816]); ffn_wu = din("ffn_wu", [1024, 2816]); ffn_wd = din("ffn_wd", [2816, 1024])
    ssd_w_in = din("ssd_w_in", [1024, 5152]); ssd_w_out = din("ssd_w_out", [2048, 1024])
    moe_wr = din("moe_wr", [1024, 8]); moe_wg = din("moe_wg", [8, 1024, 3584]); moe_wu = din("moe_wu", [8, 1024, 3584])
    moe_wd = din("moe_wd", [8, 3584, 1024])
    hs_ = [nc.dram_tensor(f"hscr{i}", [8, 128, S], F32, kind="Internal").ap() for i in range(5)]
    outT = nc.dram_tensor("outT", [8, 128, S], F32, kind="ExternalOutput").ap()
    with ExitStack() as es:
        k = KB(nc, es)
        C = setup_common(k, nc, cst_d, cm_d)
        def group(fn):
            with ExitStack() as ges:
                k.es = ges
                fn()
                k.barrier()
        BFW = {}

        def casts():
            for l in range(2):
                BFW[f"wq{l}"] = cast_w(k, nc, xa_wq[l], 1024, 1024, f"bf_wq{l}")
                BFW[f"wkv{l}"] = cast_w(k, nc, xa_wkv[l], 1024, 2048, f"bf_wkv{l}")
                BFW[f"wo{l}"] = cast_w(k, nc, xa_wo[l], 1024, 1024, f"bf_wo{l}")
                if l == 0:
                    BFW["wg"] = cast_w(k, nc, ffn_wg, 1024, 2816, "bf_wg")
                    BFW["wu"] = cast_w(k, nc, ffn_wu, 1024, 2816, "bf_wu")
                    BFW["wd"] = cast_w(k, nc, ffn_wd, 2816, 1024, "bf_wd")
                    BFW["win"] = cast_w(k, nc, ssd_w_in, 1024, 5152, "bf_win")
                    BFW["wout"] = cast_w(k, nc, ssd_w_out, 2048, 1024, "bf_wout")

        def xbf(l):
            return {"wq": BFW[f"wq{l}"], "wkv": BFW[f"wkv{l}"], "wo": BFW[f"wo{l}"]}

        group(lambda: g1_moba(k, nc, C, es, xT, hs_[0], pos, hy_w_in, hy_w_out, pool_w, after_loads=casts))
        group(lambda: g_xattn(k, nc, C, hs_[0], hs_[1], memT, xa_wq[0], xa_wkv[0], xa_wo[0], 0, bf=xbf(0)))
        group(lambda: g_ffn(k, nc, C, hs_[1], hs_[2], ffn_wg, ffn_wu, ffn_wd, bf=BFW))
        group(lambda: g_ssd(k, nc, C, hs_[2], hs_[3], rowc_d, ssd_w_in, ssd_w_out, bf=BFW))
        group(lambda: g_xattn(k, nc, C, hs_[3], hs_[4], memT, xa_wq[1], xa_wkv[1], xa_wo[1], 1, bf=xbf(1)))
        group(lambda: g_moe(k, nc, C, hs_[4], outT, moe_wr, moe_wg, moe_wu, moe_wd))
    return nc


def kernel(**inp):
    inp = {k_: np.asarray(v) for k_, v in inp.items()}
    if "nc" not in _CACHE:
        _CACHE["nc"] = build_program()
    nc = _CACHE["nc"]
    c, m = host_consts(inp)
    rowc = host_rowc(inp)
    f32 = lambda a: np.ascontiguousarray(a, dtype=np.float32)
    shared = {
        "pos": np.ascontiguousarray(inp["positions"], dtype=np.int32), "cst": c, "cm": m, "rowc": rowc,
        "hy_w_in": f32(inp["hy_w_in"][0]), "hy_w_out": f32(inp["hy_w_out"][0]), "pool_w": f32(inp["pool_w"][0]),
        "ffn_wg": f32(inp["ffn_w_gate"][0]), "ffn_wu": f32(inp["ffn_w_up"][0]), "ffn_wd": f32(inp["ffn_w_down"][0]),
        "ssd_w_in": f32(inp["ssd_w_in"][0]), "ssd_w_out": f32(inp["ssd_w_out"][0]),
        "moe_wr": f32(inp["moe_router"][0]), "moe_wg": f32(inp["moe_w_gate"][0]), "moe_wu": f32(inp["moe_w_up"][0]),
        "moe_wd": f32(inp["moe_w_down"][0]),
    }
    for l in range(2):
        shared[f"xa_wq{l}"] = f32(inp["xa_wq"][l]); shared[f"xa_wkv{l}"] = f32(inp["xa_wkv"][l]); shared[f"xa_wo{l}"] = f32(inp["xa_wo"][l])
    in_maps = []
    for b in range(8):
        mp = dict(shared)
        mp["xT"] = np.ascontiguousarray(inp["x"][b].T, dtype=np.float32).reshape(8, 128, S)
        mp["memT"] = np.ascontiguousarray(inp["mem"][b].T, dtype=np.float32).reshape(8, 128, 256)
        in_maps.append(mp)
    res = run_bass_kernel_spmd(nc, in_maps, core_ids=list(range(8)))
    out = np.empty((8, S, D), np.float32)
    for b in range(8):
        out[b] = np.asarray(res.results[b]["outT"]).reshape(D, S).T
    return out
```
